# Optimizing a Trainium2 kernel written in Bass

```python
import math
import jax
import jax.numpy as jnp
from jax import lax
import numpy as np

D_MODEL = 1024
BATCH = 8
SEQ = 2048
DEPTH = 4

GRID_W = 64
CTX_LEN = 256
N_MIXERS = 3
N_LAYERS_A = (DEPTH + 2) // 3
N_LAYERS_B = (DEPTH + 1) // 3
N_LAYERS_C = DEPTH // 3

ALPHA = (2.0 * DEPTH) ** 0.25
BETA = (8.0 * DEPTH) ** -0.25
LN_EPS = 1e-6

D_RNN = 1408
RG_BLOCKS = 16
RG_BS = D_RNN // RG_BLOCKS
RG_CONV = 4
RG_C = 8.0

D_M = 2 * D_MODEL
M_HEADS = 8
M_DK = 128
M_DV = D_M // M_HEADS
M_CONV = 4
CHUNK = 128

H_CONV = 3
H_EMB = 33
H_BANDS = (H_EMB - 1) // 2
H_FW = 64
H_DECAY_TARGET = 1e-2
H_FAST = 0.3
H_SLOW = 1.5

N_EXPERTS = 16
N_GROUPS = 4
EXP_PER_GROUP = N_EXPERTS // N_GROUPS
TOP_K = 2
D_EXPERT = 512

kernel_name = "hybrid_rglru_mlstm_hyena_moe_diffusion"


def layer_norm(x, g, b):
    xf = x.astype(jnp.float32)
    mu = jnp.mean(xf, -1, keepdims=True)
    var = jnp.mean(jnp.square(xf - mu), -1, keepdims=True)
    return ((xf - mu) * lax.rsqrt(var + LN_EPS) * g + b).astype(x.dtype)


def dwconv(x, w, b):
    k = w.shape[0]
    left = k // 2
    y = lax.conv_general_dilated(x, w[:, None, :], window_strides=(1,), padding=[(left, k - 1 - left)],
                                 dimension_numbers=("NWC", "WIO", "NWC"), feature_group_count=x.shape[-1])
    return y + b


def sincos_2d(n_tok, dtype):
    rows = n_tok // GRID_W
    quarter = D_MODEL // 4
    omega = 1.0 / (10000.0 ** (jnp.arange(quarter, dtype=jnp.float32) / quarter))
    ar = jnp.arange(rows, dtype=jnp.float32)[:, None] * omega
    ac = jnp.arange(GRID_W, dtype=jnp.float32)[:, None] * omega
    er = jnp.concatenate([jnp.sin(ar), jnp.cos(ar)], -1)
    ec = jnp.concatenate([jnp.sin(ac), jnp.cos(ac)], -1)
    half = D_MODEL // 2
    pos = jnp.concatenate([jnp.broadcast_to(er[:, None], (rows, GRID_W, half)),
                           jnp.broadcast_to(ec[None], (rows, GRID_W, half))], -1)
    return pos.reshape(rows * GRID_W, D_MODEL).astype(dtype)


def linear_scan(a, u, h0):
    def comb(lhs, rhs):
        a1, b1 = lhs
        a2, b2 = rhs
        return a1 * a2, a2 * b1 + b2
    a_cum, u_cum = lax.associative_scan(comb, (a, u), axis=1)
    return a_cum * h0[:, None, :] + u_cum


def rglru_scan(xc, w_a, b_a, w_x, b_x, lam, h0):
    bsz, L, _ = xc.shape
    xf = xc.astype(jnp.float32)
    xb = xf.reshape(bsz, L, RG_BLOCKS, RG_BS)
    r = jax.nn.sigmoid(jnp.einsum("blnk,nkj->blnj", xb, w_a.astype(jnp.float32)).reshape(bsz, L, D_RNN) + b_a)
    i = jax.nn.sigmoid(jnp.einsum("blnk,nkj->blnj", xb, w_x.astype(jnp.float32)).reshape(bsz, L, D_RNN) + b_x)
    log_a = -RG_C * r * jax.nn.softplus(-lam.astype(jnp.float32))
    u = jnp.sqrt(-jnp.expm1(2.0 * log_a)) * (i * xf)
    h = linear_scan(jnp.exp(log_a), u, h0)
    return h, h[:, -1]


def rglru_mixer(hc, hx, w_in, conv_w, conv_b, ga_w, ga_b, gx_w, gx_b, lam, w_out, need_ctx):
    def branches(h):
        gate, rec = jnp.split(h @ w_in, 2, axis=-1)
        return jax.nn.gelu(gate), dwconv(rec, conv_w, conv_b)

    gate_x, rx = branches(hx)
    if need_ctx:
        gate_c, rc = branches(hc)
    else:
        rc = dwconv(hc @ w_in[:, D_RNN:], conv_w, conv_b)
    z0 = jnp.zeros((hx.shape[0], D_RNN), jnp.float32)

    def direction(z, seq, h0, reverse):
        s = jnp.flip(seq, 1) if reverse else seq
        h, last = rglru_scan(s, ga_w[z], ga_b[z], gx_w[z], gx_b[z], lam[z], h0)
        return (jnp.flip(h, 1) if reverse else h), last

    hc_f, sc_f = direction(0, rc, z0, False)
    hx_f, _ = direction(0, rx, sc_f, False)
    hc_b, sc_b = direction(1, rc, z0, True)
    hx_b, _ = direction(1, rx, sc_b, True)
    yx = (gate_x * (hx_f + hx_b).astype(hx.dtype)) @ w_out
    yc = (gate_c * (hc_f + hc_b).astype(hc.dtype)) @ w_out if need_ctx else None
    return yc, yx


def mlstm_chunkwise(q, k, v, ig, lf, state0, return_h):
    bsz, nh, L, dk = q.shape
    dv = v.shape[-1]
    nc = L // CHUNK
    f32 = jnp.float32
    qc = q.astype(f32).reshape(bsz, nh, nc, CHUNK, dk)
    kc = k.astype(f32).reshape(bsz, nh, nc, CHUNK, dk)
    vc = v.astype(f32).reshape(bsz, nh, nc, CHUNK, dv)
    igc = ig.reshape(bsz, nh, nc, CHUNK)
    bcum = jnp.cumsum(lf.reshape(bsz, nh, nc, CHUNK), axis=-1)
    btot = bcum[..., -1]
    wlog = btot[..., None] - bcum + igc
    mloc = jnp.max(wlog, -1)
    wgt = jnp.exp(wlog - mloc[..., None])
    c_loc = jnp.einsum("bhntv,bhntk->bhnvk", vc * wgt[..., None], kc)
    n_loc = jnp.einsum("bhnt,bhntk->bhnk", wgt, kc)

    def step(carry, inp):
        C, n, m = carry
        cl, nl, ml, bt = inp
        m_new = jnp.maximum(bt + m, ml)
        sp = jnp.exp(bt + m - m_new)
        sl = jnp.exp(ml - m_new)
        new = (sp[..., None, None] * C + sl[..., None, None] * cl, sp[..., None] * n + sl[..., None] * nl, m_new)
        return new, (C, n, m)

    to_t = lambda a: jnp.moveaxis(a, 2, 0)
    final, prev = lax.scan(step, state0, (to_t(c_loc), to_t(n_loc), to_t(mloc), to_t(btot)))
    if not return_h:
        return None, final
    c_prev, n_prev, m_prev = [jnp.moveaxis(a, 0, 2) for a in prev]
    causal = jnp.tril(jnp.ones((CHUNK, CHUNK), dtype=bool))
    dlog = jnp.where(causal, bcum[..., :, None] - bcum[..., None, :] + igc[..., None, :], -jnp.inf)
    m_inter = bcum + m_prev[..., None]
    m_comb = jnp.maximum(m_inter, jnp.max(dlog, -1))
    s = jnp.einsum("bhntk,bhnsk->bhnts", qc, kc) * jnp.exp(dlog - m_comb[..., None])
    inter = jnp.exp(m_inter - m_comb)
    num = jnp.einsum("bhnts,bhnsv->bhntv", s, vc) + inter[..., None] * jnp.einsum("bhntk,bhnvk->bhntv", qc, c_prev)
    den = jnp.sum(s, -1) + inter * jnp.einsum("bhntk,bhnk->bhnt", qc, n_prev)
    h = num / jnp.maximum(jnp.abs(den), jnp.exp(-m_comb))[..., None]
    return h.reshape(bsz, nh, L, dv), final


def head_norm(h, g):
    mu = jnp.mean(h, -1, keepdims=True)
    var = jnp.mean(jnp.square(h - mu), -1, keepdims=True)
    hn = (h - mu) * lax.rsqrt(var + LN_EPS)
    bsz, nh, L, dv = h.shape
    return hn.transpose(0, 2, 1, 3).reshape(bsz, L, nh * dv) * g


def mlstm_mixer(hc, hx, w_up, conv_w, conv_b, w_q, w_k, w_v, w_o, w_if, b_if, norm_g, skip, w_down, need_ctx):
    def project(h):
        bsz, L, _ = h.shape
        xm = h @ w_up
        xc = jax.nn.silu(dwconv(xm, conv_w, conv_b))
        heads = lambda t, d: t.reshape(bsz, L, M_HEADS, d).transpose(0, 2, 1, 3)
        q = heads(xc @ w_q, M_DK)
        k = heads(xc @ w_k, M_DK) * (M_DK ** -0.5)
        v = heads(xm @ w_v, M_DV)
        g = (jnp.einsum("bld,zdg->zblg", xm, w_if) + b_if[:, None, None, :]).astype(jnp.float32)
        g = g.transpose(0, 1, 3, 2)
        ig = g[:, :, :M_HEADS]
        lf = jax.nn.log_sigmoid(g[:, :, M_HEADS:])
        return xm, xc, q, k, v, ig, lf

    xm_c, xc_c, qc, kc, vc, igc, lfc = project(hc)
    xm_x, xc_x, qx, kx, vx, igx, lfx = project(hx)
    bsz = hx.shape[0]
    st0 = (jnp.zeros((bsz, M_HEADS, M_DV, M_DK), jnp.float32), jnp.zeros((bsz, M_HEADS, M_DK), jnp.float32),
           jnp.zeros((bsz, M_HEADS), jnp.float32))

    def run(z, q, k, v, ig, lf, st, reverse, return_h):
        ig, lf = ig[z], lf[z]
        if reverse:
            q, k, v, ig, lf = [jnp.flip(a, 2) for a in (q, k, v, ig, lf)]
        h, last = mlstm_chunkwise(q, k, v, ig, lf, st, return_h)
        if reverse and return_h:
            h = jnp.flip(h, 2)
        return h, last

    hc_f, st_f = run(0, qc, kc, vc, igc, lfc, st0, False, need_ctx)
    hx_f, _ = run(0, qx, kx, vx, igx, lfx, st_f, False, True)
    hc_b, st_b = run(1, qc, kc, vc, igc, lfc, st0, True, need_ctx)
    hx_b, _ = run(1, qx, kx, vx, igx, lfx, st_b, True, True)

    def output(h, xm, xc):
        o = jax.nn.sigmoid(xm @ w_o)
        return (o * head_norm(h, norm_g).astype(xm.dtype) + skip * xc) @ w_down

    yx = output(hx_f + hx_b, xm_x, xc_x)
    yc = output(hc_f + hc_b, xm_c, xc_c) if need_ctx else None
    return yc, yx


def hyena_filters(L, w1, b1, fq1, w2, b2, fq2, w3):
    f32 = jnp.float32
    t01 = jnp.linspace(0.0, 1.0, L, dtype=f32)
    bands = jnp.linspace(1e-4, H_BANDS - 1, H_BANDS, dtype=f32)
    ang = (2.0 * math.pi / L) * jnp.arange(L, dtype=f32)[:, None] * bands[None, :]
    z = jnp.concatenate([t01[:, None], jnp.cos(ang), -jnp.sin(ang)], -1).astype(w1.dtype)
    hdn = jnp.sin(fq1 * (z @ w1 + b1))
    hdn = jnp.sin(fq2 * (hdn @ w2 + b2))
    filt = (hdn @ w3).reshape(L, 2, D_MODEL)
    dist = jnp.abs(jnp.arange(L) - L // 2).astype(f32) * (2.0 / L)
    d_max = math.log(H_DECAY_TARGET) / H_FAST
    d_min = math.log(H_DECAY_TARGET) / H_SLOW
    deltas = jnp.abs(jnp.linspace(d_min, d_max, D_MODEL, dtype=f32))
    window = jnp.exp(-dist[:, None] * deltas[None, :])
    return filt * window[:, None, :].astype(filt.dtype)


def fft_conv_centred(u, h, skip):
    L = u.shape[1]
    n = 2 * L
    uf = jnp.fft.rfft(u.astype(jnp.float32), n=n, axis=1)
    hf = jnp.fft.rfft(h.astype(jnp.float32), n=n, axis=0)
    y = jnp.fft.irfft(uf * hf[None], n=n, axis=1)[:, L // 2: L // 2 + L]
    return y.astype(u.dtype) + u * skip


def hyena_seq(h, w_in, b_in, conv_w, conv_b, f_w1, f_b1, f_fq1, f_w2, f_b2, f_fq2, f_w3, skip, w_out):
    L = h.shape[1]
    u = dwconv(h @ w_in + b_in, conv_w, conv_b)
    v, g1, g2 = jnp.split(u, 3, axis=-1)
    filt = hyena_filters(L, f_w1, f_b1, f_fq1, f_w2, f_b2, f_fq2, f_w3)
    z = g1 * fft_conv_centred(v, filt[:, 0], skip[0])
    z = g2 * fft_conv_centred(z, filt[:, 1], skip[1])
    return z @ w_out


def moe(h, router_w, router_b, w_gate, w_up, w_down):
    T = h.shape[0]
    scores = jax.nn.sigmoid((h @ router_w).astype(jnp.float32))
    sel = (scores + router_b).reshape(T, N_GROUPS, EXP_PER_GROUP)
    group_score = jnp.sum(lax.top_k(sel, TOP_K)[0], -1)
    g_best = jnp.argmax(group_score, -1)
    sel_in = jnp.take_along_axis(sel, g_best[:, None, None], axis=1)[:, 0]
    _, loc = lax.top_k(sel_in, TOP_K)
    idx = g_best[:, None] * EXP_PER_GROUP + loc
    w = jnp.take_along_axis(scores, idx, -1)
    w = w / jnp.sum(w, -1, keepdims=True)
    gates = jnp.sum(jax.nn.one_hot(idx, N_EXPERTS, dtype=jnp.float32) * w[..., None], axis=1).astype(h.dtype)
    out = jnp.zeros_like(h)
    for e in range(N_EXPERTS):
        ye = (jax.nn.silu(h @ w_gate[e]) * (h @ w_up[e])) @ w_down[e]
        out = out + gates[:, e:e + 1] * ye
    return out


def setup_inputs(seed: int = 0) -> dict:
    key = jax.random.key(seed)
    ks = iter(jax.random.split(key, 64))
    nrm = lambda shape, s: jax.random.normal(next(ks), shape, jnp.float32) * s
    D = D_MODEL
    NA, NB, NC = N_LAYERS_A, N_LAYERS_B, N_LAYERS_C
    inp = {}
    inp["x"] = nrm((BATCH, SEQ, D), 1.0)
    inp["c"] = nrm((BATCH, D), 1.0)
    inp["ctx"] = nrm((BATCH, CTX_LEN, D), 1.0)
    inp["c_ctx"] = nrm((D,), 1.0)
    inp["router_w"] = nrm((D, N_EXPERTS), D ** -0.5)
    inp["router_b"] = nrm((N_EXPERTS,), 0.01)
    inp["ada_w"] = nrm((DEPTH, D, 6 * D), 0.5 * D ** -0.5)
    inp["ada_b"] = nrm((DEPTH, 6 * D), 0.02)
    inp["ln_g"] = 1.0 + nrm((DEPTH, 2, D), 0.02)
    inp["ln_b"] = nrm((DEPTH, 2, D), 0.02)
    inp["moe_w_gate"] = nrm((DEPTH, N_EXPERTS, D, D_EXPERT), D ** -0.5)
    inp["moe_w_up"] = nrm((DEPTH, N_EXPERTS, D, D_EXPERT), D ** -0.5)
    inp["moe_w_down"] = nrm((DEPTH, N_EXPERTS, D_EXPERT, D), BETA * D_EXPERT ** -0.5)
    inp["rg_w_in"] = nrm((NA, D, 2 * D_RNN), D ** -0.5)
    inp["rg_conv_w"] = nrm((NA, RG_CONV, D_RNN), RG_CONV ** -0.5)
    inp["rg_conv_b"] = nrm((NA, D_RNN), 0.02)
    inp["rg_gate_a_w"] = nrm((NA, 2, RG_BLOCKS, RG_BS, RG_BS), RG_BS ** -0.5)
    inp["rg_gate_a_b"] = nrm((NA, 2, D_RNN), 0.02)
    inp["rg_gate_x_w"] = nrm((NA, 2, RG_BLOCKS, RG_BS, RG_BS), RG_BS ** -0.5)
    inp["rg_gate_x_b"] = nrm((NA, 2, D_RNN), 0.02)
    a8 = jax.random.uniform(next(ks), (NA, 2, D_RNN), jnp.float32, minval=0.9, maxval=0.999)
    a = a8 ** (1.0 / RG_C)
    inp["rg_lambda"] = jnp.log(a) - jnp.log1p(-a)
    inp["rg_w_out"] = nrm((NA, D_RNN, D), BETA * D_RNN ** -0.5)
    inp["ml_w_up"] = nrm((NB, D, D_M), D ** -0.5)
    inp["ml_conv_w"] = nrm((NB, M_CONV, D_M), M_CONV ** -0.5)
    inp["ml_conv_b"] = nrm((NB, D_M), 0.02)
    inp["ml_w_q"] = nrm((NB, D_M, M_HEADS * M_DK), D_M ** -0.5)
    inp["ml_w_k"] = nrm((NB, D_M, M_HEADS * M_DK), D_M ** -0.5)
    inp["ml_w_v"] = nrm((NB, D_M, M_HEADS * M_DV), D_M ** -0.5)
    inp["ml_w_o"] = nrm((NB, D_M, D_M), D_M ** -0.5)
    inp["ml_w_if"] = nrm((NB, 2, D_M, 2 * M_HEADS), D_M ** -0.5)
    inp["ml_b_if"] = jnp.concatenate([nrm((NB, 2, M_HEADS), 0.1),
                                      jnp.linspace(3.0, 6.0, M_HEADS, dtype=jnp.float32) + nrm((NB, 2, M_HEADS), 0.1)], -1)
    inp["ml_norm_g"] = 1.0 + nrm((NB, D_M), 0.02)
    inp["ml_skip"] = 1.0 + nrm((NB, D_M), 0.02)
    inp["ml_w_down"] = nrm((NB, D_M, D), BETA * D_M ** -0.5)
    inp["hy_w_in"] = nrm((NC, D, 3 * D), D ** -0.5)
    inp["hy_b_in"] = nrm((NC, 3 * D), 0.02)
    inp["hy_conv_w"] = nrm((NC, H_CONV, 3 * D), H_CONV ** -0.5)
    inp["hy_conv_b"] = nrm((NC, 3 * D), 0.02)
    inp["hy_f_w1"] = nrm((NC, H_EMB, H_FW), H_EMB ** -0.5)
    inp["hy_f_b1"] = nrm((NC, H_FW), 0.02)
    inp["hy_f_freq1"] = 1.0 + nrm((NC, H_FW), 0.02)
    inp["hy_f_w2"] = nrm((NC, H_FW, H_FW), H_FW ** -0.5)
    inp["hy_f_b2"] = nrm((NC, H_FW), 0.02)
    inp["hy_f_freq2"] = 1.0 + nrm((NC, H_FW), 0.02)
    inp["hy_f_w3"] = nrm((NC, H_FW, 2 * D), 0.1 * H_FW ** -0.5)
    inp["hy_skip"] = nrm((NC, 2, D), 0.1)
    inp["hy_w_out"] = nrm((NC, D, D), BETA * D ** -0.5)
    return inp


def reference(x, c, ctx, c_ctx, router_w, router_b, ada_w, ada_b, ln_g, ln_b, moe_w_gate, moe_w_up, moe_w_down,
              rg_w_in, rg_conv_w, rg_conv_b, rg_gate_a_w, rg_gate_a_b, rg_gate_x_w, rg_gate_x_b, rg_lambda, rg_w_out,
              ml_w_up, ml_conv_w, ml_conv_b, ml_w_q, ml_w_k, ml_w_v, ml_w_o, ml_w_if, ml_b_if, ml_norm_g, ml_skip,
              ml_w_down, hy_w_in, hy_b_in, hy_conv_w, hy_conv_b, hy_f_w1, hy_f_b1, hy_f_freq1, hy_f_w2, hy_f_b2,
              hy_f_freq2, hy_f_w3, hy_skip, hy_w_out):
    hx = x + sincos_2d(x.shape[1], x.dtype)[None]
    hc = ctx
    for i in range(DEPTH):
        last = i == DEPTH - 1
        need_ctx = not last
        kind, j = i % N_MIXERS, i // N_MIXERS
        mod_x = (jax.nn.silu(c) @ ada_w[i] + ada_b[i])[:, None, :]
        mod_c = jax.nn.silu(c_ctx) @ ada_w[i] + ada_b[i]
        shx, scx, gtx, shx2, scx2, gtx2 = jnp.split(mod_x, 6, axis=-1)
        shc, scc, gtc, shc2, scc2, gtc2 = jnp.split(mod_c, 6, axis=-1)
        in_x = hx * (1.0 + scx) + shx
        in_c = hc * (1.0 + scc) + shc if (need_ctx or kind != 2) else None
        if kind == 0:
            yc, yx = rglru_mixer(in_c, in_x, rg_w_in[j], rg_conv_w[j], rg_conv_b[j], rg_gate_a_w[j], rg_gate_a_b[j],
                                 rg_gate_x_w[j], rg_gate_x_b[j], rg_lambda[j], rg_w_out[j], need_ctx)
        elif kind == 1:
            yc, yx = mlstm_mixer(in_c, in_x, ml_w_up[j], ml_conv_w[j], ml_conv_b[j], ml_w_q[j], ml_w_k[j], ml_w_v[j],
                                 ml_w_o[j], ml_w_if[j], ml_b_if[j], ml_norm_g[j], ml_skip[j], ml_w_down[j], need_ctx)
        else:
            hp = (hy_w_in[j], hy_b_in[j], hy_conv_w[j], hy_conv_b[j], hy_f_w1[j], hy_f_b1[j], hy_f_freq1[j],
                  hy_f_w2[j], hy_f_b2[j], hy_f_freq2[j], hy_f_w3[j], hy_skip[j], hy_w_out[j])
            yx = hyena_seq(in_x, *hp)
            yc = hyena_seq(in_c, *hp) if need_ctx else None
        hx = layer_norm(ALPHA * hx + gtx * yx, ln_g[i, 0], ln_b[i, 0])
        fx = hx * (1.0 + scx2) + shx2
        if last:
            yx2 = moe(fx.reshape(-1, D_MODEL), router_w, router_b, moe_w_gate[i], moe_w_up[i],
                      moe_w_down[i]).reshape(fx.shape)
        else:
            hc = layer_norm(ALPHA * hc + gtc * yc, ln_g[i, 0], ln_b[i, 0])
            fc = hc * (1.0 + scc2) + shc2
            n_c = fc.shape[0] * fc.shape[1]
            y2 = moe(jnp.concatenate([fc.reshape(-1, D_MODEL), fx.reshape(-1, D_MODEL)], 0), router_w, router_b,
                     moe_w_gate[i], moe_w_up[i], moe_w_down[i])
            hc = layer_norm(ALPHA * hc + gtc2 * y2[:n_c].reshape(fc.shape), ln_g[i, 1], ln_b[i, 1])
            yx2 = y2[n_c:].reshape(fx.shape)
        hx = layer_norm(ALPHA * hx + gtx2 * yx2, ln_g[i, 1], ln_b[i, 1])
    return hx
```

```python
import math
from contextlib import ExitStack
import numpy as np
import concourse.bass as bass
import concourse.mybir as mybir
from concourse.bass_utils import run_bass_kernel_spmd

F32 = mybir.dt.float32
F32R = mybir.dt.float32r
AF = mybir.ActivationFunctionType
ALU = mybir.AluOpType
AX = mybir.AxisListType

D = 1024
KC = 8
SEQ = 2048
CTX = 256
T = SEQ + CTX
DEPTH = 4
ALPHA = (2.0 * DEPTH) ** 0.25
LN_EPS = 1e-6
D_RNN = 1408
RG_BS = 88
NE = 16
DEXP = 512
TT_ALL = [(0, 256), (256, 512), (768, 512), (1280, 512), (1792, 512)]
TT_X = [(256, 512), (768, 512), (1280, 512), (1792, 512)]


class Buf:
    __slots__ = ("w", "r")

    def __init__(self):
        self.w = {}
        self.r = {}


class KB:
    NRING = 12

    def __init__(self):
        nc = self.nc = bass.Bass("TRN2", target_bir_lowering=False)
        self.eng = {"pe": nc.tensor, "act": nc.scalar, "dve": nc.vector, "pool": nc.gpsimd, "sp": nc.sync}
        self.sems = {}
        self.esem = {}
        self.cnt = {}
        for e in ("pe", "act", "dve", "pool"):
            s = nc.alloc_semaphore("s_" + e)
            self.sems[s.num] = s
            self.esem[e] = s.num
            self.cnt[e] = 0
        self.dring = {}
        self.dcnt = {}
        self.dnext = {}
        for q in ("sp", "act", "pool"):
            self.dring[q] = []
            for i in range(self.NRING):
                s = nc.alloc_semaphore("d_%s%d" % (q, i))
                self.sems[s.num] = s
                self.dring[q].append(s.num)
                self.dcnt[s.num] = 0
            self.dnext[q] = 0
        self.seen = {e: {} for e in self.eng}
        self.uid = 0
        self.psum = [nc.alloc_psum_tensor("ps%d" % i, [128, 512], F32).ap() for i in range(8)]
        self.psb = [Buf() for _ in range(8)]
        self.pnext = 0

    def name(self, p):
        self.uid += 1
        return "%s_%d" % (p, self.uid)

    def _wait(self, e, need):
        seen = self.seen[e]
        own = self.esem.get(e)
        for s, c in need.items():
            if c <= 0:
                continue
            if e == "pe" and s == own:
                continue
            if seen.get(s, 0) >= c:
                continue
            self.eng[e].wait_ge(self.sems[s], c)
            seen[s] = c

    @staticmethod
    def _merge(dst, src):
        for s, c in src.items():
            if dst.get(s, 0) < c:
                dst[s] = c

    def _deps(self, reads, writes):
        need = {}
        for b in reads:
            self._merge(need, b.w)
        for b in writes:
            self._merge(need, b.w)
            self._merge(need, b.r)
        return need

    def op(self, e, fn, reads=(), writes=(), signal=True):
        self._wait(e, self._deps(reads, writes))
        inst = fn()
        s = self.esem[e]
        if signal:
            inst.then_inc(self.sems[s], 1)
            self.cnt[e] += 1
            ev = self.cnt[e]
        else:
            ev = self.cnt[e] + 1
        for b in writes:
            b.w = {s: ev}
            b.r = {}
        for b in reads:
            if b.r.get(s, 0) < ev:
                b.r[s] = ev
        return inst

    def dma(self, q, out, in_, reads=(), writes=(), **kw):
        i = self.dnext[q]
        self.dnext[q] = (i + 1) % self.NRING
        s = self.dring[q][i]
        need = self._deps(reads, writes)
        if need.get(s, 0) < self.dcnt[s]:
            need[s] = self.dcnt[s]
        self._wait(q, need)
        self.eng[q].dma_start(out=out, in_=in_, **kw).then_inc(self.sems[s], 16)
        self.dcnt[s] += 16
        ev = self.dcnt[s]
        for b in writes:
            b.w = {s: ev}
            b.r = {}
        for b in reads:
            if b.r.get(s, 0) < ev:
                b.r[s] = ev

    def barrier(self):
        need = {}
        for e, s in self.esem.items():
            need[s] = self.cnt[e]
        for s, c in self.dcnt.items():
            need[s] = c
        for e in self.eng:
            self._wait(e, need)

    def ps(self):
        i = self.pnext
        self.pnext = (i + 1) % 8
        return self.psum[i], self.psb[i]

    def tile(self, es, shape, dtype=F32, name="t"):
        t = es.enter_context(self.nc.sbuf_tensor(self.name(name), list(shape), dtype))
        return t.ap()


class Pool:
    def __init__(self, kb, es, n, shape, dtype=F32, name="p"):
        self.t = [kb.tile(es, shape, dtype, name) for _ in range(n)]
        self.b = [Buf() for _ in range(n)]
        self.i = 0
        self.n = n

    def get(self):
        i = self.i
        self.i = (i + 1) % self.n
        return self.t[i], self.b[i]


class Prog:
    def __init__(self, n_layers=DEPTH, dbg=()):
        self.kb = KB()
        self.nc = self.kb.nc
        self.n_layers = n_layers
        self.ins = {}
        self.dbg = {}
        self.dbg_want = set(dbg)

    def inp(self, name, shape):
        t = self.nc.dram_tensor(name, list(shape), F32, kind="ExternalInput").ap()
        self.ins[name] = t
        return t

    def scratch(self, name, shape):
        kind = "ExternalOutput" if name in self.dbg_want else "Internal"
        t = self.nc.dram_tensor(name, list(shape), F32, kind=kind).ap()
        if name in self.dbg_want:
            self.dbg[name] = t
        return t

    def prep(self, HT):
        kb, nc = self.kb, self.nc
        x, ctx, pos, ident = self.ins["x"], self.ins["ctx"], self.ins["pos"], self.ins["ident"]
        HTv = HT.rearrange("(c p) t -> p c t", p=128)
        with ExitStack() as es:
            idt = kb.tile(es, [128, 128], F32, "ident")
            idb = Buf()
            kb.dma("sp", idt, ident, writes=[idb])
            pin = Pool(kb, es, 3, [128, D], F32, "pin")
            ppos = Pool(kb, es, 3, [128, D], F32, "ppos")
            pout = Pool(kb, es, 3, [128, KC, 128], F32, "pout")
            for ti in range(T // 128):
                t0 = ti * 128
                a, ab = pin.get()
                if t0 < CTX:
                    kb.dma("sp", a, ctx[t0:t0 + 128, :], writes=[ab])
                else:
                    kb.dma("sp", a, x[t0 - CTX:t0 - CTX + 128, :], writes=[ab])
                    p_, pb = ppos.get()
                    kb.dma("sp", p_, pos[t0 - CTX:t0 - CTX + 128, :], writes=[pb])
                    kb.op("dve", lambda: nc.vector.tensor_tensor(out=a, in0=a, in1=p_, op=ALU.add), reads=[pb], writes=[ab])
                o, ob = pout.get()
                for half in range(2):
                    ps, psb = kb.ps()
                    for j in range(4):
                        c = half * 4 + j
                        kb.op("pe", lambda: nc.tensor.transpose(out=ps[:, j * 128:(j + 1) * 128], in_=a[:, c * 128:(c + 1) * 128], identity=idt),
                              reads=[ab, idb], writes=[psb], signal=(j == 3))
                    kb.op("act", lambda: nc.scalar.copy(out=o[:, half * 4:half * 4 + 4, :], in_=ps.rearrange("p (c t) -> p c t", c=4)),
                          reads=[psb], writes=[ob])
                kb.dma("act", HTv[:, :, t0:t0 + 128], o, reads=[ob])
        kb.barrier()

    def final(self, HT, out):
        kb, nc = self.kb, self.nc
        ident = self.ins["ident"]
        HTv = HT.rearrange("(c p) t -> p c t", p=128)
        with ExitStack() as es:
            idt = kb.tile(es, [128, 128], F32, "ident")
            idb = Buf()
            kb.dma("sp", idt, ident, writes=[idb])
            pin = Pool(kb, es, 3, [128, KC, 128], F32, "fin")
            pout = Pool(kb, es, 3, [128, D], F32, "fout")
            for ti in range(SEQ // 128):
                t0 = CTX + ti * 128
                a, ab = pin.get()
                kb.dma("sp", a, HTv[:, :, t0:t0 + 128], writes=[ab])
                o, ob = pout.get()
                for half in range(2):
                    ps, psb = kb.ps()
                    for j in range(4):
                        c = half * 4 + j
                        kb.op("pe", lambda: nc.tensor.transpose(out=ps[:, j * 128:(j + 1) * 128], in_=a[:, c, :], identity=idt),
                              reads=[ab, idb], writes=[psb], signal=(j == 3))
                    kb.op("act", lambda: nc.scalar.copy(out=o[:, half * 512:(half + 1) * 512], in_=ps), reads=[psb], writes=[ob])
                kb.dma("act", out[ti * 128:(ti + 1) * 128, :], o, reads=[ob])
        kb.barrier()

    def persist(self):
        kb, nc = self.kb, self.nc
        es = self.es_global
        self.ident = kb.tile(es, [128, 128], F32, "identp")
        self.identb = Buf()
        kb.dma("sp", self.ident, self.ins["ident"], writes=[self.identb])
        self.ones = kb.tile(es, [128, 128], F32, "ones")
        self.onesb = Buf()
        kb.op("dve", lambda: nc.vector.memset(self.ones, 1.0), writes=[self.onesb])
        self.lng = kb.tile(es, [128, DEPTH * 2 * KC], F32, "lng")
        self.lnb = kb.tile(es, [128, DEPTH * 2 * KC], F32, "lnb")
        self.lnbuf = Buf()
        kb.dma("sp", self.lng, self.ins["lng"], writes=[self.lnbuf])
        kb.dma("sp", self.lnb, self.ins["lnb"], writes=[self.lnbuf])
        self.routw = kb.tile(es, [128, KC, NE], F32, "routw")
        self.routb = kb.tile(es, [128, NE], F32, "routb")
        self.sel = kb.tile(es, [NE, NE, 128], F32, "selE")
        self.routbuf = Buf()
        kb.dma("sp", self.routw, self.ins["routw"], writes=[self.routbuf])
        kb.dma("sp", self.routb, self.ins["routb"], writes=[self.routbuf])
        kb.dma("sp", self.sel, self.ins["selE"], writes=[self.routbuf])
        self.modT = kb.tile(es, [128, 48, 2], F32, "modT")
        self.modb = Buf()
        kb.barrier()

    def mod_stage(self, layer):
        kb, nc = self.kb, self.nc
        ada_w = self.ins["ada_w"]
        modT = self.modT
        with ExitStack() as es:
            cc = kb.tile(es, [128, KC, 2], F32, "cc")
            ccb = Buf()
            kb.dma("sp", cc, self.ins["cc"], writes=[ccb])
            sc = kb.tile(es, [128, KC, 2], F32, "sc")
            scb = Buf()
            kb.op("act", lambda: nc.scalar.activation(out=sc, in_=cc, func=AF.Silu), reads=[ccb], writes=[scb])
            ab = kb.tile(es, [128, 48], F32, "adab")
            abb = Buf()
            kb.dma("sp", ab, self.ins["adab"][layer], writes=[abb])
            wp = Pool(kb, es, 2, [128, KC, 1024], F32, "adaw")
            ps, psb = kb.ps()
            for s in range(6):
                w, wb = wp.get()
                kb.dma("sp", w, ada_w[layer, :, s * 1024:(s + 1) * 1024].rearrange("(c p) f -> p c f", p=128), writes=[wb])
                for mm in range(8):
                    m = s * 8 + mm
                    for kc in range(KC):
                        kb.op("pe", lambda: nc.tensor.matmul(ps[:, 2 * m:2 * m + 2], w[:, kc, mm * 128:(mm + 1) * 128], sc[:, kc, :],
                                                             start=(kc == 0), stop=(kc == KC - 1)),
                              reads=[wb, scb], writes=[psb], signal=(kc == KC - 1 and mm == 7))
            psv = ps[:, 0:96].rearrange("p (m j) -> p m j", j=2)
            for j in range(2):
                kb.op("dve", lambda: nc.vector.tensor_tensor(out=modT[:, :, j], in0=psv[:, :, j], in1=ab, op=ALU.add),
                      reads=[psb, abb], writes=[self.modb])
            for s in (1, 4):
                kb.op("dve", lambda: nc.vector.tensor_scalar(out=modT[:, s * 8:(s + 1) * 8, :], in0=modT[:, s * 8:(s + 1) * 8, :],
                                                             scalar1=1.0, scalar2=None, op0=ALU.add), writes=[self.modb])
            for s in (2, 5):
                kb.op("dve", lambda: nc.vector.tensor_scalar(out=modT[:, s * 8:(s + 1) * 8, :], in0=modT[:, s * 8:(s + 1) * 8, :],
                                                             scalar1=1.0 / ALPHA, scalar2=None, op0=ALU.mult), writes=[self.modb])
        kb.barrier()

    def ln_stage(self, HT, YT, layer, j, tiles):
        kb, nc = self.kb, self.nc
        g_slot = 2 if j == 0 else 5
        modT = self.modT
        HTv = HT.rearrange("(c p) t -> p c t", p=128)
        YTv = YT.rearrange("(c p) t -> p c t", p=128)
        lcol = (layer * 2 + j) * KC
        eps = LN_EPS / (ALPHA * ALPHA)
        with ExitStack() as es:
            ph = Pool(kb, es, 2, [128, KC, 512], F32, "lnh")
            py = Pool(kb, es, 2, [128, KC, 512], F32, "lny")
            pq = Pool(kb, es, 1, [128, KC, 512], F32, "lnq")
            po = Pool(kb, es, 2, [128, KC, 512], F32, "lno")
            pm = Pool(kb, es, 2, [128, 512], F32, "lnm")
            pv = Pool(kb, es, 2, [128, 512], F32, "lnv")
            epst = kb.tile(es, [128, 1], F32, "eps")
            epsb = Buf()
            kb.op("dve", lambda: nc.vector.memset(epst, eps), writes=[epsb])
            for (t0, n) in tiles:
                col = 1 if t0 < CTX else 0
                h, hb = ph.get()
                y, yb = py.get()
                kb.dma("sp", h[:, :, :n], HTv[:, :, t0:t0 + n], writes=[hb])
                kb.dma("sp", y[:, :, :n], YTv[:, :, t0:t0 + n], writes=[yb])
                for c in range(KC):
                    m = g_slot * 8 + c
                    kb.op("dve", lambda: nc.vector.scalar_tensor_tensor(out=h[:, c, :n], in0=y[:, c, :n], scalar=modT[:, m, col:col + 1],
                                                                        in1=h[:, c, :n], op0=ALU.mult, op1=ALU.add),
                          reads=[yb, self.modb], writes=[hb])
                q, qb = pq.get()
                kb.op("act", lambda: nc.scalar.activation(out=q[:, :, :n], in_=h[:, :, :n], func=AF.Square), reads=[hb], writes=[qb])
                ps1, ps1b = kb.ps()
                ps2, ps2b = kb.ps()
                for c in range(KC):
                    kb.op("pe", lambda: nc.tensor.matmul(ps1[:, :n], self.ones, h[:, c, :n], start=(c == 0), stop=(c == KC - 1)),
                          reads=[hb, self.onesb], writes=[ps1b], signal=(c == KC - 1))
                for c in range(KC):
                    kb.op("pe", lambda: nc.tensor.matmul(ps2[:, :n], self.ones, q[:, c, :n], start=(c == 0), stop=(c == KC - 1)),
                          reads=[qb, self.onesb], writes=[ps2b], signal=(c == KC - 1))
                mean, mb = pm.get()
                var, vb = pv.get()
                kb.op("act", lambda: nc.scalar.mul(out=mean[:, :n], in_=ps1[:, :n], mul=1.0 / D), reads=[ps1b], writes=[mb])
                kb.op("dve", lambda: nc.vector.tensor_tensor(out=var[:, :n], in0=mean[:, :n], in1=mean[:, :n], op=ALU.mult), reads=[mb], writes=[vb])
                kb.op("dve", lambda: nc.vector.scalar_tensor_tensor(out=var[:, :n], in0=ps2[:, :n], scalar=1.0 / D, in1=var[:, :n],
                                                                    op0=ALU.mult, op1=ALU.subtract), reads=[ps2b], writes=[vb])
                kb.op("act", lambda: nc.scalar.activation(out=var[:, :n], in_=var[:, :n], func=AF.Ln, bias=epst[:, 0:1]), reads=[epsb], writes=[vb])
                kb.op("act", lambda: nc.scalar.activation(out=var[:, :n], in_=var[:, :n], func=AF.Exp, scale=-0.5), writes=[vb])
                o, ob = po.get()
                for c in range(KC):
                    kb.op("dve", lambda: nc.vector.tensor_tensor(out=h[:, c, :n], in0=h[:, c, :n], in1=mean[:, :n], op=ALU.subtract),
                          reads=[mb], writes=[hb])
                    kb.op("pool", lambda: nc.gpsimd.tensor_tensor(out=h[:, c, :n], in0=h[:, c, :n], in1=var[:, :n], op=ALU.mult),
                          reads=[vb], writes=[hb])
                    kb.op("act", lambda: nc.scalar.activation(out=o[:, c, :n], in_=h[:, c, :n], func=AF.Identity,
                                                              scale=self.lng[:, lcol + c:lcol + c + 1], bias=self.lnb[:, lcol + c:lcol + c + 1]),
                          reads=[hb, self.lnbuf], writes=[ob])
                kb.dma("act", HTv[:, :, t0:t0 + n], o[:, :, :n], reads=[ob])
        kb.barrier()

    def moe_stage(self, HT, YT, layer, with_ctx):
        kb, nc = self.kb, self.nc
        modT = self.modT
        HTv = HT.rearrange("(c p) t -> p c t", p=128)
        YTv = YT.rearrange("(c p) t -> p c t", p=128)
        wg_d, wu_d, wd_d = self.ins["moe_w_gate"], self.ins["moe_w_up"], self.ins["moe_w_down"]
        if with_ctx:
            groups = [[(0, 256), (256, 512)], [(768, 512), (1280, 256)], [(1536, 512), (2048, 256)]]
        else:
            groups = [[(256, 512), (768, 256)], [(1024, 512), (1536, 256)], [(1792, 512)]]
        GMAX = 768
        with ExitStack() as es:
            B = kb.tile(es, [128, KC, GMAX], F32R, "moeB")
            Bb = Buf()
            acc = kb.tile(es, [128, KC, GMAX], F32, "moeacc")
            accb = [Buf() for _ in range(2)]
            ar = kb.tile(es, [128, 2, GMAX], F32R, "moea")
            arb = [Buf() for _ in range(2)]
            gT = kb.tile(es, [NE, GMAX], F32, "gatesT")
            gTb = Buf()
            pfx = Pool(kb, es, 2, [128, KC, 128], F32, "fx32")
            pgbc = Pool(kb, es, 2, [128, GMAX], F32, "gbc")
            psil = Pool(kb, es, 2, [128, 512], F32, "sil")
            ptmp = Pool(kb, es, 2, [128, 512], F32, "tmp")
            pwg = Pool(kb, es, 2, [128, KC, 256], F32R, "wg")
            pwu = Pool(kb, es, 2, [128, KC, 256], F32R, "wu")
            pwd = Pool(kb, es, 2, [128, 2, D], F32R, "wd")
            prt = Pool(kb, es, 2, [128, 160], F32, "rt")
            for grp in groups:
                g0 = grp[0][0]
                G = sum(n for _, n in grp)
                tiles = []
                l0 = 0
                for (t0, n) in grp:
                    tiles.append((t0, n, l0))
                    l0 += n
                for si in range(G // 128):
                    t0 = g0 + si * 128
                    col = 1 if t0 < CTX else 0
                    fx, fxb = pfx.get()
                    kb.dma("sp", fx, HTv[:, :, t0:t0 + 128], writes=[fxb])
                    for c in range(KC):
                        kb.op("act", lambda: nc.scalar.activation(out=fx[:, c, :], in_=fx[:, c, :], func=AF.Identity,
                                                                  scale=modT[:, 4 * 8 + c, col:col + 1], bias=modT[:, 3 * 8 + c, col:col + 1]),
                              reads=[self.modb], writes=[fxb])
                    kb.op("dve", lambda: nc.vector.tensor_copy(out=B[:, :, si * 128:(si + 1) * 128], in_=fx), reads=[fxb], writes=[Bb])
                    ps, psb = kb.ps()
                    for c in range(KC):
                        kb.op("pe", lambda: nc.tensor.matmul(ps[:, 0:NE], fx[:, c, :], self.routw[:, c, :], start=(c == 0), stop=(c == KC - 1)),
                              reads=[fxb, self.routbuf], writes=[psb], signal=(c == KC - 1))
                    r, rb = prt.get()
                    sc = r[:, 0:16]
                    sel = r[:, 16:32]
                    eq = r[:, 32:48]
                    s2 = r[:, 48:64]
                    m1 = r[:, 64:68]
                    m2 = r[:, 68:72]
                    gs = r[:, 72:76]
                    og = r[:, 76:80]
                    t4 = r[:, 80:84]
                    gmax = r[:, 84:85]
                    m2b = r[:, 85:86]
                    wsum = r[:, 86:87]
                    selm = r[:, 96:112]
                    ch = r[:, 112:128]
                    w = r[:, 128:144]
                    gts = r[:, 144:160]
                    v3 = lambda a: a.rearrange("p (g k) -> p g k", k=4)
                    bc = lambda a: a.unsqueeze(2).to_broadcast([128, 4, 4])
                    V = nc.vector
                    kb.op("act", lambda: nc.scalar.activation(out=sc, in_=ps[:, 0:NE], func=AF.Sigmoid), reads=[psb], writes=[rb])
                    kb.op("dve", lambda: V.tensor_tensor(out=sel, in0=sc, in1=self.routb, op=ALU.add), reads=[self.routbuf], writes=[rb])
                    kb.op("dve", lambda: V.tensor_reduce(out=m1, in_=v3(sel), axis=AX.X, op=ALU.max), writes=[rb])
                    kb.op("dve", lambda: V.tensor_tensor(out=v3(eq), in0=v3(sel), in1=bc(m1), op=ALU.is_equal), writes=[rb])
                    kb.op("dve", lambda: V.scalar_tensor_tensor(out=s2, in0=eq, scalar=-1e30, in1=sel, op0=ALU.mult, op1=ALU.add), writes=[rb])
                    kb.op("dve", lambda: V.tensor_reduce(out=m2, in_=v3(s2), axis=AX.X, op=ALU.max), writes=[rb])
                    kb.op("dve", lambda: V.tensor_tensor(out=gs, in0=m1, in1=m2, op=ALU.add), writes=[rb])
                    kb.op("dve", lambda: V.tensor_reduce(out=gmax, in_=gs, axis=AX.X, op=ALU.max), writes=[rb])
                    kb.op("dve", lambda: V.tensor_scalar(out=og, in0=gs, scalar1=gmax, scalar2=None, op0=ALU.is_equal), writes=[rb])
                    kb.op("dve", lambda: V.tensor_tensor(out=t4, in0=og, in1=m2, op=ALU.mult), writes=[rb])
                    kb.op("dve", lambda: V.tensor_reduce(out=m2b, in_=t4, axis=AX.X, op=ALU.add), writes=[rb])
                    kb.op("dve", lambda: V.tensor_scalar(out=t4, in0=og, scalar1=-1.0, scalar2=1e30, op0=ALU.add, op1=ALU.mult), writes=[rb])
                    kb.op("dve", lambda: V.tensor_tensor(out=v3(selm), in0=v3(sel), in1=bc(t4), op=ALU.add), writes=[rb])
                    kb.op("dve", lambda: V.tensor_scalar(out=ch, in0=selm, scalar1=m2b, scalar2=None, op0=ALU.is_ge), writes=[rb])
                    kb.op("dve", lambda: V.tensor_tensor(out=w, in0=sc, in1=ch, op=ALU.mult), writes=[rb])
                    kb.op("dve", lambda: V.tensor_reduce(out=wsum, in_=w, axis=AX.X, op=ALU.add), writes=[rb])
                    kb.op("dve", lambda: V.reciprocal(out=wsum, in_=wsum), writes=[rb])
                    kb.op("dve", lambda: V.tensor_scalar(out=gts, in0=w, scalar1=wsum, scalar2=None, op0=ALU.mult), writes=[rb])
                    pst, pstb = kb.ps()
                    kb.op("pe", lambda: nc.tensor.transpose(out=pst[0:NE, 0:128], in_=gts, identity=self.ident), reads=[rb, self.identb], writes=[pstb])
                    kb.op("act", lambda: nc.scalar.copy(out=gT[:, si * 128:(si + 1) * 128], in_=pst[0:NE, 0:128]), reads=[pstb], writes=[gTb])
                first = True
                for e in range(NE):
                    gbc, gbcb = pgbc.get()
                    for (t0, n, l0) in tiles:
                        psg, psgb = kb.ps()
                        kb.op("pe", lambda: nc.tensor.matmul(psg[:, :n], self.sel[:, e, :], gT[:, l0:l0 + n], start=True, stop=True),
                              reads=[gTb, self.routbuf], writes=[psgb])
                        kb.op("act", lambda: nc.scalar.copy(out=gbc[:, l0:l0 + n], in_=psg[:, :n]), reads=[psgb], writes=[gbcb])
                    for jh in range(2):
                        wg, wgb = pwg.get()
                        wu, wub = pwu.get()
                        wd, wdb = pwd.get()
                        kb.dma("pool", wg, wg_d[layer, e, :, jh * 256:(jh + 1) * 256].rearrange("(c p) f -> p c f", p=128), writes=[wgb])
                        kb.dma("pool", wu, wu_d[layer, e, :, jh * 256:(jh + 1) * 256].rearrange("(c p) f -> p c f", p=128), writes=[wub])
                        kb.dma("pool", wd, wd_d[layer, e, jh * 256:(jh + 1) * 256, :].rearrange("(j p) d -> p j d", p=128), writes=[wdb])
                        for (t0, n, l0) in tiles:
                            for jj in range(2):
                                pg, pgb = kb.ps()
                                pu, pub = kb.ps()
                                for c in range(KC):
                                    kb.op("pe", lambda: nc.tensor.matmul(pg[:, :n], wg[:, c, jj * 128:(jj + 1) * 128], B[:, c, l0:l0 + n],
                                                                         start=(c == 0), stop=(c == KC - 1)),
                                          reads=[wgb, Bb], writes=[pgb], signal=(c == KC - 1))
                                for c in range(KC):
                                    kb.op("pe", lambda: nc.tensor.matmul(pu[:, :n], wu[:, c, jj * 128:(jj + 1) * 128], B[:, c, l0:l0 + n],
                                                                         start=(c == 0), stop=(c == KC - 1)),
                                          reads=[wub, Bb], writes=[pub], signal=(c == KC - 1))
                                sl, slb = psil.get()
                                tm, tmb = ptmp.get()
                                kb.op("act", lambda: nc.scalar.activation(out=sl[:, :n], in_=pg[:, :n], func=AF.Silu), reads=[pgb], writes=[slb])
                                kb.op("dve", lambda: nc.vector.tensor_tensor(out=tm[:, :n], in0=sl[:, :n], in1=pu[:, :n], op=ALU.mult),
                                      reads=[slb, pub], writes=[tmb])
                                kb.op("pool", lambda: nc.gpsimd.tensor_tensor(out=ar[:, jj, l0:l0 + n], in0=tm[:, :n], in1=gbc[:, l0:l0 + n], op=ALU.mult),
                                      reads=[tmb, gbcb], writes=[arb[jj]])
                        for (t0, n, l0) in tiles:
                            for dc in range(KC):
                                po, pob = kb.ps()
                                for jj in range(2):
                                    kb.op("pe", lambda: nc.tensor.matmul(po[:, :n], wd[:, jj, dc * 128:(dc + 1) * 128], ar[:, jj, l0:l0 + n],
                                                                         start=(jj == 0), stop=(jj == 1)),
                                          reads=[wdb, arb[jj]], writes=[pob], signal=(jj == 1))
                                ab_ = accb[dc % 2]
                                if first:
                                    kb.op("dve", lambda: nc.vector.tensor_copy(out=acc[:, dc, l0:l0 + n], in_=po[:, :n]), reads=[pob], writes=[ab_])
                                else:
                                    kb.op("dve", lambda: nc.vector.tensor_tensor(out=acc[:, dc, l0:l0 + n], in0=acc[:, dc, l0:l0 + n], in1=po[:, :n], op=ALU.add),
                                          reads=[pob], writes=[ab_])
                        first = False
                kb.dma("act", YTv[:, :, g0:g0 + G], acc[:, :, :G], reads=accb)
        kb.barrier()

    def linear_stage(self, XT, W, YT, kp, tiles, mb=512, evac=None, mod=None, extra=None):
        kb, nc = self.kb, self.nc
        K_, M = W.shape
        nk = K_ // kp
        XTv = XT.rearrange("(c p) t -> p c t", p=kp)
        Wv = W.rearrange("(c p) m -> p c m", p=kp)
        YTv = YT.rearrange("(c p) t -> p c t", p=128)
        mb = min(mb, M)
        with ExitStack() as es:
            if mod is not None:
                U, Ub = self.load_mod(es, XT, mod[0], mod[1], tiles)
            pw = Pool(kb, es, 1 if M <= mb else 2, [kp, nk, mb], F32R, "linw")
            if mod is None:
                px = Pool(kb, es, 2, [kp, nk, 512], F32R, "linx")
            po = Pool(kb, es, 2, [128, mb // 128, 512], F32, "lino")
            for m0 in range(0, M, mb):
                w, wb = pw.get()
                kb.dma("pool", w, Wv[:, :, m0:m0 + mb], writes=[wb])
                for (t0, n) in tiles:
                    col = 1 if t0 < CTX else 0
                    if mod is None:
                        x, xb = px.get()
                        kb.dma("pool", x[:, :, :n], XTv[:, :, t0:t0 + n], writes=[xb])
                    else:
                        x, xb = U[:, :, t0:t0 + n], Ub[t0]
                    if extra is not None and m0 == 0:
                        extra(x, xb, t0, n)
                    o, ob = po.get()
                    for mc in range(mb // 128):
                        ps, psb = kb.ps()
                        for c in range(nk):
                            kb.op("pe", lambda: nc.tensor.matmul(ps[:, :n], w[:, c, mc * 128:(mc + 1) * 128], x[:, c, :n], start=(c == 0), stop=(c == nk - 1)),
                                  reads=[wb, xb], writes=[psb], signal=(c == nk - 1))
                        if evac is None:
                            kb.op("act", lambda: nc.scalar.copy(out=o[:, mc, :n], in_=ps[:, :n]), reads=[psb], writes=[ob])
                        else:
                            evac(ps[:, :n], o[:, mc, :n], m0 // 128 + mc, col, [psb], [ob])
                    kb.dma("sp", YTv[:, m0 // 128:(m0 + mb) // 128, t0:t0 + n], o[:, :, :n], reads=[ob])
        kb.barrier()

    def load_mod(self, es, HT, sh_slot, sc_slot, tiles):
        kb, nc = self.kb, self.nc
        HTv = HT.rearrange("(c p) t -> p c t", p=128)
        U = kb.tile(es, [128, KC, T], F32R, "U")
        Ub = {}
        with ExitStack() as es2:
            ph = Pool(kb, es2, 2, [128, KC, 512], F32, "uh")
            for (t0, n) in tiles:
                col = 1 if t0 < CTX else 0
                h, hb = ph.get()
                kb.dma("sp", h[:, :, :n], HTv[:, :, t0:t0 + n], writes=[hb])
                Ub[t0] = Buf()
                for c in range(KC):
                    kb.op("act", lambda: nc.scalar.activation(out=U[:, c, t0:t0 + n], in_=h[:, c, :n], func=AF.Identity,
                                                              scale=self.modT[:, sc_slot * 8 + c, col:col + 1], bias=self.modT[:, sh_slot * 8 + c, col:col + 1]),
                          reads=[hb, self.modb], writes=[Ub[t0]])
            kb.barrier()
        return U, Ub

    def rglru_stage(self, HT, MT, jl):
        kb, nc = self.kb, self.nc
        w_in = self.ins["rg_w_in"]
        NB = 16
        P = RG_BS
        with ExitStack() as es:
            U, Ub = self.load_mod(es, HT, 0, 1, TT_ALL)
            Uall = list(Ub.values())
            rgv = kb.tile(es, [P, NB, 11], F32, "rgv")
            rgvb = Buf()
            kb.dma("sp", rgv, self.ins["rgv"][jl], writes=[rgvb])
            pgw = Pool(kb, es, 2, [P, 2, 2, P], F32R, "rggw")
            coef = kb.tile(es, [P, 2, NB], F32, "coef")
            coef2 = kb.tile(es, [P, 2, NB], F32, "coef2")
            cb = Buf()
            for z in range(2):
                kb.op("act", lambda: nc.scalar.activation(out=coef[:, z, :], in_=rgv[:, :, 9 + z], func=AF.Exp, scale=-1.0), reads=[rgvb], writes=[cb])
            kb.op("act", lambda: nc.scalar.activation(out=coef, in_=coef, func=AF.Ln, bias=self.ones[:P, 0:1]), reads=[self.onesb], writes=[cb])
            kb.op("dve", lambda: nc.vector.tensor_scalar(out=coef2, in0=coef, scalar1=-16.0, scalar2=None, op0=ALU.mult), writes=[cb])
            kb.op("dve", lambda: nc.vector.tensor_scalar(out=coef, in0=coef, scalar1=-8.0, scalar2=None, op0=ALU.mult), writes=[cb])
            pwi = Pool(kb, es, 2, [128, KC, 2, P], F32R, "rgwin")
            prec = Pool(kb, es, 1, [P, T], F32, "rec")
            pgate = Pool(kb, es, 2, [P, T], F32, "gate")
            mk = lambda nm: (kb.tile(es, [P, T], F32, nm), Buf())
            xc, xcb = mk("xc")
            xcr = kb.tile(es, [P, T], F32R, "xcr")
            xcrb = Buf()
            gts1 = [mk("r"), mk("i")]
            gts = [gts1, gts1]
            a_, ab_ = mk("a")
            w_, wb_ = mk("w")
            hs, hsb = mk("hs")
            w_in_v = w_in[jl].rearrange("(c p) (g n f) -> p c g n f", p=128, g=2, n=NB)
            segs = [(0, CTX), (CTX, T)]
            for n in range(NB):
                wi, wib = pwi.get()
                for g in range(2):
                    kb.dma("pool", wi[:, :, g, :], w_in_v[:, :, g, n, :], writes=[wib])
                gw4, gwb = pgw.get()
                kb.dma("pool", gw4[:, 0], self.ins["rg_gate_a_w"][jl, :, n].rearrange("z k j -> k z j"), writes=[gwb])
                kb.dma("pool", gw4[:, 1], self.ins["rg_gate_x_w"][jl, :, n].rearrange("z k j -> k z j"), writes=[gwb])
                rec, recb = prec.get()
                gate, gateb = pgate.get()
                for (t0, nn) in TT_ALL:
                    ps, psb = kb.ps()
                    for c in range(KC):
                        kb.op("pe", lambda: nc.tensor.matmul(ps[:P, :nn], wi[:, c, 1, :], U[:, c, t0:t0 + nn], start=(c == 0), stop=(c == KC - 1)),
                              reads=[wib, Ub[t0]], writes=[psb], signal=(c == KC - 1))
                    kb.op("act", lambda: nc.scalar.copy(out=rec[:, t0:t0 + nn], in_=ps[:P, :nn]), reads=[psb], writes=[recb])
                for (t0, nn) in TT_ALL:
                    ps, psb = kb.ps()
                    for c in range(KC):
                        kb.op("pe", lambda: nc.tensor.matmul(ps[:P, :nn], wi[:, c, 0, :], U[:, c, t0:t0 + nn], start=(c == 0), stop=(c == KC - 1)),
                              reads=[wib, Ub[t0]], writes=[psb], signal=(c == KC - 1))
                    kb.op("act", lambda: nc.scalar.activation(out=gate[:, t0:t0 + nn], in_=ps[:P, :nn], func=AF.Gelu_apprx_tanh), reads=[psb], writes=[gateb])
                kb.op("dve", lambda: nc.vector.tensor_scalar(out=xc, in0=rec, scalar1=rgv[:, n, 2:3], scalar2=rgv[:, n, 4:5], op0=ALU.mult, op1=ALU.add),
                      reads=[recb, rgvb], writes=[xcb])
                for k in (0, 1, 3):
                    d = k - 2
                    for (s0, s1) in segs:
                        ta, tb = max(s0, s0 - d), min(s1, s1 - d)
                        kb.op("dve", lambda: nc.vector.scalar_tensor_tensor(out=xc[:, ta:tb], in0=rec[:, ta + d:tb + d], scalar=rgv[:, n, k:k + 1],
                                                                            in1=xc[:, ta:tb], op0=ALU.mult, op1=ALU.add),
                              reads=[recb, rgvb], writes=[xcb])
                kb.op("act", lambda: nc.scalar.copy(out=xcr, in_=xc), reads=[xcb], writes=[xcrb])
                for z in range(2):
                    for gi, bcol in enumerate((5 + z, 7 + z)):
                        gt, gtb = gts[z][gi]
                        for (t0, nn) in TT_ALL:
                            ps, psb = kb.ps()
                            kb.op("pe", lambda: nc.tensor.matmul(ps[:P, :nn], gw4[:, gi, z, :], xcr[:, t0:t0 + nn], start=True, stop=True),
                                  reads=[gwb, xcrb], writes=[psb])
                            kb.op("act", lambda: nc.scalar.activation(out=gt[:, t0:t0 + nn], in_=ps[:P, :nn], func=AF.Sigmoid, bias=rgv[:, n, bcol:bcol + 1]),
                                  reads=[psb, rgvb], writes=[gtb])
                    (r, rb), (ig, igb) = gts[z]
                    kb.op("act", lambda: nc.scalar.activation(out=a_, in_=r, func=AF.Exp, scale=coef[:, z, n:n + 1]), reads=[rb, cb], writes=[ab_])
                    kb.op("act", lambda: nc.scalar.activation(out=w_, in_=r, func=AF.Exp, scale=coef2[:, z, n:n + 1]), reads=[rb, cb], writes=[wb_])
                    kb.op("act", lambda: nc.scalar.activation(out=w_, in_=w_, func=AF.Ln, scale=-1.0, bias=self.ones[:P, 0:1]), reads=[self.onesb], writes=[wb_])
                    kb.op("act", lambda: nc.scalar.activation(out=w_, in_=w_, func=AF.Exp, scale=0.5), writes=[wb_])
                    kb.op("dve", lambda: nc.vector.tensor_tensor(out=ig, in0=ig, in1=xc, op=ALU.mult), reads=[xcb], writes=[igb])
                    kb.op("pool", lambda: nc.gpsimd.tensor_tensor(out=ig, in0=ig, in1=w_, op=ALU.mult), reads=[wb_], writes=[igb])
                    if z == 0:
                        kb.op("dve", lambda: nc.vector.tensor_tensor_scan(out=hs, data0=a_, data1=ig, initial=0.0, op0=ALU.mult, op1=ALU.add),
                              reads=[ab_, igb], writes=[hsb])
                    else:
                        kb.op("dve", lambda: nc.vector.tensor_tensor_scan(out=r[:, 0:CTX][:, ::-1], data0=a_[:, 0:CTX][:, ::-1], data1=ig[:, 0:CTX][:, ::-1],
                                                                          initial=0.0, op0=ALU.mult, op1=ALU.add),
                              reads=[ab_, igb], writes=[rb])
                        kb.op("dve", lambda: nc.vector.tensor_tensor_scan(out=r[:, CTX:T][:, ::-1], data0=a_[:, CTX:T][:, ::-1], data1=ig[:, CTX:T][:, ::-1],
                                                                          initial=r[:, 0:1], op0=ALU.mult, op1=ALU.add),
                              reads=[ab_, igb], writes=[rb])
                        kb.op("pool", lambda: nc.gpsimd.tensor_tensor(out=hs, in0=hs, in1=r, op=ALU.add), reads=[rb], writes=[hsb])
                kb.op("dve", lambda: nc.vector.tensor_tensor(out=hs, in0=hs, in1=gate, op=ALU.mult), reads=[gateb], writes=[hsb])
                kb.dma("sp", MT[n * P:(n + 1) * P, :], hs, reads=[hsb])
        kb.barrier()

    def linear_tok_stage(self, XT, W, Y, evac=None):
        kb, nc = self.kb, self.nc
        K_, M = W.shape
        nk = K_ // 128
        XTv = XT.rearrange("(c p) t -> p c t", p=128)
        Wv = W.rearrange("(c p) m -> p c m", p=128)
        mb = 512
        with ExitStack() as es:
            pw = Pool(kb, es, 2, [128, nk, mb], F32R, "ltw")
            px = Pool(kb, es, 3, [128, nk, 128], F32R, "ltx")
            po = Pool(kb, es, 3, [128, mb], F32, "lto")
            for m0 in range(0, M, mb):
                w, wb = pw.get()
                kb.dma("pool", w, Wv[:, :, m0:m0 + mb], writes=[wb])
                for ti in range(T // 128):
                    t0 = ti * 128
                    x, xb = px.get()
                    kb.dma("pool", x, XTv[:, :, t0:t0 + 128], writes=[xb])
                    ps, psb = kb.ps()
                    for c in range(nk):
                        kb.op("pe", lambda: nc.tensor.matmul(ps, x[:, c, :], w[:, c, :], start=(c == 0), stop=(c == nk - 1)),
                              reads=[wb, xb], writes=[psb], signal=(c == nk - 1))
                    o, ob = po.get()
                    if evac is None:
                        kb.op("act", lambda: nc.scalar.copy(out=o, in_=ps), reads=[psb], writes=[ob])
                    else:
                        evac(ps, o, [psb], [ob])
                    kb.dma("sp", Y[t0:t0 + 128, m0:m0 + mb], o, reads=[ob])
        kb.barrier()

    def dwconv(self, out, outb, x, xb, vec, vecb, ntap, left, bias_col, P=128, eng="dve"):
        kb, nc = self.kb, self.nc
        kb.op("dve", lambda: nc.vector.tensor_scalar(out=out, in0=x, scalar1=vec[:, left:left + 1], scalar2=vec[:, bias_col:bias_col + 1],
                                                     op0=ALU.mult, op1=ALU.add), reads=[xb, vecb], writes=[outb])
        for k in range(ntap):
            d = k - left
            if d == 0:
                continue
            for (s0, s1) in ((0, CTX), (CTX, T)):
                ta, tb = max(s0, s0 - d), min(s1, s1 - d)
                kb.op("dve", lambda: nc.vector.scalar_tensor_tensor(out=out[:, ta:tb], in0=x[:, ta + d:tb + d], scalar=vec[:, k:k + 1],
                                                                    in1=out[:, ta:tb], op0=ALU.mult, op1=ALU.add),
                      reads=[xb, vecb], writes=[outb])

    def ml_conv_stage(self, XM, XC):
        kb, nc = self.kb, self.nc
        with ExitStack() as es:
            vec = kb.tile(es, [128, 16, 7], F32, "mlvec")
            vecb = Buf()
            kb.dma("sp", vec, self.ins["mlvec"], writes=[vecb])
            pi = Pool(kb, es, 2, [128, T], F32, "mci")
            po = Pool(kb, es, 2, [128, T], F32, "mco")
            for c in range(16):
                x, xb = pi.get()
                kb.dma("sp", x, XM[c * 128:(c + 1) * 128, :], writes=[xb])
                o, ob = po.get()
                self.dwconv(o, ob, x, xb, vec[:, c, :], vecb, 4, 2, 4)
                kb.op("act", lambda: nc.scalar.activation(out=o, in_=o, func=AF.Silu), writes=[ob])
                kb.dma("act", XC[c * 128:(c + 1) * 128, :], o, reads=[ob])
        kb.barrier()

    def ml_core_stage(self, GT, QT, KT, KTOK, VTOK, HSF, HNT):
        kb, nc = self.kb, self.nc
        NCH = T // 128
        V = nc.vector
        QTv = QT.rearrange("(h p) t -> p h t", p=128)
        KTv = KT.rearrange("(h p) t -> p h t", p=128)
        HNTv = HNT.rearrange("(c p) t -> p c t", p=128)
        for z in range(2):
            order = list(range(NCH)) if z == 0 else [1, 0] + list(range(NCH - 1, 1, -1))
            li = 127 if z == 0 else 0
            with ExitStack() as es:
                colz = kb.tile(es, [128, NCH, 32], F32, "colz")
                colb = Buf()
                spb = kb.tile(es, [128, 8, NCH], F32, "spb")
                slb = kb.tile(es, [128, 8, NCH], F32, "slb")
                spbb = Buf()
                mask = kb.tile(es, [128, 128], F32, "mask")
                maskb = Buf()
                kb.dma("sp", mask, self.ins["maskF" if z == 0 else "maskB"], writes=[maskb])
                with ExitStack() as es2:
                    rt = lambda nm: kb.tile(es2, [8, T], F32, nm)
                    ig, fg, G, A, cm, Mx, E1, F_, inter, edm, one8 = [rt(nm) for nm in ("ig", "fg", "G", "A", "cm", "Mx", "E1", "F", "inter", "edm", "one8")]
                    rb = Buf()
                    ch = lambda nm: kb.tile(es2, [8, NCH], F32, nm)
                    Gend, Gprev, maxA, btot, mloc, mq, mprev, Pq, Pn, sp_, sl_ = [ch(nm) for nm in ("Gend", "Gprev", "maxA", "btot", "mloc", "mq", "mprev", "Pq", "Pn", "sp", "sl")]
                    spd = kb.tile(es2, [8, 8, NCH], F32, "spd")
                    sld = kb.tile(es2, [8, 8, NCH], F32, "sld")
                    bif = kb.tile(es2, [8, 4], F32, "bif")
                    kb.dma("sp", ig, GT[z, 0], writes=[rb])
                    kb.dma("sp", fg, GT[z, 1], writes=[rb])
                    kb.op("dve", lambda: V.memset(one8, 1.0), writes=[rb])
                    kb.op("act", lambda: nc.scalar.activation(out=fg, in_=fg, func=AF.Exp, scale=-1.0), writes=[rb])
                    kb.op("act", lambda: nc.scalar.activation(out=fg, in_=fg, func=AF.Ln, bias=self.ones[:8, 0:1]), reads=[self.onesb], writes=[rb])
                    if z == 0:
                        kb.op("dve", lambda: V.tensor_tensor_scan(out=G, data0=one8, data1=fg, initial=0.0, op0=ALU.mult, op1=ALU.subtract), writes=[rb])
                    else:
                        kb.op("dve", lambda: V.tensor_tensor_scan(out=G[:, 0:CTX][:, ::-1], data0=one8[:, 0:CTX], data1=fg[:, 0:CTX][:, ::-1], initial=0.0,
                                                                  op0=ALU.mult, op1=ALU.subtract), writes=[rb])
                        kb.op("dve", lambda: V.tensor_tensor_scan(out=G[:, CTX:T][:, ::-1], data0=one8[:, CTX:T], data1=fg[:, CTX:T][:, ::-1], initial=G[:, 0:1],
                                                                  op0=ALU.mult, op1=ALU.subtract), writes=[rb])
                    kb.op("dve", lambda: V.tensor_tensor(out=A, in0=ig, in1=G, op=ALU.subtract), writes=[rb])
                    for c in range(NCH):
                        sl = slice(c * 128, (c + 1) * 128)
                        rv = (lambda a: a[:, sl][:, ::-1]) if z == 1 else (lambda a: a[:, sl])
                        kb.op("dve", lambda: V.tensor_tensor_scan(out=rv(cm), data0=one8[:, sl], data1=rv(A), initial=-1e30, op0=ALU.mult, op1=ALU.max), writes=[rb])
                    c3 = lambda a: a.rearrange("p (c i) -> p c i", i=128)
                    maxA_nat = c3(cm)[:, :, li]
                    Gend_nat = c3(G)[:, :, li]

                    def to_proc(dst, src):
                        if z == 0:
                            kb.op("dve", lambda: V.tensor_copy(out=dst, in_=src), writes=[rb])
                        else:
                            kb.op("dve", lambda: V.tensor_copy(out=dst[:, 0:1], in_=src[:, 1:2]), writes=[rb])
                            kb.op("dve", lambda: V.tensor_copy(out=dst[:, 1:2], in_=src[:, 0:1]), writes=[rb])
                            kb.op("dve", lambda: V.tensor_copy(out=dst[:, 2:NCH], in_=src[:, 2:NCH][:, ::-1]), writes=[rb])

                    to_proc(Gend, Gend_nat)
                    to_proc(maxA, maxA_nat)
                    kb.op("dve", lambda: V.memset(Gprev[:, 0:1], 0.0), writes=[rb])
                    kb.op("dve", lambda: V.tensor_copy(out=Gprev[:, 1:NCH], in_=Gend[:, 0:NCH - 1]), writes=[rb])
                    kb.op("dve", lambda: V.tensor_tensor(out=btot, in0=Gend, in1=Gprev, op=ALU.subtract), writes=[rb])
                    kb.op("dve", lambda: V.tensor_tensor(out=mloc, in0=Gend, in1=maxA, op=ALU.add), writes=[rb])
                    kb.op("dve", lambda: V.tensor_tensor_scan(out=mq, data0=btot, data1=mloc, initial=0.0, op0=ALU.add, op1=ALU.max), writes=[rb])
                    kb.op("dve", lambda: V.memset(mprev[:, 0:1], 0.0), writes=[rb])
                    kb.op("dve", lambda: V.tensor_copy(out=mprev[:, 1:NCH], in_=mq[:, 0:NCH - 1]), writes=[rb])
                    kb.op("dve", lambda: V.tensor_tensor(out=Pq, in0=mprev, in1=Gprev, op=ALU.subtract), writes=[rb])
                    kb.op("dve", lambda: V.tensor_tensor(out=sp_, in0=btot, in1=mprev, op=ALU.add), writes=[rb])
                    kb.op("dve", lambda: V.tensor_tensor(out=sp_, in0=sp_, in1=mq, op=ALU.subtract), writes=[rb])
                    kb.op("act", lambda: nc.scalar.activation(out=sp_, in_=sp_, func=AF.Exp), writes=[rb])
                    kb.op("dve", lambda: V.tensor_tensor(out=sl_, in0=mloc, in1=mq, op=ALU.subtract), writes=[rb])
                    kb.op("act", lambda: nc.scalar.activation(out=sl_, in_=sl_, func=AF.Exp), writes=[rb])
                    to_proc(Pn, Pq)
                    bcc = lambda a: a.unsqueeze(2).to_broadcast([8, NCH, 128])
                    kb.op("dve", lambda: V.tensor_tensor(out=c3(Mx), in0=c3(cm), in1=bcc(Pn), op=ALU.max), writes=[rb])
                    kb.op("dve", lambda: V.tensor_tensor(out=c3(E1), in0=c3(A), in1=bcc(maxA_nat), op=ALU.subtract), writes=[rb])
                    kb.op("act", lambda: nc.scalar.activation(out=E1, in_=E1, func=AF.Exp), writes=[rb])
                    kb.op("dve", lambda: V.tensor_tensor(out=c3(F_), in0=bcc(maxA_nat), in1=c3(Mx), op=ALU.subtract), writes=[rb])
                    kb.op("act", lambda: nc.scalar.activation(out=F_, in_=F_, func=AF.Exp), writes=[rb])
                    kb.op("dve", lambda: V.tensor_tensor(out=c3(inter), in0=bcc(Pn), in1=c3(Mx), op=ALU.subtract), writes=[rb])
                    kb.op("act", lambda: nc.scalar.activation(out=inter, in_=inter, func=AF.Exp), writes=[rb])
                    kb.op("dve", lambda: V.tensor_tensor(out=edm, in0=G, in1=Mx, op=ALU.add), writes=[rb])
                    kb.op("act", lambda: nc.scalar.activation(out=edm, in_=edm, func=AF.Exp, scale=-1.0), writes=[rb])
                    for c in range(NCH):
                        ps, psb = kb.ps()
                        for ai, arr in enumerate((E1, F_, inter, edm)):
                            kb.op("pe", lambda: nc.tensor.matmul(ps[:, ai * 8:(ai + 1) * 8], arr[:, c * 128:(c + 1) * 128], self.ident[:8, :8], start=True, stop=True),
                                  reads=[rb, self.identb], writes=[psb], signal=(ai == 3))
                        kb.op("act", lambda: nc.scalar.copy(out=colz[:, c, :], in_=ps[:, 0:32]), reads=[psb], writes=[colb])
                    idb = self.ident[:8, :8].unsqueeze(2).to_broadcast([8, 8, NCH])
                    for (src, dd_, dst) in ((sp_, spd, spb), (sl_, sld, slb)):
                        kb.op("dve", lambda: V.tensor_tensor(out=dd_, in0=idb, in1=src.unsqueeze(1).to_broadcast([8, 8, NCH]), op=ALU.mult),
                              reads=[self.identb], writes=[rb])
                        ps, psb = kb.ps()
                        kb.op("pe", lambda: nc.tensor.matmul(ps[:, 0:8 * NCH], self.ones[:8, :], dd_.rearrange("p h q -> p (h q)"), start=True, stop=True),
                              reads=[rb, self.onesb], writes=[psb])
                        kb.op("act", lambda: nc.scalar.copy(out=dst.rearrange("p h q -> p (h q)"), in_=ps[:, 0:8 * NCH]), reads=[psb], writes=[spbb])
                    kb.barrier()
                pkt = Pool(kb, es, 2, [128, 8, 128], F32R, "kt")
                pqt = Pool(kb, es, 2, [128, 8, 128], F32R, "qt")
                pktok = Pool(kb, es, 2, [128, 1024], F32R, "ktok")
                pvx = Pool(kb, es, 2, [128, 8, 258], F32R, "vext")
                pvw = Pool(kb, es, 2, [128, 8, 258], F32R, "vw")
                for t_, b_ in zip(pvx.t, pvx.b):
                    kb.op("dve", lambda: V.tensor_copy(out=t_[:, :, 256:257], in_=self.ones[:, 0:8].unsqueeze(2)), reads=[self.onesb], writes=[b_])
                    kb.op("dve", lambda: V.tensor_scalar(out=t_[:, :, 257:258], in0=self.ones[:, 0:8].unsqueeze(2), scalar1=0.0, scalar2=None, op0=ALU.mult),
                          reads=[self.onesb], writes=[b_])
                cn = kb.tile(es, [128, 8, 258], F32, "cn")
                cnr = kb.tile(es, [128, 8, 258], F32R, "cnr")
                cnb = [Buf() for _ in range(8)]
                cnrb = [Buf() for _ in range(8)]
                pst = Pool(kb, es, 3, [128, 128], F32R, "sT")
                pt1 = Pool(kb, es, 3, [128, 258], F32, "t1")
                ptc = Pool(kb, es, 3, [128, 258], F32, "tc")
                pdd = Pool(kb, es, 4, [128, 2], F32, "dd")
                phc = Pool(kb, es, 2, [128, 8, 256], F32, "hch")
                if z == 1:
                    phf = Pool(kb, es, 2, [128, 8, 256], F32, "hf")
                    pstt = Pool(kb, es, 2, [128, 8, 6], F32, "bst")
                    pmv = Pool(kb, es, 2, [128, 8, 2], F32, "bmv")
                    prs = Pool(kb, es, 2, [128, 8], F32, "brs")
                    phnt = Pool(kb, es, 2, [128, 16, 128], F32, "hnt")
                    epst = kb.tile(es, [128, 1], F32, "eps")
                    epsb = Buf()
                    kb.op("dve", lambda: V.memset(epst, LN_EPS), writes=[epsb])
                for q, c in enumerate(order):
                    sl = slice(c * 128, (c + 1) * 128)
                    kt, ktb = pkt.get()
                    qt, qtb = pqt.get()
                    ktok, ktokb = pktok.get()
                    vx, vxb = pvx.get()
                    vw, vwb = pvw.get()
                    kb.dma("pool", kt, KTv[:, :, sl], writes=[ktb])
                    kb.dma("pool", qt, QTv[:, :, sl], writes=[qtb])
                    kb.dma("pool", ktok, KTOK[sl, :], writes=[ktokb])
                    kb.dma("pool", vx[:, :, 0:256], VTOK[sl, :].rearrange("s (h v) -> s h v", h=8), writes=[vxb])
                    kb.op("dve", lambda: V.tensor_tensor(out=vw, in0=vx, in1=colz[:, c, 0:8].unsqueeze(2).to_broadcast([128, 8, 258]), op=ALU.mult),
                          reads=[vxb, colb], writes=[vwb])
                    hch, hcb = phc.get()
                    for h in range(8):
                        E1c = colz[:, c, h:h + 1]
                        Fc = colz[:, c, 8 + h:9 + h]
                        inc = colz[:, c, 16 + h:17 + h]
                        edc = colz[:, c, 24 + h:25 + h]
                        psS, psSb = kb.ps()
                        kb.op("pe", lambda: nc.tensor.matmul(psS[:, 0:128], kt[:, h, :], qt[:, h, :], start=True, stop=True), reads=[ktb, qtb], writes=[psSb])
                        sT, sTb = pst.get()
                        kb.op("dve", lambda: V.scalar_tensor_tensor(out=sT, in0=psS[:, 0:128], scalar=E1c, in1=mask, op0=ALU.mult, op1=ALU.mult),
                              reads=[psSb, colb, maskb], writes=[sTb])
                        psN, psNb = kb.ps()
                        kb.op("pe", lambda: nc.tensor.matmul(psN[:, 0:258], sT, vx[:, h, :], start=True, stop=True), reads=[sTb, vxb], writes=[psNb])
                        t1, t1b = pt1.get()
                        if q > 0:
                            psI, psIb = kb.ps()
                            kb.op("pe", lambda: nc.tensor.matmul(psI[:, 0:258], qt[:, h, :], cnr[:, h, :], start=True, stop=True), reads=[qtb, cnrb[h]], writes=[psIb])
                            kb.op("act", lambda: nc.scalar.activation(out=t1, in_=psI[:, 0:258], func=AF.Copy, scale=inc), reads=[psIb, colb], writes=[t1b])
                            kb.op("dve", lambda: V.scalar_tensor_tensor(out=t1, in0=psN[:, 0:258], scalar=Fc, in1=t1, op0=ALU.mult, op1=ALU.add),
                                  reads=[psNb, colb], writes=[t1b])
                        else:
                            kb.op("act", lambda: nc.scalar.activation(out=t1, in_=psN[:, 0:258], func=AF.Copy, scale=Fc), reads=[psNb, colb], writes=[t1b])
                        dd, ddb = pdd.get()
                        kb.op("act", lambda: nc.scalar.activation(out=dd[:, 0:1], in_=t1[:, 256:257], func=AF.Abs), reads=[t1b], writes=[ddb])
                        kb.op("dve", lambda: V.tensor_scalar(out=dd[:, 0:1], in0=dd[:, 0:1], scalar1=edc, scalar2=None, op0=ALU.max),
                              reads=[colb], writes=[ddb])
                        kb.op("dve", lambda: V.reciprocal(out=dd[:, 1:2], in_=dd[:, 0:1]), writes=[ddb])
                        kb.op("pool", lambda: nc.gpsimd.tensor_scalar(out=hch[:, h, :], in0=t1[:, 0:256], scalar1=dd[:, 1:2], scalar2=None, op0=ALU.mult),
                              reads=[t1b, ddb], writes=[hcb])
                        if q < NCH - 1:
                            psC, psCb = kb.ps()
                            kb.op("pe", lambda: nc.tensor.matmul(psC[:, 0:258], ktok[:, h * 128:(h + 1) * 128], vw[:, h, :], start=True, stop=True),
                                  reads=[ktokb, vwb], writes=[psCb])
                            if q == 0:
                                kb.op("act", lambda: nc.scalar.activation(out=cn[:, h, :], in_=psC[:, 0:258], func=AF.Copy, scale=slb[:, h, q:q + 1]),
                                      reads=[psCb, spbb], writes=[cnb[h]])
                            else:
                                tc_, tcb = ptc.get()
                                kb.op("act", lambda: nc.scalar.activation(out=tc_, in_=psC[:, 0:258], func=AF.Copy, scale=slb[:, h, q:q + 1]),
                                      reads=[psCb, spbb], writes=[tcb])
                                kb.op("dve", lambda: V.scalar_tensor_tensor(out=cn[:, h, :], in0=cn[:, h, :], scalar=spb[:, h, q:q + 1], in1=tc_,
                                                                            op0=ALU.mult, op1=ALU.add), reads=[tcb, spbb], writes=[cnb[h]])
                            kb.op("pool", lambda: nc.gpsimd.tensor_copy(out=cnr[:, h, :], in_=cn[:, h, :]), reads=[cnb[h]], writes=[cnrb[h]])
                    if z == 0:
                        kb.dma("sp", HSF[sl, :], hch.rearrange("p h v -> p (h v)"), reads=[hcb])
                    else:
                        hf, hfb = phf.get()
                        kb.dma("sp", hf.rearrange("p h v -> p (h v)"), HSF[sl, :], writes=[hfb])
                        kb.op("pool", lambda: nc.gpsimd.tensor_tensor(out=hch, in0=hch, in1=hf, op=ALU.add), reads=[hfb], writes=[hcb])
                        st, stb = pstt.get()
                        mv, mvb = pmv.get()
                        rs, rsb = prs.get()
                        for h in range(8):
                            kb.op("dve", lambda: V.bn_stats(out=st[:, h, :], in_=hch[:, h, :]), reads=[hcb], writes=[stb])
                            kb.op("dve", lambda: V.bn_aggr(out=mv[:, h, :], in_=st[:, h, :]), reads=[stb], writes=[mvb])
                        kb.op("act", lambda: nc.scalar.activation(out=rs, in_=mv[:, :, 1], func=AF.Ln, bias=epst[:, 0:1]), reads=[mvb, epsb], writes=[rsb])
                        kb.op("act", lambda: nc.scalar.activation(out=rs, in_=rs, func=AF.Exp, scale=-0.5), writes=[rsb])
                        for h in range(8):
                            kb.op("dve", lambda: V.tensor_scalar(out=hch[:, h, :], in0=hch[:, h, :], scalar1=mv[:, h, 0:1], scalar2=rs[:, h:h + 1],
                                                                 op0=ALU.subtract, op1=ALU.mult), reads=[mvb, rsb], writes=[hcb])
                        hnt, hntb = phnt.get()
                        h2 = hch.rearrange("p h v -> p (h v)")
                        for g4 in range(4):
                            ps, psb = kb.ps()
                            for j in range(4):
                                fc = g4 * 4 + j
                                kb.op("pe", lambda: nc.tensor.transpose(out=ps[:, j * 128:(j + 1) * 128], in_=h2[:, fc * 128:(fc + 1) * 128], identity=self.ident),
                                      reads=[hcb, self.identb], writes=[psb], signal=(j == 3))
                            kb.op("act", lambda: nc.scalar.copy(out=hnt[:, g4 * 4:(g4 + 1) * 4, :], in_=ps.rearrange("p (c t) -> p c t", c=4)), reads=[psb], writes=[hntb])
                        kb.dma("sp", HNTv[:, :, sl], hnt, reads=[hntb])
            kb.barrier()

    def ml_combine_stage(self, HNT, OT, XC, YPT):
        kb, nc = self.kb, self.nc
        with ExitStack() as es:
            vec = kb.tile(es, [128, 16, 7], F32, "mlvec")
            vecb = Buf()
            kb.dma("sp", vec, self.ins["mlvec"], writes=[vecb])
            p1 = Pool(kb, es, 2, [128, T], F32, "cb1")
            p2 = Pool(kb, es, 2, [128, T], F32, "cb2")
            p3 = Pool(kb, es, 2, [128, T], F32, "cb3")
            for c in range(16):
                rows = slice(c * 128, (c + 1) * 128)
                a, ab = p1.get()
                o, ob = p2.get()
                x, xb = p3.get()
                kb.dma("sp", a, HNT[rows, :], writes=[ab])
                kb.dma("sp", o, OT[rows, :], writes=[ob])
                kb.dma("sp", x, XC[rows, :], writes=[xb])
                kb.op("dve", lambda: nc.vector.scalar_tensor_tensor(out=a, in0=a, scalar=vec[:, c, 5:6], in1=o, op0=ALU.mult, op1=ALU.mult),
                      reads=[ob, vecb], writes=[ab])
                kb.op("dve", lambda: nc.vector.scalar_tensor_tensor(out=a, in0=x, scalar=vec[:, c, 6:7], in1=a, op0=ALU.mult, op1=ALU.add),
                      reads=[xb, vecb], writes=[ab])
                kb.dma("act", YPT[rows, :], a, reads=[ab])
        kb.barrier()

    def mlstm_layer(self, HT, YT, S):
        kb, nc = self.kb, self.nc
        I = self.ins
        XM, XC, QT, KT, KTOK, VTOK, OT, GT, HSF, HNT, YPT = (S[k] for k in ("XM", "XC", "QT", "KT", "KTOK", "VTOK", "OT", "GT", "HSF", "HNT", "YPT"))
        self.linear_stage(HT, I["ml_w_up"][0], XM, 128, TT_ALL, mb=512, mod=(0, 1))
        self.ml_conv_stage(XM, XC)
        self.linear_stage(XC, I["ml_w_q"][0], QT, 128, TT_ALL)
        kscale = 128.0 ** -0.5
        self.linear_stage(XC, I["ml_w_k"][0], KT, 128, TT_ALL,
                          evac=lambda ps, o, mc, col, rd, wr: kb.op("act", lambda: nc.scalar.mul(out=o, in_=ps, mul=kscale), reads=rd, writes=wr))
        self.linear_tok_stage(XC, I["ml_w_k"][0], KTOK,
                              evac=lambda ps, o, rd, wr: kb.op("act", lambda: nc.scalar.mul(out=o, in_=ps, mul=kscale), reads=rd, writes=wr))
        self.linear_tok_stage(XM, I["ml_w_v"][0], VTOK)
        with ExitStack() as esg:
            wif = kb.tile(esg, [128, 16, 2, 16], F32R, "wif")
            wifb = Buf()
            for z in range(2):
                kb.dma("pool", wif[:, :, z, :], I["ml_w_if"][0, z].rearrange("(c p) g -> p c g", p=128), writes=[wifb])
            bif = kb.tile(esg, [8, 4], F32, "bif")
            bifb = Buf()
            kb.dma("sp", bif, I["mlbif"], writes=[bifb])
            pg = Pool(kb, esg, 2, [8, 4, 512], F32, "gout")

            def extra(x, xb, t0, n):
                g, gb = pg.get()
                for z in range(2):
                    for gi in range(2):
                        ps, psb = kb.ps()
                        for c in range(16):
                            kb.op("pe", lambda: nc.tensor.matmul(ps[:8, :n], wif[:, c, z, gi * 8:(gi + 1) * 8], x[:, c, :n], start=(c == 0), stop=(c == 15)),
                                  reads=[wifb, xb], writes=[psb], signal=(c == 15))
                        kb.op("act", lambda: nc.scalar.activation(out=g[:, z * 2 + gi, :n], in_=ps[:8, :n], func=AF.Identity, bias=bif[:, z * 2 + gi:z * 2 + gi + 1]),
                              reads=[psb, bifb], writes=[gb])
                kb.dma("sp", GT.rearrange("z g h t -> h (z g) t")[:, :, t0:t0 + n], g[:, :, :n], reads=[gb])

            self.linear_stage(XM, I["ml_w_o"][0], OT, 128, TT_ALL, extra=extra,
                              evac=lambda ps, o, mc, col, rd, wr: kb.op("act", lambda: nc.scalar.activation(out=o, in_=ps, func=AF.Sigmoid), reads=rd, writes=wr))
        self.ml_core_stage(GT, QT, KT, KTOK, VTOK, HSF, HNT)
        self.ml_combine_stage(HNT, OT, XC, YPT)
        self.linear_stage(YPT, I["ml_w_down"][0], YT, 128, TT_ALL)

    def to_tok_stage(self, XT, XTOK, C, t_lo=0, t_hi=T):
        kb, nc = self.kb, self.nc
        nc_ = C // 128
        XTv = XT.rearrange("(c p) t -> p c t", p=128)
        with ExitStack() as es:
            pin = Pool(kb, es, 3, [128, nc_, 128], F32, "tti")
            pout = Pool(kb, es, 3, [128, C], F32, "tto")
            for t0 in range(t_lo, t_hi, 128):
                a, ab = pin.get()
                kb.dma("sp", a, XTv[:, :, t0:t0 + 128], writes=[ab])
                o, ob = pout.get()
                for g4 in range(nc_ // 4):
                    ps, psb = kb.ps()
                    for j in range(4):
                        c = g4 * 4 + j
                        kb.op("pe", lambda: nc.tensor.transpose(out=ps[:, j * 128:(j + 1) * 128], in_=a[:, c, :], identity=self.ident),
                              reads=[ab, self.identb], writes=[psb], signal=(j == 3))
                    kb.op("act", lambda: nc.scalar.copy(out=o[:, g4 * 512:(g4 + 1) * 512], in_=ps), reads=[psb], writes=[ob])
                kb.dma("act", XTOK[t0:t0 + 128, :], o, reads=[ob])
        kb.barrier()

    def hy_conv_stage(self, UT, UC):
        kb, nc = self.kb, self.nc
        with ExitStack() as es:
            vec = kb.tile(es, [128, 24, 5], F32, "hyvec")
            vecb = Buf()
            kb.dma("sp", vec, self.ins["hyvec"], writes=[vecb])
            pi = Pool(kb, es, 2, [128, T], F32, "hci")
            po = Pool(kb, es, 2, [128, T], F32, "hco")
            for c in range(24):
                x, xb = pi.get()
                kb.dma("sp", x, UT[c * 128:(c + 1) * 128, :], writes=[xb])
                o, ob = po.get()
                self.dwconv(o, ob, x, xb, vec[:, c, 1:5], vecb, 3, 1, 3)
                kb.dma("act", UC[c * 128:(c + 1) * 128, :], o, reads=[ob])
        kb.barrier()

    def hy_filter_stage(self, L, zT, win, FILT):
        kb, nc = self.kb, self.nc
        V = nc.vector
        I = self.ins
        MAGIC = 12582912.0
        with ExitStack() as es:
            zt = kb.tile(es, [33, L], F32, "zt")
            w1 = kb.tile(es, [33, 64], F32, "fw1")
            w2 = kb.tile(es, [64, 64], F32, "fw2")
            w3 = kb.tile(es, [64, 2 * D], F32, "fw3")
            hyf = kb.tile(es, [64, 4], F32, "hyf")
            cb = Buf()
            kb.dma("sp", zt, zT, writes=[cb])
            kb.dma("sp", w1, I["hy_f_w1"][0], writes=[cb])
            kb.dma("sp", w2, I["hy_f_w2"][0], writes=[cb])
            kb.dma("sp", w3, I["hy_f_w3"][0], writes=[cb])
            kb.dma("sp", hyf, I["hyf"], writes=[cb])
            hd1 = kb.tile(es, [64, L], F32, "hd1")
            hd2 = kb.tile(es, [64, L], F32, "hd2")
            h1b, h2b = Buf(), Buf()
            pa = Pool(kb, es, 2, [64, 512], F32, "farg")
            pk = Pool(kb, es, 2, [64, 512], F32, "fk")
            for (lhs, rhs_t, rhsb, dst, dstb, bc, fc) in ((w1, zt, cb, hd1, h1b, 0, 1), (w2, hd1, h1b, hd2, h2b, 2, 3)):
                for t0 in range(0, L, 512):
                    n = min(512, L - t0)
                    ps, psb = kb.ps()
                    kb.op("pe", lambda: nc.tensor.matmul(ps[:64, :n], lhs, rhs_t[:, t0:t0 + n], start=True, stop=True), reads=[cb, rhsb], writes=[psb])
                    a, ab = pa.get()
                    k, kbf = pk.get()
                    kb.op("dve", lambda: V.tensor_scalar(out=a[:, :n], in0=ps[:64, :n], scalar1=hyf[:, bc:bc + 1], scalar2=hyf[:, fc:fc + 1], op0=ALU.add, op1=ALU.mult),
                          reads=[psb, cb], writes=[ab])
                    kb.op("dve", lambda: V.tensor_scalar(out=k[:, :n], in0=a[:, :n], scalar1=1.0 / (2.0 * math.pi), scalar2=MAGIC, op0=ALU.mult, op1=ALU.add),
                          reads=[ab], writes=[kbf])
                    kb.op("dve", lambda: V.tensor_scalar(out=k[:, :n], in0=k[:, :n], scalar1=MAGIC, scalar2=None, op0=ALU.subtract), writes=[kbf])
                    kb.op("dve", lambda: V.scalar_tensor_tensor(out=a[:, :n], in0=k[:, :n], scalar=-2.0 * math.pi, in1=a[:, :n], op0=ALU.mult, op1=ALU.add),
                          reads=[kbf], writes=[ab])
                    kb.op("dve", lambda: V.tensor_scalar(out=a[:, :n], in0=a[:, :n], scalar1=3.1415925, scalar2=-3.1415925, op0=ALU.min, op1=ALU.max), writes=[ab])
                    kb.op("act", lambda: nc.scalar.activation(out=dst[:, t0:t0 + n], in_=a[:, :n], func=AF.Sin), reads=[ab], writes=[dstb])
            pw = Pool(kb, es, 2, [128, D], F32, "fwin")
            po = Pool(kb, es, 2, [128, 2 * D], F32, "fout")
            for sc in range(L // 128):
                wn, wnb = pw.get()
                kb.dma("sp", wn, win[sc * 128:(sc + 1) * 128, :], writes=[wnb])
                o, ob = po.get()
                for cbk in range(4):
                    ps, psb = kb.ps()
                    kb.op("pe", lambda: nc.tensor.matmul(ps, hd2[:, sc * 128:(sc + 1) * 128], w3[:, cbk * 512:(cbk + 1) * 512], start=True, stop=True),
                          reads=[h2b, cb], writes=[psb])
                    kb.op("dve", lambda: V.tensor_tensor(out=o[:, cbk * 512:(cbk + 1) * 512], in0=ps, in1=wn[:, (cbk % 2) * 512:(cbk % 2 + 1) * 512], op=ALU.mult),
                          reads=[psb, wnb], writes=[ob])
                kb.dma("act", FILT[sc * 128:(sc + 1) * 128, :], o, reads=[ob])
        kb.barrier()

    def dft_fwd_stage(self, X, L, Fre, Fim, SPEC):
        kb, nc = self.kb, self.nc
        ns = L // 128
        Xv = X.rearrange("(c p) d -> p c d", p=128)
        with ExitStack() as es:
            xt = kb.tile(es, [128, ns, D], F32R, "dfx")
            xb = Buf()
            for c in range(ns):
                kb.dma("pool", xt[:, c, :], Xv[:, c, :], writes=[xb])
            pf = Pool(kb, es, 2, [128, 2, ns, 128], F32R, "dff")
            po = Pool(kb, es, 2, [128, 2, D], F32, "dfo")
            for fc in range(ns):
                f, fb = pf.get()
                for ri, Fm in enumerate((Fre, Fim)):
                    kb.dma("pool", f[:, ri], Fm[:, fc * 128:(fc + 1) * 128].rearrange("(c p) f -> p c f", p=128), writes=[fb])
                o, ob = po.get()
                for ri in range(2):
                    for cbk in range(2):
                        ps, psb = kb.ps()
                        for c in range(ns):
                            kb.op("pe", lambda: nc.tensor.matmul(ps, f[:, ri, c, :], xt[:, c, cbk * 512:(cbk + 1) * 512], start=(c == 0), stop=(c == ns - 1)),
                                  reads=[fb, xb], writes=[psb], signal=(c == ns - 1))
                        kb.op("act", lambda: nc.scalar.copy(out=o[:, ri, cbk * 512:(cbk + 1) * 512], in_=ps), reads=[psb], writes=[ob])
                kb.dma("sp", SPEC[:, fc * 128:(fc + 1) * 128, :].rearrange("r f d -> f r d"), o, reads=[ob])
        kb.barrier()

    def dft_inv_stage(self, US, HS, L, Cre, Cim, YT, t_off):
        kb, nc = self.kb, self.nc
        V = nc.vector
        ns = L // 128
        TB = 256
        with ExitStack() as es:
            yre = kb.tile(es, [128, ns, 512], F32R, "yre")
            yim = kb.tile(es, [128, ns, 512], F32R, "yim")
            pl = Pool(kb, es, 2, [128, 4, 512], F32, "spl")
            pt = Pool(kb, es, 2, [128, 4, 512], F32, "spt")
            pc = Pool(kb, es, 2, [128, 2, ns, TB], F32R, "cmat")
            po = Pool(kb, es, 2, [128, 4, TB], F32, "ivo")
            YTv = YT.rearrange("(c p) t -> p c t", p=128)
            for half in range(2):
                cs = slice(half * 512, (half + 1) * 512)
                yb = Buf()
                for fc in range(ns):
                    fs = slice(fc * 128, (fc + 1) * 128)
                    l, lb = pl.get()
                    kb.dma("sp", l[:, 0:2, :], US[:, fs, cs].rearrange("r f d -> f r d"), writes=[lb])
                    kb.dma("sp", l[:, 2:4, :], HS[:, fs, cs].rearrange("r f d -> f r d"), writes=[lb])
                    t, tb = pt.get()
                    kb.op("dve", lambda: V.tensor_tensor(out=t[:, 0, :], in0=l[:, 0, :], in1=l[:, 2, :], op=ALU.mult), reads=[lb], writes=[tb])
                    kb.op("dve", lambda: V.tensor_tensor(out=t[:, 1, :], in0=l[:, 1, :], in1=l[:, 3, :], op=ALU.mult), reads=[lb], writes=[tb])
                    kb.op("pool", lambda: nc.gpsimd.tensor_tensor(out=t[:, 2, :], in0=l[:, 0, :], in1=l[:, 3, :], op=ALU.mult), reads=[lb], writes=[tb])
                    kb.op("pool", lambda: nc.gpsimd.tensor_tensor(out=t[:, 3, :], in0=l[:, 1, :], in1=l[:, 2, :], op=ALU.mult), reads=[lb], writes=[tb])
                    kb.op("dve", lambda: V.tensor_tensor(out=yre[:, fc, :], in0=t[:, 0, :], in1=t[:, 1, :], op=ALU.subtract), reads=[tb], writes=[yb])
                    kb.op("dve", lambda: V.tensor_tensor(out=yim[:, fc, :], in0=t[:, 2, :], in1=t[:, 3, :], op=ALU.add), reads=[tb], writes=[yb])
                    if fc == 0:
                        kb.op("dve", lambda: V.tensor_copy(out=yre[0:1, 0, :], in_=t[0:1, 0, :]), reads=[tb], writes=[yb])
                        kb.op("dve", lambda: V.tensor_copy(out=yim[0:1, 0, :], in_=t[0:1, 1, :]), reads=[tb], writes=[yb])
                for tb0 in range(0, L, TB):
                    cm, cmb = pc.get()
                    for ri, Cm in enumerate((Cre, Cim)):
                        kb.dma("pool", cm[:, ri], Cm[:, tb0:tb0 + TB].rearrange("(c p) t -> p c t", p=128), writes=[cmb])
                    o, ob = po.get()
                    for cc in range(4):
                        ps, psb = kb.ps()
                        for fc in range(ns):
                            kb.op("pe", lambda: nc.tensor.matmul(ps[:, :TB], yre[:, fc, cc * 128:(cc + 1) * 128], cm[:, 0, fc, :], start=(fc == 0), stop=False),
                                  reads=[yb, cmb], writes=[psb], signal=False)
                            kb.op("pe", lambda: nc.tensor.matmul(ps[:, :TB], yim[:, fc, cc * 128:(cc + 1) * 128], cm[:, 1, fc, :], start=False, stop=(fc == ns - 1)),
                                  reads=[yb, cmb], writes=[psb], signal=(fc == ns - 1))
                        kb.op("act", lambda: nc.scalar.copy(out=o[:, cc, :], in_=ps[:, :TB]), reads=[psb], writes=[ob])
                    kb.dma("sp", YTv[:, half * 4:(half + 1) * 4, t_off + tb0:t_off + tb0 + TB], o, reads=[ob])
        kb.barrier()

    def hy_combine_stage(self, YT_, VT, GT_, ZT, skip_idx):
        kb, nc = self.kb, self.nc
        with ExitStack() as es:
            sk = kb.tile(es, [128, KC, 2], F32, "hyskip")
            skb = Buf()
            kb.dma("sp", sk, self.ins["hyskip"], writes=[skb])
            p1 = Pool(kb, es, 2, [128, T], F32, "hb1")
            p2 = Pool(kb, es, 2, [128, T], F32, "hb2")
            p3 = Pool(kb, es, 2, [128, T], F32, "hb3")
            for c in range(KC):
                rows = slice(c * 128, (c + 1) * 128)
                y, yb = p1.get()
                v, vb = p2.get()
                g, gb = p3.get()
                kb.dma("sp", y, YT_[rows, :], writes=[yb])
                kb.dma("sp", v, VT[rows, :], writes=[vb])
                kb.dma("sp", g, GT_[rows, :], writes=[gb])
                kb.op("dve", lambda: nc.vector.scalar_tensor_tensor(out=y, in0=v, scalar=sk[:, c, skip_idx:skip_idx + 1], in1=y, op0=ALU.mult, op1=ALU.add),
                      reads=[vb, skb], writes=[yb])
                kb.op("pool", lambda: nc.gpsimd.tensor_tensor(out=y, in0=y, in1=g, op=ALU.mult), reads=[gb], writes=[yb])
                kb.dma("act", ZT[rows, :], y, reads=[yb])
        kb.barrier()

    def hyena_layer(self, HT, YT, S):
        kb, nc = self.kb, self.nc
        I = self.ins
        UT, UC, VTOK, Y1T, Z1T, Z1TOK, Y2T, Z2T, US = (S[k] for k in ("hUT", "hUC", "hVTOK", "hY1T", "hZ1T", "hZ1TOK", "hY2T", "hZ2T", "hUS"))
        vec_bias = {}
        with ExitStack() as esb:
            vec = kb.tile(esb, [128, 24, 5], F32, "hyvecb")
            vecb = Buf()
            kb.dma("sp", vec, I["hyvec"], writes=[vecb])
            self.linear_stage(HT, I["hy_w_in"][0], UT, 128, TT_ALL, mb=512, mod=(0, 1),
                              evac=lambda ps, o, mc, col, rd, wr: kb.op("act", lambda: nc.scalar.activation(out=o, in_=ps, func=AF.Identity, bias=vec[:, mc, 0:1]),
                                                                        reads=rd + [vecb], writes=wr))
        steps = []
        steps.append(lambda: self.hy_conv_stage(UT, UC))
        steps.append(lambda: self.to_tok_stage(UC[0:D, :], VTOK, D))
        cfg = {}
        for L in (CTX, SEQ):
            FILT = S["hFILT%d" % L]
            steps.append(lambda L=L, FILT=FILT: self.hy_filter_stage(L, I["hzT%d" % L], I["hwin%d" % L], FILT))
            HSs = []
            for k in range(2):
                HS = S["hHS%d_%d" % (L, k)]
                steps.append(lambda L=L, FILT=FILT, k=k, HS=HS: self.dft_fwd_stage(FILT[:, k * D:(k + 1) * D], L, I["Fre%d" % L], I["Fim%d" % L], HS))
                HSs.append(HS)
            cfg[L] = HSs
        for (L, off) in ((CTX, 0), (SEQ, CTX)):
            steps.append(lambda L=L, off=off: self.dft_fwd_stage(VTOK[off:off + L, :], L, I["Fre%d" % L], I["Fim%d" % L], US[:, 0:L, :]))
            steps.append(lambda L=L, off=off: self.dft_inv_stage(US[:, 0:L, :], cfg[L][0], L, I["Cre%d" % L], I["Cim%d" % L], Y1T, off))
        steps.append(lambda: self.hy_combine_stage(Y1T, UC[0:D, :], UC[D:2 * D, :], Z1T, 0))
        steps.append(lambda: self.to_tok_stage(Z1T, Z1TOK, D))
        for (L, off) in ((CTX, 0), (SEQ, CTX)):
            steps.append(lambda L=L, off=off: self.dft_fwd_stage(Z1TOK[off:off + L, :], L, I["Fre%d" % L], I["Fim%d" % L], US[:, 0:L, :]))
            steps.append(lambda L=L, off=off: self.dft_inv_stage(US[:, 0:L, :], cfg[L][1], L, I["Cre%d" % L], I["Cim%d" % L], Y2T, off))
        steps.append(lambda: self.hy_combine_stage(Y2T, Z1T, UC[2 * D:3 * D, :], Z2T, 1))
        steps.append(lambda: self.linear_stage(Z2T, I["hy_w_out"][0], YT, 128, TT_ALL, mb=1024))
        for st in steps[:getattr(self, "hy_stop", 999)]:
            st()

    def decl_hyena(self):
        for nm, shp in (("hy_w_in", [1, D, 3 * D]), ("hyvec", [128, 24, 5]), ("hy_f_w1", [1, 33, 64]), ("hy_f_w2", [1, 64, 64]), ("hy_f_w3", [1, 64, 2 * D]),
                        ("hyf", [64, 4]), ("hyskip", [128, KC, 2]), ("hy_w_out", [1, D, D])):
            self.inp(nm, shp)
        for L in (CTX, SEQ):
            self.inp("hzT%d" % L, [33, L])
            self.inp("hwin%d" % L, [L, D])
            for nm in ("Fre", "Fim", "Cre", "Cim"):
                self.inp("%s%d" % (nm, L), [L, L])
        S = {}
        for nm, shp in (("hUT", [3 * D, T]), ("hUC", [3 * D, T]), ("hVTOK", [T, D]), ("hY1T", [D, T]), ("hZ1T", [D, T]), ("hZ1TOK", [T, D]),
                        ("hY2T", [D, T]), ("hZ2T", [D, T]), ("hUS", [2, SEQ, D])):
            S[nm] = self.scratch(nm, shp)
        for L in (CTX, SEQ):
            S["hFILT%d" % L] = self.scratch("hFILT%d" % L, [L, 2 * D])
            for k in range(2):
                S["hHS%d_%d" % (L, k)] = self.scratch("hHS%d_%d" % (L, k), [2, L, D])
        return S

    def decl_all(self):
        for nm, shp in (("x", [SEQ, D]), ("ctx", [CTX, D]), ("pos", [SEQ, D]), ("ident", [128, 128]), ("cc", [128, KC, 2]),
                        ("ada_w", [DEPTH, D, 6 * D]), ("adab", [DEPTH, 128, 48]), ("lng", [128, DEPTH * 2 * KC]), ("lnb", [128, DEPTH * 2 * KC]),
                        ("routw", [128, KC, NE]), ("routb", [128, NE]), ("selE", [NE, NE, 128]),
                        ("moe_w_gate", [DEPTH, NE, D, DEXP]), ("moe_w_up", [DEPTH, NE, D, DEXP]), ("moe_w_down", [DEPTH, NE, DEXP, D]),
                        ("rg_w_in", [2, D, 2 * D_RNN]), ("rgv", [2, RG_BS, 16, 11]), ("rg_gate_a_w", [2, 2, 16, RG_BS, RG_BS]),
                        ("rg_gate_x_w", [2, 2, 16, RG_BS, RG_BS]), ("rg_w_out", [2, D_RNN, D]),
                        ("ml_w_up", [1, D, 2048]), ("ml_w_q", [1, 2048, D]), ("ml_w_k", [1, 2048, D]), ("ml_w_v", [1, 2048, 2048]),
                        ("ml_w_o", [1, 2048, 2048]), ("ml_w_if", [1, 2, 2048, 16]), ("ml_w_down", [1, 2048, D]),
                        ("mlvec", [128, 16, 7]), ("mlbif", [8, 4]), ("maskF", [128, 128]), ("maskB", [128, 128])):
            self.inp(nm, shp)
        S = self.decl_hyena()
        for nm, shp in (("XM", [2048, T]), ("XC", [2048, T]), ("QT", [D, T]), ("KT", [D, T]), ("KTOK", [T, D]), ("VTOK", [T, 2048]),
                        ("OT", [2048, T]), ("GT", [2, 2, 8, T]), ("HSF", [T, 2048]), ("HNT", [2048, T]), ("YPT", [2048, T]), ("MT", [D_RNN, T])):
            S[nm] = self.scratch(nm, shp)
        return S

    def mixer(self, layer, HT, YT, S):
        kind, jl = layer % 3, layer // 3
        if kind == 0:
            self.rglru_stage(HT, S["MT"], jl)
            self.linear_stage(S["MT"], self.ins["rg_w_out"][jl], YT, RG_BS, TT_ALL, mb=1024)
        elif kind == 1:
            self.mlstm_layer(HT, YT, S)
        else:
            self.hyena_layer(HT, YT, S)

    def build(self, stages=None):
        kb = self.kb
        self.es_global = ExitStack()
        S = self.decl_all()
        out = self.nc.dram_tensor("out", [SEQ, D], F32, kind="ExternalOutput").ap()
        HT = self.scratch("HT", [D, T])
        YT = self.scratch("YT", [D, T])
        self.persist()
        self.prep(HT)
        if stages is None:
            for layer in range(self.n_layers):
                last = layer == DEPTH - 1
                self.mod_stage(layer)
                self.mixer(layer, HT, YT, S)
                self.ln_stage(HT, YT, layer, 0, TT_ALL)
                self.moe_stage(HT, YT, layer, with_ctx=not last)
                self.ln_stage(HT, YT, layer, 1, TT_X if last else TT_ALL)
        elif stages == "ln_test":
            self.mod_stage(1)
            self.prep(YT)
            self.ln_stage(HT, YT, 1, 1, TT_ALL)
        elif stages == "rg_test":
            self.mod_stage(0)
            self.mixer(0, HT, YT, S)
        elif stages == "ml_test":
            self.mod_stage(1)
            self.mixer(1, HT, YT, S)
        elif stages == "hy_test":
            self.mod_stage(2)
            self.mixer(2, HT, YT, S)
        elif stages == "moe_test":
            self.mod_stage(0)
            self.moe_stage(HT, YT, 0, True)
        self.final(HT, out)
        kb.barrier()
        self.es_global.close()
        return self.nc


def host_consts():
    c = {}
    c["ident"] = np.eye(128, dtype=np.float32)
    rows = SEQ // 64
    quarter = D // 4
    omega = (1.0 / (10000.0 ** (np.arange(quarter, dtype=np.float32) / np.float32(quarter)))).astype(np.float32)
    ar = np.arange(rows, dtype=np.float32)[:, None] * omega
    ac = np.arange(64, dtype=np.float32)[:, None] * omega
    er = np.concatenate([np.sin(ar), np.cos(ar)], -1)
    ec = np.concatenate([np.sin(ac), np.cos(ac)], -1)
    half = D // 2
    pos = np.concatenate([np.broadcast_to(er[:, None], (rows, 64, half)), np.broadcast_to(ec[None], (rows, 64, half))], -1)
    c["pos"] = np.ascontiguousarray(pos.reshape(SEQ, D).astype(np.float32))
    sel = np.zeros((NE, NE, 128), np.float32)
    for e in range(NE):
        sel[e, e, :] = 1.0
    c["selE"] = sel
    c["maskF"] = np.ascontiguousarray(np.triu(np.ones((128, 128), np.float32)))
    c["maskB"] = np.ascontiguousarray(np.tril(np.ones((128, 128), np.float32)))
    for L in (CTX, SEQ):
        n = 2 * L
        t01 = np.linspace(0.0, 1.0, L, dtype=np.float32)
        bands = np.linspace(1e-4, 15.0, 16, dtype=np.float32)
        ang = (np.float32(2.0 * math.pi / L) * np.arange(L, dtype=np.float32)[:, None]) * bands[None, :]
        z = np.concatenate([t01[:, None], np.cos(ang), -np.sin(ang)], -1).astype(np.float32)
        c["hzT%d" % L] = np.ascontiguousarray(z.T)
        dist = np.abs(np.arange(L) - L // 2).astype(np.float32) * np.float32(2.0 / L)
        d_max = math.log(1e-2) / 0.3
        d_min = math.log(1e-2) / 1.5
        deltas = np.abs(np.linspace(d_min, d_max, D, dtype=np.float32))
        c["hwin%d" % L] = np.ascontiguousarray(np.exp(-dist[:, None] * deltas[None, :]).astype(np.float32))
        sidx = np.arange(L, dtype=np.int64)
        fidx = np.arange(L, dtype=np.int64)
        th = 2.0 * np.pi * ((sidx[:, None] * fidx[None, :]) % n).astype(np.float64) / n
        Fre = np.cos(th)
        Fim = -np.sin(th)
        Fim[:, 0] = (-1.0) ** sidx
        tau = sidx + L // 2
        th2 = 2.0 * np.pi * ((fidx[:, None] * tau[None, :]) % n).astype(np.float64) / n
        Cre = (2.0 / n) * np.cos(th2)
        Cim = -(2.0 / n) * np.sin(th2)
        Cre[0, :] = 1.0 / n
        Cim[0, :] = (1.0 / n) * ((-1.0) ** tau)
        for nm, a in (("Fre", Fre), ("Fim", Fim), ("Cre", Cre), ("Cim", Cim)):
            c["%s%d" % (nm, L)] = np.ascontiguousarray(a.astype(np.float32))
    return c


def host_inputs(inputs, b, consts):
    f = lambda a: np.ascontiguousarray(np.asarray(a, dtype=np.float32))
    m = {}
    m["x"] = f(inputs["x"][b])
    m["ctx"] = f(inputs["ctx"][b])
    m["pos"] = consts["pos"]
    m["ident"] = consts["ident"]
    cc = np.stack([np.asarray(inputs["c"][b]), np.asarray(inputs["c_ctx"])], -1)
    m["cc"] = f(cc.reshape(KC, 128, 2).transpose(1, 0, 2))
    m["ada_w"] = f(inputs["ada_w"])
    m["adab"] = f(np.asarray(inputs["ada_b"]).reshape(DEPTH, 48, 128).transpose(0, 2, 1))
    m["lng"] = f(np.asarray(inputs["ln_g"]).reshape(DEPTH * 2 * KC, 128).T)
    m["lnb"] = f(np.asarray(inputs["ln_b"]).reshape(DEPTH * 2 * KC, 128).T)
    m["routw"] = f(np.asarray(inputs["router_w"]).reshape(KC, 128, NE).transpose(1, 0, 2))
    m["routb"] = f(np.broadcast_to(np.asarray(inputs["router_b"])[None, :], (128, NE)))
    m["selE"] = consts["selE"]
    for k in ("moe_w_gate", "moe_w_up", "moe_w_down", "rg_w_in", "rg_gate_a_w", "rg_gate_x_w", "rg_w_out"):
        m[k] = f(inputs[k])
    A = np.asarray
    na = A(inputs["rg_conv_w"]).shape[0]
    vecs = [A(inputs["rg_conv_w"])[:, k] for k in range(4)] + [A(inputs["rg_conv_b"])]
    vecs += [A(inputs["rg_gate_a_b"])[:, z] for z in range(2)] + [A(inputs["rg_gate_x_b"])[:, z] for z in range(2)]
    vecs += [A(inputs["rg_lambda"])[:, z] for z in range(2)]
    rgv = np.stack(vecs, -1)
    m["rgv"] = f(rgv.reshape(na, 16, RG_BS, 11).transpose(0, 2, 1, 3))
    for k in ("ml_w_up", "ml_w_q", "ml_w_k", "ml_w_v", "ml_w_o", "ml_w_if", "ml_w_down"):
        m[k] = f(inputs[k])
    mv = [A(inputs["ml_conv_w"])[0, k] for k in range(4)] + [A(inputs["ml_conv_b"])[0], A(inputs["ml_norm_g"])[0], A(inputs["ml_skip"])[0]]
    m["mlvec"] = f(np.stack(mv, -1).reshape(16, 128, 7).transpose(1, 0, 2))
    bif = A(inputs["ml_b_if"])[0]
    m["mlbif"] = f(bif.reshape(2, 2, 8).transpose(2, 0, 1).reshape(8, 4))
    m["maskF"] = consts["maskF"]
    m["maskB"] = consts["maskB"]
    for k in ("hy_w_in", "hy_f_w1", "hy_f_w2", "hy_f_w3", "hy_w_out"):
        m[k] = f(inputs[k])
    hv = [A(inputs["hy_b_in"])[0]] + [A(inputs["hy_conv_w"])[0, k] for k in range(3)] + [A(inputs["hy_conv_b"])[0]]
    m["hyvec"] = f(np.stack(hv, -1).reshape(24, 128, 5).transpose(1, 0, 2))
    m["hyf"] = f(np.stack([A(inputs["hy_f_b1"])[0], A(inputs["hy_f_freq1"])[0], A(inputs["hy_f_b2"])[0], A(inputs["hy_f_freq2"])[0]], -1))
    m["hyskip"] = f(A(inputs["hy_skip"])[0].reshape(2, KC, 128).transpose(2, 1, 0))
    for k, v in consts.items():
        if k[0] in "hFC" and k not in m:
            m[k] = v
    return m


_CACHE = {}


def kernel(**inputs):
    consts = host_consts()
    prog = Prog()
    nc = prog.build()
    shared = None
    in_maps = []
    for b in range(8):
        m = host_inputs(inputs, b, consts) if shared is None else dict(shared)
        if shared is None:
            shared = m
        else:
            m["x"] = np.ascontiguousarray(np.asarray(inputs["x"][b], dtype=np.float32))
            m["ctx"] = np.ascontiguousarray(np.asarray(inputs["ctx"][b], dtype=np.float32))
            cc = np.stack([np.asarray(inputs["c"][b]), np.asarray(inputs["c_ctx"])], -1)
            m["cc"] = np.ascontiguousarray(cc.reshape(KC, 128, 2).transpose(1, 0, 2).astype(np.float32))
        in_maps.append({k: v for k, v in m.items() if k in prog.ins})
    res = run_bass_kernel_spmd(nc, in_maps, core_ids=list(range(8)))
    return np.stack([np.asarray(r["out"], dtype=np.float32) for r in res.results], 0)
```

```python
import math
from contextlib import ExitStack
import numpy as np
import concourse.bass as bass
import concourse.mybir as mybir
from concourse.bass_utils import run_bass_kernel_spmd

F32 = mybir.dt.float32
F32R = mybir.dt.float32r
I32 = mybir.dt.int32
AF = mybir.ActivationFunctionType
ALU = mybir.AluOpType
AX = mybir.AxisListType

D = 1024
KC = 8
SEQ = 2048
CTX = 256
T = SEQ + CTX
DEPTH = 4
ALPHA = (2.0 * DEPTH) ** 0.25
LN_EPS = 1e-6
D_RNN = 1408
RG_BS = 88
NE = 16
DEXP = 512
TT_ALL = [(0, 256), (256, 512), (768, 512), (1280, 512), (1792, 512)]
TT_X = [(256, 512), (768, 512), (1280, 512), (1792, 512)]


class Buf:
    __slots__ = ("w", "r")

    def __init__(self):
        self.w = {}
        self.r = {}


class KB:
    NRING = 12

    def __init__(self):
        nc = self.nc = bass.Bass("TRN2", target_bir_lowering=False)
        self.eng = {"pe": nc.tensor, "act": nc.scalar, "dve": nc.vector, "pool": nc.gpsimd, "sp": nc.sync}
        self.sems = {}
        self.esem = {}
        self.cnt = {}
        for e in ("pe", "act", "dve", "pool"):
            s = nc.alloc_semaphore("s_" + e)
            self.sems[s.num] = s
            self.esem[e] = s.num
            self.cnt[e] = 0
        self.dring = {}
        self.dcnt = {}
        self.dnext = {}
        for q in ("sp", "act", "pool"):
            self.dring[q] = []
            for i in range(self.NRING):
                s = nc.alloc_semaphore("d_%s%d" % (q, i))
                self.sems[s.num] = s
                self.dring[q].append(s.num)
                self.dcnt[s.num] = 0
            self.dnext[q] = 0
        self.seen = {e: {} for e in self.eng}
        self.uid = 0
        self.psum = [nc.alloc_psum_tensor("ps%d" % i, [128, 512], F32).ap() for i in range(8)]
        self.psb = [Buf() for _ in range(8)]
        self.pnext = 0

    def name(self, p):
        self.uid += 1
        return "%s_%d" % (p, self.uid)

    def _wait(self, e, need):
        seen = self.seen[e]
        own = self.esem.get(e)
        for s, c in need.items():
            if c <= 0:
                continue
            if e == "pe" and s == own:
                continue
            if seen.get(s, 0) >= c:
                continue
            self.eng[e].wait_ge(self.sems[s], c)
            seen[s] = c

    @staticmethod
    def _merge(dst, src):
        for s, c in src.items():
            if dst.get(s, 0) < c:
                dst[s] = c

    def _deps(self, reads, writes):
        need = {}
        for b in reads:
            self._merge(need, b.w)
        for b in writes:
            self._merge(need, b.w)
            self._merge(need, b.r)
        return need

    def op(self, e, fn, reads=(), writes=(), signal=True):
        self._wait(e, self._deps(reads, writes))
        inst = fn()
        s = self.esem[e]
        if signal:
            inst.then_inc(self.sems[s], 1)
            self.cnt[e] += 1
            ev = self.cnt[e]
        else:
            ev = self.cnt[e] + 1
        for b in writes:
            b.w = {s: ev}
            b.r = {}
        for b in reads:
            if b.r.get(s, 0) < ev:
                b.r[s] = ev
        return inst

    def dma(self, q, out, in_, reads=(), writes=(), **kw):
        i = self.dnext[q]
        self.dnext[q] = (i + 1) % self.NRING
        s = self.dring[q][i]
        need = self._deps(reads, writes)
        if need.get(s, 0) < self.dcnt[s]:
            need[s] = self.dcnt[s]
        self._wait(q, need)
        self.eng[q].dma_start(out=out, in_=in_, **kw).then_inc(self.sems[s], 16)
        self.dcnt[s] += 16
        ev = self.dcnt[s]
        for b in writes:
            b.w = {s: ev}
            b.r = {}
        for b in reads:
            if b.r.get(s, 0) < ev:
                b.r[s] = ev

    def idma(self, out, out_offset, in_, in_offset, reads=(), writes=()):
        q = "pool"
        i = self.dnext[q]
        self.dnext[q] = (i + 1) % self.NRING
        s = self.dring[q][i]
        need = self._deps(reads, writes)
        if need.get(s, 0) < self.dcnt[s]:
            need[s] = self.dcnt[s]
        self._wait(q, need)
        self.nc.gpsimd.indirect_dma_start(out=out, out_offset=out_offset, in_=in_, in_offset=in_offset).then_inc(self.sems[s], 16)
        self.dcnt[s] += 16
        ev = self.dcnt[s]
        for b in writes:
            b.w = {s: ev}
            b.r = {}
        for b in reads:
            if b.r.get(s, 0) < ev:
                b.r[s] = ev

    def barrier(self):
        need = {}
        for e, s in self.esem.items():
            need[s] = self.cnt[e]
        for s, c in self.dcnt.items():
            need[s] = c
        for e in self.eng:
            self._wait(e, need)

    def ps(self):
        i = self.pnext
        self.pnext = (i + 1) % 8
        return self.psum[i], self.psb[i]

    def tile(self, es, shape, dtype=F32, name="t"):
        t = es.enter_context(self.nc.sbuf_tensor(self.name(name), list(shape), dtype))
        return t.ap()


class Pool:
    def __init__(self, kb, es, n, shape, dtype=F32, name="p"):
        self.t = [kb.tile(es, shape, dtype, name) for _ in range(n)]
        self.b = [Buf() for _ in range(n)]
        self.i = 0
        self.n = n

    def get(self):
        i = self.i
        self.i = (i + 1) % self.n
        return self.t[i], self.b[i]


class Prog:
    def __init__(self, n_layers=DEPTH, dbg=()):
        self.kb = KB()
        self.nc = self.kb.nc
        self.n_layers = n_layers
        self.ins = {}
        self.dbg = {}
        self.dbg_want = set(dbg)

    def inp(self, name, shape):
        t = self.nc.dram_tensor(name, list(shape), F32, kind="ExternalInput").ap()
        self.ins[name] = t
        return t

    def scratch(self, name, shape):
        kind = "ExternalOutput" if name in self.dbg_want else "Internal"
        t = self.nc.dram_tensor(name, list(shape), F32, kind=kind).ap()
        if name in self.dbg_want:
            self.dbg[name] = t
        return t

    def prep(self, HT):
        kb, nc = self.kb, self.nc
        x, ctx, pos, ident = self.ins["x"], self.ins["ctx"], self.ins["pos"], self.ins["ident"]
        HTv = HT.rearrange("(c p) t -> p c t", p=128)
        with ExitStack() as es:
            idt = kb.tile(es, [128, 128], F32, "ident")
            idb = Buf()
            kb.dma("sp", idt, ident, writes=[idb])
            pin = Pool(kb, es, 3, [128, D], F32, "pin")
            ppos = Pool(kb, es, 3, [128, D], F32, "ppos")
            pout = Pool(kb, es, 3, [128, KC, 128], F32, "pout")
            for ti in range(T // 128):
                t0 = ti * 128
                a, ab = pin.get()
                if t0 < CTX:
                    kb.dma("sp", a, ctx[t0:t0 + 128, :], writes=[ab])
                else:
                    kb.dma("sp", a, x[t0 - CTX:t0 - CTX + 128, :], writes=[ab])
                    p_, pb = ppos.get()
                    kb.dma("sp", p_, pos[t0 - CTX:t0 - CTX + 128, :], writes=[pb])
                    kb.op("dve", lambda: nc.vector.tensor_tensor(out=a, in0=a, in1=p_, op=ALU.add), reads=[pb], writes=[ab])
                o, ob = pout.get()
                for half in range(2):
                    ps, psb = kb.ps()
                    for j in range(4):
                        c = half * 4 + j
                        kb.op("pe", lambda: nc.tensor.transpose(out=ps[:, j * 128:(j + 1) * 128], in_=a[:, c * 128:(c + 1) * 128], identity=idt),
                              reads=[ab, idb], writes=[psb], signal=(j == 3))
                    kb.op("act", lambda: nc.scalar.copy(out=o[:, half * 4:half * 4 + 4, :], in_=ps.rearrange("p (c t) -> p c t", c=4)),
                          reads=[psb], writes=[ob])
                kb.dma("act", HTv[:, :, t0:t0 + 128], o, reads=[ob])
        kb.barrier()

    def final(self, HT, out):
        kb, nc = self.kb, self.nc
        ident = self.ins["ident"]
        HTv = HT.rearrange("(c p) t -> p c t", p=128)
        with ExitStack() as es:
            idt = kb.tile(es, [128, 128], F32, "ident")
            idb = Buf()
            kb.dma("sp", idt, ident, writes=[idb])
            pin = Pool(kb, es, 3, [128, KC, 128], F32, "fin")
            pout = Pool(kb, es, 3, [128, D], F32, "fout")
            for ti in range(SEQ // 128):
                t0 = CTX + ti * 128
                a, ab = pin.get()
                kb.dma("sp", a, HTv[:, :, t0:t0 + 128], writes=[ab])
                o, ob = pout.get()
                for half in range(2):
                    ps, psb = kb.ps()
                    for j in range(4):
                        c = half * 4 + j
                        kb.op("pe", lambda: nc.tensor.transpose(out=ps[:, j * 128:(j + 1) * 128], in_=a[:, c, :], identity=idt),
                              reads=[ab, idb], writes=[psb], signal=(j == 3))
                    kb.op("act", lambda: nc.scalar.copy(out=o[:, half * 512:(half + 1) * 512], in_=ps), reads=[psb], writes=[ob])
                kb.dma("act", out[ti * 128:(ti + 1) * 128, :], o, reads=[ob])
        kb.barrier()

    def persist(self):
        kb, nc = self.kb, self.nc
        es = self.es_global
        self.ident = kb.tile(es, [128, 128], F32, "identp")
        self.identb = Buf()
        kb.dma("sp", self.ident, self.ins["ident"], writes=[self.identb])
        self.ones = kb.tile(es, [128, 128], F32, "ones")
        self.onesb = Buf()
        kb.op("dve", lambda: nc.vector.memset(self.ones, 1.0), writes=[self.onesb])
        self.lng = kb.tile(es, [128, DEPTH * 2 * KC], F32, "lng")
        self.lnb = kb.tile(es, [128, DEPTH * 2 * KC], F32, "lnb")
        self.lnbuf = Buf()
        kb.dma("sp", self.lng, self.ins["lng"], writes=[self.lnbuf])
        kb.dma("sp", self.lnb, self.ins["lnb"], writes=[self.lnbuf])
        self.routw = kb.tile(es, [128, KC, NE], F32, "routw")
        self.routb = kb.tile(es, [128, NE], F32, "routb")
        self.sel = kb.tile(es, [NE, NE, 128], F32, "selE")
        self.routbuf = Buf()
        kb.dma("sp", self.routw, self.ins["routw"], writes=[self.routbuf])
        kb.dma("sp", self.routb, self.ins["routb"], writes=[self.routbuf])
        kb.dma("sp", self.sel, self.ins["selE"], writes=[self.routbuf])
        self.modT = kb.tile(es, [128, 48, 2], F32, "modT")
        self.modb = Buf()
        kb.barrier()

    def mod_stage(self, layer):
        kb, nc = self.kb, self.nc
        ada_w = self.ins["ada_w"]
        modT = self.modT
        with ExitStack() as es:
            cc = kb.tile(es, [128, KC, 2], F32, "cc")
            ccb = Buf()
            kb.dma("sp", cc, self.ins["cc"], writes=[ccb])
            sc = kb.tile(es, [128, KC, 2], F32, "sc")
            scb = Buf()
            kb.op("act", lambda: nc.scalar.activation(out=sc, in_=cc, func=AF.Silu), reads=[ccb], writes=[scb])
            ab = kb.tile(es, [128, 48], F32, "adab")
            abb = Buf()
            kb.dma("sp", ab, self.ins["adab"][layer], writes=[abb])
            wp = Pool(kb, es, 2, [128, KC, 1024], F32, "adaw")
            ps, psb = kb.ps()
            for s in range(6):
                w, wb = wp.get()
                kb.dma("sp", w, ada_w[layer, :, s * 1024:(s + 1) * 1024].rearrange("(c p) f -> p c f", p=128), writes=[wb])
                for mm in range(8):
                    m = s * 8 + mm
                    for kc in range(KC):
                        kb.op("pe", lambda: nc.tensor.matmul(ps[:, 2 * m:2 * m + 2], w[:, kc, mm * 128:(mm + 1) * 128], sc[:, kc, :],
                                                             start=(kc == 0), stop=(kc == KC - 1)),
                              reads=[wb, scb], writes=[psb], signal=(kc == KC - 1 and mm == 7))
            psv = ps[:, 0:96].rearrange("p (m j) -> p m j", j=2)
            for j in range(2):
                kb.op("dve", lambda: nc.vector.tensor_tensor(out=modT[:, :, j], in0=psv[:, :, j], in1=ab, op=ALU.add),
                      reads=[psb, abb], writes=[self.modb])
            for s in (1, 4):
                kb.op("dve", lambda: nc.vector.tensor_scalar(out=modT[:, s * 8:(s + 1) * 8, :], in0=modT[:, s * 8:(s + 1) * 8, :],
                                                             scalar1=1.0, scalar2=None, op0=ALU.add), writes=[self.modb])
            for s in (2, 5):
                kb.op("dve", lambda: nc.vector.tensor_scalar(out=modT[:, s * 8:(s + 1) * 8, :], in0=modT[:, s * 8:(s + 1) * 8, :],
                                                             scalar1=1.0 / ALPHA, scalar2=None, op0=ALU.mult), writes=[self.modb])
        kb.barrier()

    def ln_stage(self, HT, YT, layer, j, tiles):
        kb, nc = self.kb, self.nc
        g_slot = 2 if j == 0 else 5
        modT = self.modT
        HTv = HT.rearrange("(c p) t -> p c t", p=128)
        YTv = YT.rearrange("(c p) t -> p c t", p=128)
        lcol = (layer * 2 + j) * KC
        eps = LN_EPS / (ALPHA * ALPHA)
        with ExitStack() as es:
            ph = Pool(kb, es, 2, [128, KC, 512], F32, "lnh")
            py = Pool(kb, es, 2, [128, KC, 512], F32, "lny")
            pq = Pool(kb, es, 1, [128, KC, 512], F32, "lnq")
            po = Pool(kb, es, 2, [128, KC, 512], F32, "lno")
            pm = Pool(kb, es, 2, [128, 512], F32, "lnm")
            pv = Pool(kb, es, 2, [128, 512], F32, "lnv")
            epst = kb.tile(es, [128, 1], F32, "eps")
            epsb = Buf()
            kb.op("dve", lambda: nc.vector.memset(epst, eps), writes=[epsb])
            for (t0, n) in tiles:
                col = 1 if t0 < CTX else 0
                h, hb = ph.get()
                y, yb = py.get()
                kb.dma("sp", h[:, :, :n], HTv[:, :, t0:t0 + n], writes=[hb])
                kb.dma("sp", y[:, :, :n], YTv[:, :, t0:t0 + n], writes=[yb])
                for c in range(KC):
                    m = g_slot * 8 + c
                    kb.op("dve", lambda: nc.vector.scalar_tensor_tensor(out=h[:, c, :n], in0=y[:, c, :n], scalar=modT[:, m, col:col + 1],
                                                                        in1=h[:, c, :n], op0=ALU.mult, op1=ALU.add),
                          reads=[yb, self.modb], writes=[hb])
                q, qb = pq.get()
                kb.op("act", lambda: nc.scalar.activation(out=q[:, :, :n], in_=h[:, :, :n], func=AF.Square), reads=[hb], writes=[qb])
                ps1, ps1b = kb.ps()
                ps2, ps2b = kb.ps()
                for c in range(KC):
                    kb.op("pe", lambda: nc.tensor.matmul(ps1[:, :n], self.ones, h[:, c, :n], start=(c == 0), stop=(c == KC - 1)),
                          reads=[hb, self.onesb], writes=[ps1b], signal=(c == KC - 1))
                for c in range(KC):
                    kb.op("pe", lambda: nc.tensor.matmul(ps2[:, :n], self.ones, q[:, c, :n], start=(c == 0), stop=(c == KC - 1)),
                          reads=[qb, self.onesb], writes=[ps2b], signal=(c == KC - 1))
                mean, mb = pm.get()
                var, vb = pv.get()
                kb.op("act", lambda: nc.scalar.mul(out=mean[:, :n], in_=ps1[:, :n], mul=1.0 / D), reads=[ps1b], writes=[mb])
                kb.op("dve", lambda: nc.vector.tensor_tensor(out=var[:, :n], in0=mean[:, :n], in1=mean[:, :n], op=ALU.mult), reads=[mb], writes=[vb])
                kb.op("dve", lambda: nc.vector.scalar_tensor_tensor(out=var[:, :n], in0=ps2[:, :n], scalar=1.0 / D, in1=var[:, :n],
                                                                    op0=ALU.mult, op1=ALU.subtract), reads=[ps2b], writes=[vb])
                kb.op("act", lambda: nc.scalar.activation(out=var[:, :n], in_=var[:, :n], func=AF.Ln, bias=epst[:, 0:1]), reads=[epsb], writes=[vb])
                kb.op("act", lambda: nc.scalar.activation(out=var[:, :n], in_=var[:, :n], func=AF.Exp, scale=-0.5), writes=[vb])
                o, ob = po.get()
                for c in range(KC):
                    kb.op("dve", lambda: nc.vector.tensor_tensor(out=h[:, c, :n], in0=h[:, c, :n], in1=mean[:, :n], op=ALU.subtract),
                          reads=[mb], writes=[hb])
                    kb.op("pool", lambda: nc.gpsimd.tensor_tensor(out=h[:, c, :n], in0=h[:, c, :n], in1=var[:, :n], op=ALU.mult),
                          reads=[vb], writes=[hb])
                    kb.op("act", lambda: nc.scalar.activation(out=o[:, c, :n], in_=h[:, c, :n], func=AF.Identity,
                                                              scale=self.lng[:, lcol + c:lcol + c + 1], bias=self.lnb[:, lcol + c:lcol + c + 1]),
                          reads=[hb, self.lnbuf], writes=[ob])
                kb.dma("act", HTv[:, :, t0:t0 + n], o[:, :, :n], reads=[ob])
        kb.barrier()

    def moe_stage(self, HT, YT, layer, with_ctx):
        kb, nc = self.kb, self.nc
        modT = self.modT
        HTv = HT.rearrange("(c p) t -> p c t", p=128)
        YTv = YT.rearrange("(c p) t -> p c t", p=128)
        wg_d, wu_d, wd_d = self.ins.get("moe_w_gate"), self.ins.get("moe_w_up"), self.ins.get("moe_w_down")
        if with_ctx:
            groups = [[(0, 256), (256, 512)], [(768, 512), (1280, 256)], [(1536, 512), (2048, 256)]]
        else:
            groups = [[(256, 512), (768, 256)], [(1024, 512), (1536, 256)], [(1792, 512)]]
        GMAX = 768
        with ExitStack() as es:
            B = kb.tile(es, [128, KC, GMAX], F32R, "moeB")
            Bb = Buf()
            acc = kb.tile(es, [128, KC, GMAX], F32, "moeacc")
            accb = [Buf() for _ in range(2)]
            ar = kb.tile(es, [128, 2, GMAX], F32R, "moea")
            arb = [Buf() for _ in range(2)]
            gT = kb.tile(es, [NE, GMAX], F32, "gatesT")
            gTb = Buf()
            pfx = Pool(kb, es, 2, [128, KC, 128], F32, "fx32")
            pgbc = Pool(kb, es, 2, [128, GMAX], F32, "gbc")
            psil = Pool(kb, es, 2, [128, 512], F32, "sil")
            ptmp = Pool(kb, es, 2, [128, 512], F32, "tmp")
            pwg = Pool(kb, es, 2, [128, KC, 256], F32R, "wg")
            pwu = Pool(kb, es, 2, [128, KC, 256], F32R, "wu")
            pwd = Pool(kb, es, 2, [128, 2, D], F32R, "wd")
            prt = Pool(kb, es, 2, [128, 160], F32, "rt")
            for grp in groups:
                g0 = grp[0][0]
                G = sum(n for _, n in grp)
                tiles = []
                l0 = 0
                for (t0, n) in grp:
                    tiles.append((t0, n, l0))
                    l0 += n
                for si in range(G // 128):
                    t0 = g0 + si * 128
                    col = 1 if t0 < CTX else 0
                    fx, fxb = pfx.get()
                    kb.dma("sp", fx, HTv[:, :, t0:t0 + 128], writes=[fxb])
                    for c in range(KC):
                        kb.op("act", lambda: nc.scalar.activation(out=fx[:, c, :], in_=fx[:, c, :], func=AF.Identity,
                                                                  scale=modT[:, 4 * 8 + c, col:col + 1], bias=modT[:, 3 * 8 + c, col:col + 1]),
                              reads=[self.modb], writes=[fxb])
                    kb.op("dve", lambda: nc.vector.tensor_copy(out=B[:, :, si * 128:(si + 1) * 128], in_=fx), reads=[fxb], writes=[Bb])
                    ps, psb = kb.ps()
                    for c in range(KC):
                        kb.op("pe", lambda: nc.tensor.matmul(ps[:, 0:NE], fx[:, c, :], self.routw[:, c, :], start=(c == 0), stop=(c == KC - 1)),
                              reads=[fxb, self.routbuf], writes=[psb], signal=(c == KC - 1))
                    r, rb = prt.get()
                    sc = r[:, 0:16]
                    sel = r[:, 16:32]
                    eq = r[:, 32:48]
                    s2 = r[:, 48:64]
                    m1 = r[:, 64:68]
                    m2 = r[:, 68:72]
                    gs = r[:, 72:76]
                    og = r[:, 76:80]
                    t4 = r[:, 80:84]
                    gmax = r[:, 84:85]
                    m2b = r[:, 85:86]
                    wsum = r[:, 86:87]
                    selm = r[:, 96:112]
                    ch = r[:, 112:128]
                    w = r[:, 128:144]
                    gts = r[:, 144:160]
                    v3 = lambda a: a.rearrange("p (g k) -> p g k", k=4)
                    bc = lambda a: a.unsqueeze(2).to_broadcast([128, 4, 4])
                    V = nc.vector
                    kb.op("act", lambda: nc.scalar.activation(out=sc, in_=ps[:, 0:NE], func=AF.Sigmoid), reads=[psb], writes=[rb])
                    kb.op("dve", lambda: V.tensor_tensor(out=sel, in0=sc, in1=self.routb, op=ALU.add), reads=[self.routbuf], writes=[rb])
                    kb.op("dve", lambda: V.tensor_reduce(out=m1, in_=v3(sel), axis=AX.X, op=ALU.max), writes=[rb])
                    kb.op("dve", lambda: V.tensor_tensor(out=v3(eq), in0=v3(sel), in1=bc(m1), op=ALU.is_equal), writes=[rb])
                    kb.op("dve", lambda: V.scalar_tensor_tensor(out=s2, in0=eq, scalar=-1e30, in1=sel, op0=ALU.mult, op1=ALU.add), writes=[rb])
                    kb.op("dve", lambda: V.tensor_reduce(out=m2, in_=v3(s2), axis=AX.X, op=ALU.max), writes=[rb])
                    kb.op("dve", lambda: V.tensor_tensor(out=gs, in0=m1, in1=m2, op=ALU.add), writes=[rb])
                    kb.op("dve", lambda: V.tensor_reduce(out=gmax, in_=gs, axis=AX.X, op=ALU.max), writes=[rb])
                    kb.op("dve", lambda: V.tensor_scalar(out=og, in0=gs, scalar1=gmax, scalar2=None, op0=ALU.is_equal), writes=[rb])
                    kb.op("dve", lambda: V.tensor_tensor(out=t4, in0=og, in1=m2, op=ALU.mult), writes=[rb])
                    kb.op("dve", lambda: V.tensor_reduce(out=m2b, in_=t4, axis=AX.X, op=ALU.add), writes=[rb])
                    kb.op("dve", lambda: V.tensor_scalar(out=t4, in0=og, scalar1=-1.0, scalar2=1e30, op0=ALU.add, op1=ALU.mult), writes=[rb])
                    kb.op("dve", lambda: V.tensor_tensor(out=v3(selm), in0=v3(sel), in1=bc(t4), op=ALU.add), writes=[rb])
                    kb.op("dve", lambda: V.tensor_scalar(out=ch, in0=selm, scalar1=m2b, scalar2=None, op0=ALU.is_ge), writes=[rb])
                    kb.op("dve", lambda: V.tensor_tensor(out=w, in0=sc, in1=ch, op=ALU.mult), writes=[rb])
                    kb.op("dve", lambda: V.tensor_reduce(out=wsum, in_=w, axis=AX.X, op=ALU.add), writes=[rb])
                    kb.op("dve", lambda: V.reciprocal(out=wsum, in_=wsum), writes=[rb])
                    kb.op("dve", lambda: V.tensor_scalar(out=gts, in0=w, scalar1=wsum, scalar2=None, op0=ALU.mult), writes=[rb])
                    pst, pstb = kb.ps()
                    kb.op("pe", lambda: nc.tensor.transpose(out=pst[0:NE, 0:128], in_=gts, identity=self.ident), reads=[rb, self.identb], writes=[pstb])
                    kb.op("act", lambda: nc.scalar.copy(out=gT[:, si * 128:(si + 1) * 128], in_=pst[0:NE, 0:128]), reads=[pstb], writes=[gTb])
                first = True
                for e in range(NE):
                    gbc, gbcb = pgbc.get()
                    for (t0, n, l0) in tiles:
                        psg, psgb = kb.ps()
                        kb.op("pe", lambda: nc.tensor.matmul(psg[:, :n], self.sel[:, e, :], gT[:, l0:l0 + n], start=True, stop=True),
                              reads=[gTb, self.routbuf], writes=[psgb])
                        kb.op("act", lambda: nc.scalar.copy(out=gbc[:, l0:l0 + n], in_=psg[:, :n]), reads=[psgb], writes=[gbcb])
                    for jh in range(2):
                        wg, wgb = pwg.get()
                        wu, wub = pwu.get()
                        wd, wdb = pwd.get()
                        kb.dma("pool", wg, wg_d[layer * NE + e, :, jh * 256:(jh + 1) * 256].rearrange("(c p) f -> p c f", p=128), writes=[wgb])
                        kb.dma("pool", wu, wu_d[layer * NE + e, :, jh * 256:(jh + 1) * 256].rearrange("(c p) f -> p c f", p=128), writes=[wub])
                        kb.dma("pool", wd, wd_d[layer * NE + e, jh * 256:(jh + 1) * 256, :].rearrange("(j p) d -> p j d", p=128), writes=[wdb])
                        for (t0, n, l0) in tiles:
                            for jj in range(2):
                                pg, pgb = kb.ps()
                                pu, pub = kb.ps()
                                for c in range(KC):
                                    kb.op("pe", lambda: nc.tensor.matmul(pg[:, :n], wg[:, c, jj * 128:(jj + 1) * 128], B[:, c, l0:l0 + n],
                                                                         start=(c == 0), stop=(c == KC - 1)),
                                          reads=[wgb, Bb], writes=[pgb], signal=(c == KC - 1))
                                for c in range(KC):
                                    kb.op("pe", lambda: nc.tensor.matmul(pu[:, :n], wu[:, c, jj * 128:(jj + 1) * 128], B[:, c, l0:l0 + n],
                                                                         start=(c == 0), stop=(c == KC - 1)),
                                          reads=[wub, Bb], writes=[pub], signal=(c == KC - 1))
                                sl, slb = psil.get()
                                tm, tmb = ptmp.get()
                                kb.op("act", lambda: nc.scalar.activation(out=sl[:, :n], in_=pg[:, :n], func=AF.Silu), reads=[pgb], writes=[slb])
                                kb.op("dve", lambda: nc.vector.tensor_tensor(out=tm[:, :n], in0=sl[:, :n], in1=pu[:, :n], op=ALU.mult),
                                      reads=[slb, pub], writes=[tmb])
                                kb.op("pool", lambda: nc.gpsimd.tensor_tensor(out=ar[:, jj, l0:l0 + n], in0=tm[:, :n], in1=gbc[:, l0:l0 + n], op=ALU.mult),
                                      reads=[tmb, gbcb], writes=[arb[jj]])
                        for (t0, n, l0) in tiles:
                            for dc in range(KC):
                                po, pob = kb.ps()
                                for jj in range(2):
                                    kb.op("pe", lambda: nc.tensor.matmul(po[:, :n], wd[:, jj, dc * 128:(dc + 1) * 128], ar[:, jj, l0:l0 + n],
                                                                         start=(jj == 0), stop=(jj == 1)),
                                          reads=[wdb, arb[jj]], writes=[pob], signal=(jj == 1))
                                ab_ = accb[dc % 2]
                                if first:
                                    kb.op("dve", lambda: nc.vector.tensor_copy(out=acc[:, dc, l0:l0 + n], in_=po[:, :n]), reads=[pob], writes=[ab_])
                                else:
                                    kb.op("dve", lambda: nc.vector.tensor_tensor(out=acc[:, dc, l0:l0 + n], in0=acc[:, dc, l0:l0 + n], in1=po[:, :n], op=ALU.add),
                                          reads=[pob], writes=[ab_])
                        first = False
                kb.dma("act", YTv[:, :, g0:g0 + G], acc[:, :, :G], reads=accb)
        kb.barrier()

    def moe_sparse_stage(self, HT, YT, layer, with_ctx):
        kb, nc = self.kb, self.nc
        V = nc.vector
        modT = self.modT
        I = self.ins
        XS, YS = self.XS, self.YS
        HTv = HT.rearrange("(c p) t -> p c t", p=128)
        YTv = YT.rearrange("(c p) t -> p c t", p=128)
        wg_d = [I["moe_w_gate0"], I["moe_w_gate1"]]
        wu_d = [I["moe_w_up0"], I["moe_w_up1"]]
        wd_d = [I["moe_w_down0"], I["moe_w_down1"]]
        t_lo = 0 if with_ctx else CTX
        Tn = T - t_lo
        NTK = Tn // 128
        NT = (2 * Tn + NE * 255) // 256
        MAGIC = 12582912.0
        with ExitStack() as es:
            gsel = kb.tile(es, [128, NTK, 2], F32, "gsel")
            idxa = kb.tile(es, [128, NTK, 2], I32, "idxa")
            selb = Buf()
            widx = kb.tile(es, [128, 64], I32, "widx")
            widxf = kb.tile(es, [128, 64], F32, "widxf")
            pcol = kb.tile(es, [128, 1], F32, "pcol")
            widxb = Buf()
            with ExitStack() as es1:
                fxtok = kb.tile(es1, [128, NTK, D], F32, "fxtok")
                fxtb = [Buf() for _ in range(NTK)]
                gall = kb.tile(es1, [128, NTK, NE], F32, "gall")
                gallb = Buf()
                gT = kb.tile(es1, [NE, Tn], F32, "gTs")
                gTb = Buf()
                pos = kb.tile(es1, [NE, Tn], F32, "pos")
                one16 = kb.tile(es1, [NE, Tn], F32, "one16")
                rb = Buf()
                sm = kb.tile(es1, [NE, 8], F32, "smallr")
                cmp_ = kb.tile(es1, [NE, 64], F32, "cmp")
                iot = kb.tile(es1, [NE, 64], F32, "iota")
                tril = kb.tile(es1, [NE, NE], F32, "tril")
                eidf = kb.tile(es1, [1, 64], F32, "eidf")
                kb.dma("sp", iot, I["iota64"], writes=[rb])
                kb.dma("sp", pcol, I["pidx"], writes=[rb])
                kb.dma("sp", tril, I["triL"], writes=[rb])
                kb.op("dve", lambda: V.memset(one16, 1.0), writes=[rb])
                pfx = Pool(kb, es1, 2, [128, KC, 128], F32, "fx32s")
                prt = Pool(kb, es1, 2, [128, 160], F32, "rts")
                for si in range(NTK):
                    t0 = t_lo + si * 128
                    col = 1 if t0 < CTX else 0
                    fx, fxb = pfx.get()
                    kb.dma("sp", fx, HTv[:, :, t0:t0 + 128], writes=[fxb])
                    for c in range(KC):
                        kb.op("act", lambda: nc.scalar.activation(out=fx[:, c, :], in_=fx[:, c, :], func=AF.Identity,
                                                                  scale=modT[:, 4 * 8 + c, col:col + 1], bias=modT[:, 3 * 8 + c, col:col + 1]),
                              reads=[self.modb], writes=[fxb])
                    ps, psb = kb.ps()
                    for c in range(KC):
                        kb.op("pe", lambda: nc.tensor.matmul(ps[:, 0:NE], fx[:, c, :], self.routw[:, c, :], start=(c == 0), stop=(c == KC - 1)),
                              reads=[fxb, self.routbuf], writes=[psb], signal=(c == KC - 1))
                    for half in range(2):
                        pst2, pst2b = kb.ps()
                        for j in range(4):
                            c = half * 4 + j
                            kb.op("pe", lambda: nc.tensor.transpose(out=pst2[:, j * 128:(j + 1) * 128], in_=fx[:, c, :], identity=self.ident),
                                  reads=[fxb, self.identb], writes=[pst2b], signal=(j == 3))
                        kb.op("act" if half == 0 else "dve",
                              (lambda: nc.scalar.copy(out=fxtok[:, si, half * 512:(half + 1) * 512], in_=pst2)) if half == 0 else
                              (lambda: V.tensor_copy(out=fxtok[:, si, half * 512:(half + 1) * 512], in_=pst2)),
                              reads=[pst2b], writes=[fxtb[si]])
                    r, rb2 = prt.get()
                    sc = r[:, 0:16]
                    sel = r[:, 16:32]
                    eq = r[:, 32:48]
                    s2 = r[:, 48:64]
                    m1 = r[:, 64:68]
                    m2 = r[:, 68:72]
                    gs = r[:, 72:76]
                    og = r[:, 76:80]
                    t4 = r[:, 80:84]
                    gmax = r[:, 84:85]
                    m2b = r[:, 85:86]
                    wsum = r[:, 86:87]
                    selm = r[:, 96:112]
                    ch = r[:, 112:128]
                    w = r[:, 128:144]
                    gts = gall[:, si, :]
                    v3 = lambda a: a.rearrange("p (g k) -> p g k", k=4)
                    bc = lambda a: a.unsqueeze(2).to_broadcast([128, 4, 4])
                    kb.op("act", lambda: nc.scalar.activation(out=sc, in_=ps[:, 0:NE], func=AF.Sigmoid), reads=[psb], writes=[rb2])
                    kb.op("dve", lambda: V.tensor_tensor(out=sel, in0=sc, in1=self.routb, op=ALU.add), reads=[self.routbuf], writes=[rb2])
                    kb.op("dve", lambda: V.tensor_reduce(out=m1, in_=v3(sel), axis=AX.X, op=ALU.max), writes=[rb2])
                    kb.op("dve", lambda: V.tensor_tensor(out=v3(eq), in0=v3(sel), in1=bc(m1), op=ALU.is_equal), writes=[rb2])
                    kb.op("dve", lambda: V.scalar_tensor_tensor(out=s2, in0=eq, scalar=-1e30, in1=sel, op0=ALU.mult, op1=ALU.add), writes=[rb2])
                    kb.op("dve", lambda: V.tensor_reduce(out=m2, in_=v3(s2), axis=AX.X, op=ALU.max), writes=[rb2])
                    kb.op("dve", lambda: V.tensor_tensor(out=gs, in0=m1, in1=m2, op=ALU.add), writes=[rb2])
                    kb.op("dve", lambda: V.tensor_reduce(out=gmax, in_=gs, axis=AX.X, op=ALU.max), writes=[rb2])
                    kb.op("dve", lambda: V.tensor_scalar(out=og, in0=gs, scalar1=gmax, scalar2=None, op0=ALU.is_equal), writes=[rb2])
                    kb.op("dve", lambda: V.tensor_tensor(out=t4, in0=og, in1=m2, op=ALU.mult), writes=[rb2])
                    kb.op("dve", lambda: V.tensor_reduce(out=m2b, in_=t4, axis=AX.X, op=ALU.add), writes=[rb2])
                    kb.op("dve", lambda: V.tensor_scalar(out=t4, in0=og, scalar1=-1.0, scalar2=1e30, op0=ALU.add, op1=ALU.mult), writes=[rb2])
                    kb.op("dve", lambda: V.tensor_tensor(out=v3(selm), in0=v3(sel), in1=bc(t4), op=ALU.add), writes=[rb2])
                    kb.op("dve", lambda: V.tensor_scalar(out=ch, in0=selm, scalar1=m2b, scalar2=None, op0=ALU.is_ge), writes=[rb2])
                    kb.op("dve", lambda: V.tensor_tensor(out=w, in0=sc, in1=ch, op=ALU.mult), writes=[rb2])
                    kb.op("dve", lambda: V.tensor_reduce(out=wsum, in_=w, axis=AX.X, op=ALU.add), writes=[rb2])
                    kb.op("dve", lambda: V.reciprocal(out=wsum, in_=wsum), writes=[rb2])
                    kb.op("dve", lambda: V.tensor_scalar(out=gts, in0=w, scalar1=wsum, scalar2=None, op0=ALU.mult), reads=[rb2], writes=[gallb])
                    pst, pstb = kb.ps()
                    kb.op("pe", lambda: nc.tensor.transpose(out=pst[0:NE, 0:128], in_=gts, identity=self.ident), reads=[gallb, self.identb], writes=[pstb])
                    kb.op("act", lambda: nc.scalar.copy(out=gT[:, si * 128:(si + 1) * 128], in_=pst[0:NE, 0:128]), reads=[pstb], writes=[gTb])
                cnt, nt_, toff, tend, roff = (sm[:, k:k + 1] for k in range(5))
                kb.op("dve", lambda: V.tensor_scalar(out=gT, in0=gT, scalar1=0.0, scalar2=None, op0=ALU.is_gt), writes=[gTb])
                kb.op("dve", lambda: V.tensor_tensor_scan(out=pos, data0=one16, data1=gT, initial=0.0, op0=ALU.mult, op1=ALU.add), reads=[gTb], writes=[rb])
                kb.op("dve", lambda: V.tensor_scalar(out=nt_, in0=pos[:, Tn - 1:Tn], scalar1=1.0 / 256.0, scalar2=255.0 / 512.0, op0=ALU.mult, op1=ALU.add), writes=[rb])
                kb.op("dve", lambda: V.tensor_scalar(out=nt_, in0=nt_, scalar1=MAGIC, scalar2=None, op0=ALU.add), writes=[rb])
                kb.op("dve", lambda: V.tensor_scalar(out=nt_, in0=nt_, scalar1=MAGIC, scalar2=None, op0=ALU.subtract), writes=[rb])
                ps, psb = kb.ps()
                kb.op("pe", lambda: nc.tensor.matmul(ps[0:NE, 0:1], tril, nt_, start=True, stop=True), reads=[rb], writes=[psb])
                kb.op("dve", lambda: V.tensor_copy(out=toff, in_=ps[0:NE, 0:1]), reads=[psb], writes=[rb])
                kb.op("dve", lambda: V.tensor_tensor(out=tend, in0=toff, in1=nt_, op=ALU.add), writes=[rb])
                kb.op("dve", lambda: V.tensor_scalar(out=roff, in0=toff, scalar1=256.0, scalar2=None, op0=ALU.mult), writes=[rb])
                kb.op("dve", lambda: V.tensor_scalar(out=pos, in0=pos, scalar1=roff, scalar2=None, op0=ALU.add), writes=[rb])
                kb.op("dve", lambda: V.tensor_tensor(out=pos, in0=pos, in1=gT, op=ALU.mult), reads=[gTb], writes=[rb])
                kb.op("dve", lambda: V.tensor_scalar(out=cmp_, in0=iot, scalar1=tend, scalar2=None, op0=ALU.is_ge), writes=[rb])
                ps, psb = kb.ps()
                kb.op("pe", lambda: nc.tensor.matmul(ps[0:1, 0:64], self.ones[:NE, 0:1], cmp_, start=True, stop=True), reads=[rb, self.onesb], writes=[psb])
                kb.op("dve", lambda: V.tensor_scalar(out=eidf, in0=ps[0:1, 0:64], scalar1=float(NE - 1), scalar2=float(layer * NE), op0=ALU.min, op1=ALU.add),
                      reads=[psb], writes=[rb])
                psw, pswb = kb.ps()
                kb.op("pe", lambda: nc.tensor.matmul(psw[:, 0:64], self.ones[0:1, :], eidf, start=True, stop=True), reads=[rb, self.onesb], writes=[pswb])
                kb.op("dve", lambda: V.tensor_scalar(out=widxf, in0=psw[:, 0:64], scalar1=128.0, scalar2=pcol[:, 0:1], op0=ALU.mult, op1=ALU.add),
                      reads=[pswb, rb], writes=[widxb])
                kb.op("dve", lambda: V.tensor_copy(out=widx, in_=widxf), writes=[widxb])
                prb = Pool(kb, es1, 2, [128, 96], F32, "rtb")
                for si in range(NTK):
                    ps, psb = kb.ps()
                    kb.op("pe", lambda: nc.tensor.transpose(out=ps[:, 0:NE], in_=pos[:, si * 128:(si + 1) * 128], identity=self.ident[:NE, :NE]),
                          reads=[rb, self.identb], writes=[psb])
                    r, rb2 = prb.get()
                    dt_ = r[:, 0:16]
                    e1 = r[:, 16:32]
                    tm = r[:, 32:48]
                    v2 = r[:, 48:64]
                    e2 = r[:, 64:80]
                    r1 = r[:, 80:81]
                    r2 = r[:, 81:82]
                    gts = gall[:, si, :]
                    kb.op("act", lambda: nc.scalar.copy(out=dt_, in_=ps[:, 0:NE]), reads=[psb], writes=[rb2])
                    kb.op("dve", lambda: V.tensor_reduce(out=r1, in_=dt_, axis=AX.X, op=ALU.max), writes=[rb2])
                    kb.op("dve", lambda: V.tensor_scalar(out=e1, in0=dt_, scalar1=r1, scalar2=None, op0=ALU.is_equal), writes=[rb2])
                    kb.op("dve", lambda: V.tensor_tensor(out=tm, in0=e1, in1=dt_, op=ALU.mult), writes=[rb2])
                    kb.op("dve", lambda: V.tensor_tensor(out=v2, in0=dt_, in1=tm, op=ALU.subtract), writes=[rb2])
                    kb.op("dve", lambda: V.tensor_reduce(out=r2, in_=v2, axis=AX.X, op=ALU.max), writes=[rb2])
                    kb.op("dve", lambda: V.tensor_scalar(out=e2, in0=v2, scalar1=r2, scalar2=None, op0=ALU.is_equal), writes=[rb2])
                    kb.op("dve", lambda: V.tensor_tensor(out=e1, in0=e1, in1=gts, op=ALU.mult), reads=[gallb], writes=[rb2])
                    kb.op("dve", lambda: V.tensor_tensor(out=e2, in0=e2, in1=gts, op=ALU.mult), reads=[gallb], writes=[rb2])
                    kb.op("dve", lambda: V.tensor_reduce(out=gsel[:, si, 0:1], in_=e1, axis=AX.X, op=ALU.add), reads=[rb2], writes=[selb])
                    kb.op("dve", lambda: V.tensor_reduce(out=gsel[:, si, 1:2], in_=e2, axis=AX.X, op=ALU.add), reads=[rb2], writes=[selb])
                    kb.op("dve", lambda: V.tensor_scalar(out=idxa[:, si, :], in0=r[:, 80:82], scalar1=-1.0, scalar2=None, op0=ALU.add), reads=[rb2], writes=[selb])
                    for j in range(2):
                        kb.idma(XS, bass.IndirectOffsetOnAxis(ap=idxa[:, si, j:j + 1], axis=0), fxtok[:, si, :], None, reads=[selb, fxtb[si]])
                kb.barrier()
            if getattr(self, "moe_stop", 9) < 2:
                return
            with ExitStack() as es2:
                pwg = Pool(kb, es2, 2, [128, KC, DEXP], F32R, "swg")
                pwu = Pool(kb, es2, 2, [128, KC, DEXP], F32R, "swu")
                pwd = Pool(kb, es2, 2, [128, 4, D], F32R, "swd")
                pxt = Pool(kb, es2, 4, [128, D], F32, "sxt")
                pxT = Pool(kb, es2, 2, [128, KC, 256], F32R, "sxT")
                psl = Pool(kb, es2, 3, [128, 256], F32, "ssl")
                pa = Pool(kb, es2, 2, [128, 4, 256], F32R, "sa")
                pys = Pool(kb, es2, 4, [128, D], F32, "sys")
                for i in range(NT):
                    wg, wgb = pwg.get()
                    wu, wub = pwu.get()
                    wd, wdb = pwd.get()
                    off = bass.IndirectOffsetOnAxis(ap=widx[:, i:i + 1], axis=0)
                    for hh in range(2):
                        kb.idma(wg[:, hh * 4:(hh + 1) * 4, :].rearrange("p c f -> p (c f)"), None, wg_d[hh], off, reads=[widxb], writes=[wgb])
                        kb.idma(wu[:, hh * 4:(hh + 1) * 4, :].rearrange("p c f -> p (c f)"), None, wu_d[hh], off, reads=[widxb], writes=[wub])
                        kb.idma(wd[:, hh * 2:(hh + 1) * 2, :].rearrange("p j d -> p (j d)"), None, wd_d[hh], off, reads=[widxb], writes=[wdb])
                    xT, xTb = pxT.get()
                    for h in range(2):
                        xt, xtb = pxt.get()
                        kb.dma("sp", xt, XS[i * 256 + h * 128:i * 256 + (h + 1) * 128, :], writes=[xtb])
                        for half in range(2):
                            ps, psb = kb.ps()
                            for j in range(4):
                                c = half * 4 + j
                                kb.op("pe", lambda: nc.tensor.transpose(out=ps[:, j * 128:(j + 1) * 128], in_=xt[:, c * 128:(c + 1) * 128], identity=self.ident),
                                      reads=[xtb, self.identb], writes=[psb], signal=(j == 3))
                            src = ps.rearrange("p (c t) -> p c t", c=4)
                            dst = xT[:, half * 4:(half + 1) * 4, h * 128:(h + 1) * 128]
                            if half == 0:
                                kb.op("act", lambda: nc.scalar.copy(out=dst, in_=src), reads=[psb], writes=[xTb])
                            else:
                                kb.op("dve", lambda: V.tensor_copy(out=dst, in_=src), reads=[psb], writes=[xTb])
                    a, ab = pa.get()
                    for j in range(4):
                        pg, pgb = kb.ps()
                        pu, pub = kb.ps()
                        for c in range(KC):
                            kb.op("pe", lambda: nc.tensor.matmul(pg[:, 0:256], wg[:, c, j * 128:(j + 1) * 128], xT[:, c, :], start=(c == 0), stop=(c == KC - 1)),
                                  reads=[wgb, xTb], writes=[pgb], signal=(c == KC - 1))
                        for c in range(KC):
                            kb.op("pe", lambda: nc.tensor.matmul(pu[:, 0:256], wu[:, c, j * 128:(j + 1) * 128], xT[:, c, :], start=(c == 0), stop=(c == KC - 1)),
                                  reads=[wub, xTb], writes=[pub], signal=(c == KC - 1))
                        sl, slb = psl.get()
                        kb.op("act", lambda: nc.scalar.activation(out=sl, in_=pg[:, 0:256], func=AF.Silu), reads=[pgb], writes=[slb])
                        kb.op("dve", lambda: V.tensor_tensor(out=a[:, j, :], in0=sl, in1=pu[:, 0:256], op=ALU.mult), reads=[slb, pub], writes=[ab])
                    for th in range(2):
                        ys, ysb = pys.get()
                        for dh in range(2):
                            po, pob = kb.ps()
                            for j in range(4):
                                kb.op("pe", lambda: nc.tensor.matmul(po, a[:, j, th * 128:(th + 1) * 128], wd[:, j, dh * 512:(dh + 1) * 512], start=(j == 0), stop=(j == 3)),
                                      reads=[ab, wdb], writes=[pob], signal=(j == 3))
                            if dh == 0:
                                kb.op("act", lambda: nc.scalar.copy(out=ys[:, 0:512], in_=po), reads=[pob], writes=[ysb])
                            else:
                                kb.op("dve", lambda: V.tensor_copy(out=ys[:, 512:1024], in_=po), reads=[pob], writes=[ysb])
                        kb.dma("act", YS[i * 256 + th * 128:i * 256 + (th + 1) * 128, :], ys, reads=[ysb])
                kb.barrier()
            if getattr(self, "moe_stop", 9) < 3:
                return
            with ExitStack() as es3:
                pya = Pool(kb, es3, 2, [128, D], F32, "cya")
                pyb = Pool(kb, es3, 2, [128, D], F32, "cyb")
                pot = Pool(kb, es3, 2, [128, KC, 128], F32, "cot")
                for si in range(NTK):
                    t0 = t_lo + si * 128
                    ya, yab = pya.get()
                    yb, ybb = pyb.get()
                    kb.idma(ya, None, YS, bass.IndirectOffsetOnAxis(ap=idxa[:, si, 0:1], axis=0), reads=[selb], writes=[yab])
                    kb.idma(yb, None, YS, bass.IndirectOffsetOnAxis(ap=idxa[:, si, 1:2], axis=0), reads=[selb], writes=[ybb])
                    kb.op("act", lambda: nc.scalar.activation(out=ya, in_=ya, func=AF.Copy, scale=gsel[:, si, 0:1]), reads=[selb], writes=[yab])
                    kb.op("dve", lambda: V.scalar_tensor_tensor(out=ya, in0=yb, scalar=gsel[:, si, 1:2], in1=ya, op0=ALU.mult, op1=ALU.add),
                          reads=[ybb, selb], writes=[yab])
                    o, ob = pot.get()
                    for half in range(2):
                        ps, psb = kb.ps()
                        for j in range(4):
                            c = half * 4 + j
                            kb.op("pe", lambda: nc.tensor.transpose(out=ps[:, j * 128:(j + 1) * 128], in_=ya[:, c * 128:(c + 1) * 128], identity=self.ident),
                                  reads=[yab, self.identb], writes=[psb], signal=(j == 3))
                        kb.op("act" if half == 0 else "dve",
                              (lambda: nc.scalar.copy(out=o[:, half * 4:half * 4 + 4, :], in_=ps.rearrange("p (c t) -> p c t", c=4))) if half == 0 else
                              (lambda: V.tensor_copy(out=o[:, half * 4:half * 4 + 4, :], in_=ps.rearrange("p (c t) -> p c t", c=4))),
                              reads=[psb], writes=[ob])
                    kb.dma("sp", YTv[:, :, t0:t0 + 128], o, reads=[ob])
                kb.barrier()

    def zero_scratch(self, X, rows):
        kb, nc = self.kb, self.nc
        with ExitStack() as es:
            z = kb.tile(es, [128, D], F32, "zeros")
            zb = Buf()
            kb.op("dve", lambda: nc.vector.memset(z, 0.0), writes=[zb])
            for r0 in range(0, rows, 128):
                kb.dma("sp" if (r0 // 128) % 2 == 0 else "act", X[r0:r0 + 128, :], z, reads=[zb])
            kb.barrier()

    def linear_stage(self, XT, W, YT, kp, tiles, mb=512, evac=None, mod=None, extra=None):
        kb, nc = self.kb, self.nc
        K_, M = W.shape
        nk = K_ // kp
        XTv = XT.rearrange("(c p) t -> p c t", p=kp)
        Wv = W.rearrange("(c p) m -> p c m", p=kp)
        YTv = YT.rearrange("(c p) t -> p c t", p=128)
        mb = min(mb, M)
        with ExitStack() as es:
            if mod is not None:
                U, Ub = self.load_mod(es, XT, mod[0], mod[1], tiles)
            pw = Pool(kb, es, 1 if M <= mb else 2, [kp, nk, mb], F32R, "linw")
            if mod is None:
                px = Pool(kb, es, 2, [kp, nk, 512], F32R, "linx")
            po = Pool(kb, es, 2, [128, mb // 128, 512], F32, "lino")
            for m0 in range(0, M, mb):
                w, wb = pw.get()
                kb.dma("pool", w, Wv[:, :, m0:m0 + mb], writes=[wb])
                for (t0, n) in tiles:
                    col = 1 if t0 < CTX else 0
                    if mod is None:
                        x, xb = px.get()
                        kb.dma("pool", x[:, :, :n], XTv[:, :, t0:t0 + n], writes=[xb])
                    else:
                        x, xb = U[:, :, t0:t0 + n], Ub[t0]
                    if extra is not None and m0 == 0:
                        extra(x, xb, t0, n)
                    o, ob = po.get()
                    for mc in range(mb // 128):
                        ps, psb = kb.ps()
                        for c in range(nk):
                            kb.op("pe", lambda: nc.tensor.matmul(ps[:, :n], w[:, c, mc * 128:(mc + 1) * 128], x[:, c, :n], start=(c == 0), stop=(c == nk - 1)),
                                  reads=[wb, xb], writes=[psb], signal=(c == nk - 1))
                        if evac is None:
                            kb.op("act", lambda: nc.scalar.copy(out=o[:, mc, :n], in_=ps[:, :n]), reads=[psb], writes=[ob])
                        else:
                            evac(ps[:, :n], o[:, mc, :n], m0 // 128 + mc, col, [psb], [ob])
                    kb.dma("sp", YTv[:, m0 // 128:(m0 + mb) // 128, t0:t0 + n], o[:, :, :n], reads=[ob])
        kb.barrier()

    def load_mod(self, es, HT, sh_slot, sc_slot, tiles):
        kb, nc = self.kb, self.nc
        HTv = HT.rearrange("(c p) t -> p c t", p=128)
        U = kb.tile(es, [128, KC, T], F32R, "U")
        Ub = {}
        with ExitStack() as es2:
            ph = Pool(kb, es2, 2, [128, KC, 512], F32, "uh")
            for (t0, n) in tiles:
                col = 1 if t0 < CTX else 0
                h, hb = ph.get()
                kb.dma("sp", h[:, :, :n], HTv[:, :, t0:t0 + n], writes=[hb])
                Ub[t0] = Buf()
                for c in range(KC):
                    kb.op("act", lambda: nc.scalar.activation(out=U[:, c, t0:t0 + n], in_=h[:, c, :n], func=AF.Identity,
                                                              scale=self.modT[:, sc_slot * 8 + c, col:col + 1], bias=self.modT[:, sh_slot * 8 + c, col:col + 1]),
                          reads=[hb, self.modb], writes=[Ub[t0]])
            kb.barrier()
        return U, Ub

    def rglru_stage(self, HT, MT, jl):
        kb, nc = self.kb, self.nc
        w_in = self.ins["rg_w_in"]
        NB = 16
        P = RG_BS
        with ExitStack() as es:
            U, Ub = self.load_mod(es, HT, 0, 1, TT_ALL)
            Uall = list(Ub.values())
            rgv = kb.tile(es, [P, NB, 11], F32, "rgv")
            rgvb = Buf()
            kb.dma("sp", rgv, self.ins["rgv"][jl], writes=[rgvb])
            pgw = Pool(kb, es, 2, [P, 2, 2, P], F32R, "rggw")
            coef = kb.tile(es, [P, 2, NB], F32, "coef")
            coef2 = kb.tile(es, [P, 2, NB], F32, "coef2")
            cb = Buf()
            for z in range(2):
                kb.op("act", lambda: nc.scalar.activation(out=coef[:, z, :], in_=rgv[:, :, 9 + z], func=AF.Exp, scale=-1.0), reads=[rgvb], writes=[cb])
            kb.op("act", lambda: nc.scalar.activation(out=coef, in_=coef, func=AF.Ln, bias=self.ones[:P, 0:1]), reads=[self.onesb], writes=[cb])
            kb.op("dve", lambda: nc.vector.tensor_scalar(out=coef2, in0=coef, scalar1=-16.0, scalar2=None, op0=ALU.mult), writes=[cb])
            kb.op("dve", lambda: nc.vector.tensor_scalar(out=coef, in0=coef, scalar1=-8.0, scalar2=None, op0=ALU.mult), writes=[cb])
            pwi = Pool(kb, es, 2, [128, KC, 2, P], F32R, "rgwin")
            prec = Pool(kb, es, 1, [P, T], F32, "rec")
            pgate = Pool(kb, es, 2, [P, T], F32, "gate")
            mk = lambda nm: (kb.tile(es, [P, T], F32, nm), Buf())
            xc, xcb = mk("xc")
            xcr = kb.tile(es, [P, T], F32R, "xcr")
            xcrb = Buf()
            gts1 = [mk("r"), mk("i")]
            gts = [gts1, gts1]
            a_, ab_ = mk("a")
            w_, wb_ = mk("w")
            hs, hsb = mk("hs")
            w_in_v = w_in[jl].rearrange("(c p) (g n f) -> p c g n f", p=128, g=2, n=NB)
            segs = [(0, CTX), (CTX, T)]
            for n in range(NB):
                wi, wib = pwi.get()
                for g in range(2):
                    kb.dma("pool", wi[:, :, g, :], w_in_v[:, :, g, n, :], writes=[wib])
                gw4, gwb = pgw.get()
                kb.dma("pool", gw4[:, 0], self.ins["rg_gate_a_w"][jl, :, n].rearrange("z k j -> k z j"), writes=[gwb])
                kb.dma("pool", gw4[:, 1], self.ins["rg_gate_x_w"][jl, :, n].rearrange("z k j -> k z j"), writes=[gwb])
                rec, recb = prec.get()
                gate, gateb = pgate.get()
                for (t0, nn) in TT_ALL:
                    ps, psb = kb.ps()
                    for c in range(KC):
                        kb.op("pe", lambda: nc.tensor.matmul(ps[:P, :nn], wi[:, c, 1, :], U[:, c, t0:t0 + nn], start=(c == 0), stop=(c == KC - 1)),
                              reads=[wib, Ub[t0]], writes=[psb], signal=(c == KC - 1))
                    kb.op("act", lambda: nc.scalar.copy(out=rec[:, t0:t0 + nn], in_=ps[:P, :nn]), reads=[psb], writes=[recb])
                for (t0, nn) in TT_ALL:
                    ps, psb = kb.ps()
                    for c in range(KC):
                        kb.op("pe", lambda: nc.tensor.matmul(ps[:P, :nn], wi[:, c, 0, :], U[:, c, t0:t0 + nn], start=(c == 0), stop=(c == KC - 1)),
                              reads=[wib, Ub[t0]], writes=[psb], signal=(c == KC - 1))
                    kb.op("act", lambda: nc.scalar.activation(out=gate[:, t0:t0 + nn], in_=ps[:P, :nn], func=AF.Gelu_apprx_tanh), reads=[psb], writes=[gateb])
                kb.op("dve", lambda: nc.vector.tensor_scalar(out=xc, in0=rec, scalar1=rgv[:, n, 2:3], scalar2=rgv[:, n, 4:5], op0=ALU.mult, op1=ALU.add),
                      reads=[recb, rgvb], writes=[xcb])
                for k in (0, 1, 3):
                    d = k - 2
                    for (s0, s1) in segs:
                        ta, tb = max(s0, s0 - d), min(s1, s1 - d)
                        kb.op("dve", lambda: nc.vector.scalar_tensor_tensor(out=xc[:, ta:tb], in0=rec[:, ta + d:tb + d], scalar=rgv[:, n, k:k + 1],
                                                                            in1=xc[:, ta:tb], op0=ALU.mult, op1=ALU.add),
                              reads=[recb, rgvb], writes=[xcb])
                kb.op("act", lambda: nc.scalar.copy(out=xcr, in_=xc), reads=[xcb], writes=[xcrb])
                for z in range(2):
                    for gi, bcol in enumerate((5 + z, 7 + z)):
                        gt, gtb = gts[z][gi]
                        for (t0, nn) in TT_ALL:
                            ps, psb = kb.ps()
                            kb.op("pe", lambda: nc.tensor.matmul(ps[:P, :nn], gw4[:, gi, z, :], xcr[:, t0:t0 + nn], start=True, stop=True),
                                  reads=[gwb, xcrb], writes=[psb])
                            kb.op("act", lambda: nc.scalar.activation(out=gt[:, t0:t0 + nn], in_=ps[:P, :nn], func=AF.Sigmoid, bias=rgv[:, n, bcol:bcol + 1]),
                                  reads=[psb, rgvb], writes=[gtb])
                    (r, rb), (ig, igb) = gts[z]
                    kb.op("act", lambda: nc.scalar.activation(out=a_, in_=r, func=AF.Exp, scale=coef[:, z, n:n + 1]), reads=[rb, cb], writes=[ab_])
                    kb.op("act", lambda: nc.scalar.activation(out=w_, in_=r, func=AF.Exp, scale=coef2[:, z, n:n + 1]), reads=[rb, cb], writes=[wb_])
                    kb.op("act", lambda: nc.scalar.activation(out=w_, in_=w_, func=AF.Ln, scale=-1.0, bias=self.ones[:P, 0:1]), reads=[self.onesb], writes=[wb_])
                    kb.op("act", lambda: nc.scalar.activation(out=w_, in_=w_, func=AF.Exp, scale=0.5), writes=[wb_])
                    kb.op("dve", lambda: nc.vector.tensor_tensor(out=ig, in0=ig, in1=xc, op=ALU.mult), reads=[xcb], writes=[igb])
                    kb.op("pool", lambda: nc.gpsimd.tensor_tensor(out=ig, in0=ig, in1=w_, op=ALU.mult), reads=[wb_], writes=[igb])
                    if z == 0:
                        kb.op("dve", lambda: nc.vector.tensor_tensor_scan(out=hs, data0=a_, data1=ig, initial=0.0, op0=ALU.mult, op1=ALU.add),
                              reads=[ab_, igb], writes=[hsb])
                    else:
                        kb.op("dve", lambda: nc.vector.tensor_tensor_scan(out=r[:, 0:CTX][:, ::-1], data0=a_[:, 0:CTX][:, ::-1], data1=ig[:, 0:CTX][:, ::-1],
                                                                          initial=0.0, op0=ALU.mult, op1=ALU.add),
                              reads=[ab_, igb], writes=[rb])
                        kb.op("dve", lambda: nc.vector.tensor_tensor_scan(out=r[:, CTX:T][:, ::-1], data0=a_[:, CTX:T][:, ::-1], data1=ig[:, CTX:T][:, ::-1],
                                                                          initial=r[:, 0:1], op0=ALU.mult, op1=ALU.add),
                              reads=[ab_, igb], writes=[rb])
                        kb.op("pool", lambda: nc.gpsimd.tensor_tensor(out=hs, in0=hs, in1=r, op=ALU.add), reads=[rb], writes=[hsb])
                kb.op("dve", lambda: nc.vector.tensor_tensor(out=hs, in0=hs, in1=gate, op=ALU.mult), reads=[gateb], writes=[hsb])
                kb.dma("sp", MT[n * P:(n + 1) * P, :], hs, reads=[hsb])
        kb.barrier()

    def linear_tok_stage(self, XT, W, Y, evac=None):
        kb, nc = self.kb, self.nc
        K_, M = W.shape
        nk = K_ // 128
        XTv = XT.rearrange("(c p) t -> p c t", p=128)
        Wv = W.rearrange("(c p) m -> p c m", p=128)
        mb = 512
        with ExitStack() as es:
            pw = Pool(kb, es, 2, [128, nk, mb], F32R, "ltw")
            px = Pool(kb, es, 3, [128, nk, 128], F32R, "ltx")
            po = Pool(kb, es, 3, [128, mb], F32, "lto")
            for m0 in range(0, M, mb):
                w, wb = pw.get()
                kb.dma("pool", w, Wv[:, :, m0:m0 + mb], writes=[wb])
                for ti in range(T // 128):
                    t0 = ti * 128
                    x, xb = px.get()
                    kb.dma("pool", x, XTv[:, :, t0:t0 + 128], writes=[xb])
                    ps, psb = kb.ps()
                    for c in range(nk):
                        kb.op("pe", lambda: nc.tensor.matmul(ps, x[:, c, :], w[:, c, :], start=(c == 0), stop=(c == nk - 1)),
                              reads=[wb, xb], writes=[psb], signal=(c == nk - 1))
                    o, ob = po.get()
                    if evac is None:
                        kb.op("act", lambda: nc.scalar.copy(out=o, in_=ps), reads=[psb], writes=[ob])
                    else:
                        evac(ps, o, [psb], [ob])
                    kb.dma("sp", Y[t0:t0 + 128, m0:m0 + mb], o, reads=[ob])
        kb.barrier()

    def dwconv(self, out, outb, x, xb, vec, vecb, ntap, left, bias_col, P=128, eng="dve"):
        kb, nc = self.kb, self.nc
        kb.op("dve", lambda: nc.vector.tensor_scalar(out=out, in0=x, scalar1=vec[:, left:left + 1], scalar2=vec[:, bias_col:bias_col + 1],
                                                     op0=ALU.mult, op1=ALU.add), reads=[xb, vecb], writes=[outb])
        for k in range(ntap):
            d = k - left
            if d == 0:
                continue
            for (s0, s1) in ((0, CTX), (CTX, T)):
                ta, tb = max(s0, s0 - d), min(s1, s1 - d)
                kb.op("dve", lambda: nc.vector.scalar_tensor_tensor(out=out[:, ta:tb], in0=x[:, ta + d:tb + d], scalar=vec[:, k:k + 1],
                                                                    in1=out[:, ta:tb], op0=ALU.mult, op1=ALU.add),
                      reads=[xb, vecb], writes=[outb])

    def ml_conv_stage(self, XM, XC):
        kb, nc = self.kb, self.nc
        with ExitStack() as es:
            vec = kb.tile(es, [128, 16, 7], F32, "mlvec")
            vecb = Buf()
            kb.dma("sp", vec, self.ins["mlvec"], writes=[vecb])
            pi = Pool(kb, es, 2, [128, T], F32, "mci")
            po = Pool(kb, es, 2, [128, T], F32, "mco")
            for c in range(16):
                x, xb = pi.get()
                kb.dma("sp", x, XM[c * 128:(c + 1) * 128, :], writes=[xb])
                o, ob = po.get()
                self.dwconv(o, ob, x, xb, vec[:, c, :], vecb, 4, 2, 4)
                kb.op("act", lambda: nc.scalar.activation(out=o, in_=o, func=AF.Silu), writes=[ob])
                kb.dma("act", XC[c * 128:(c + 1) * 128, :], o, reads=[ob])
        kb.barrier()

    def ml_core_stage(self, GT, QT, KT, KTOK, VTOK, HSF, HNT):
        kb, nc = self.kb, self.nc
        NCH = T // 128
        V = nc.vector
        QTv = QT.rearrange("(h p) t -> p h t", p=128)
        KTv = KT.rearrange("(h p) t -> p h t", p=128)
        HNTv = HNT.rearrange("(c p) t -> p c t", p=128)
        for z in range(2):
            order = list(range(NCH)) if z == 0 else [1, 0] + list(range(NCH - 1, 1, -1))
            li = 127 if z == 0 else 0
            with ExitStack() as es:
                colz = kb.tile(es, [128, NCH, 32], F32, "colz")
                colb = Buf()
                spb = kb.tile(es, [128, 8, NCH], F32, "spb")
                slb = kb.tile(es, [128, 8, NCH], F32, "slb")
                spbb = Buf()
                mask = kb.tile(es, [128, 128], F32, "mask")
                maskb = Buf()
                kb.dma("sp", mask, self.ins["maskF" if z == 0 else "maskB"], writes=[maskb])
                with ExitStack() as es2:
                    rt = lambda nm: kb.tile(es2, [8, T], F32, nm)
                    ig, fg, G, A, cm, Mx, E1, F_, inter, edm, one8 = [rt(nm) for nm in ("ig", "fg", "G", "A", "cm", "Mx", "E1", "F", "inter", "edm", "one8")]
                    rb = Buf()
                    ch = lambda nm: kb.tile(es2, [8, NCH], F32, nm)
                    Gend, Gprev, maxA, btot, mloc, mq, mprev, Pq, Pn, sp_, sl_ = [ch(nm) for nm in ("Gend", "Gprev", "maxA", "btot", "mloc", "mq", "mprev", "Pq", "Pn", "sp", "sl")]
                    spd = kb.tile(es2, [8, 8, NCH], F32, "spd")
                    sld = kb.tile(es2, [8, 8, NCH], F32, "sld")
                    bif = kb.tile(es2, [8, 4], F32, "bif")
                    kb.dma("sp", ig, GT[z, 0], writes=[rb])
                    kb.dma("sp", fg, GT[z, 1], writes=[rb])
                    kb.op("dve", lambda: V.memset(one8, 1.0), writes=[rb])
                    kb.op("act", lambda: nc.scalar.activation(out=fg, in_=fg, func=AF.Exp, scale=-1.0), writes=[rb])
                    kb.op("act", lambda: nc.scalar.activation(out=fg, in_=fg, func=AF.Ln, bias=self.ones[:8, 0:1]), reads=[self.onesb], writes=[rb])
                    if z == 0:
                        kb.op("dve", lambda: V.tensor_tensor_scan(out=G, data0=one8, data1=fg, initial=0.0, op0=ALU.mult, op1=ALU.subtract), writes=[rb])
                    else:
                        kb.op("dve", lambda: V.tensor_tensor_scan(out=G[:, 0:CTX][:, ::-1], data0=one8[:, 0:CTX], data1=fg[:, 0:CTX][:, ::-1], initial=0.0,
                                                                  op0=ALU.mult, op1=ALU.subtract), writes=[rb])
                        kb.op("dve", lambda: V.tensor_tensor_scan(out=G[:, CTX:T][:, ::-1], data0=one8[:, CTX:T], data1=fg[:, CTX:T][:, ::-1], initial=G[:, 0:1],
                                                                  op0=ALU.mult, op1=ALU.subtract), writes=[rb])
                    kb.op("dve", lambda: V.tensor_tensor(out=A, in0=ig, in1=G, op=ALU.subtract), writes=[rb])
                    for c in range(NCH):
                        sl = slice(c * 128, (c + 1) * 128)
                        rv = (lambda a: a[:, sl][:, ::-1]) if z == 1 else (lambda a: a[:, sl])
                        kb.op("dve", lambda: V.tensor_tensor_scan(out=rv(cm), data0=one8[:, sl], data1=rv(A), initial=-1e30, op0=ALU.mult, op1=ALU.max), writes=[rb])
                    c3 = lambda a: a.rearrange("p (c i) -> p c i", i=128)
                    maxA_nat = c3(cm)[:, :, li]
                    Gend_nat = c3(G)[:, :, li]

                    def to_proc(dst, src):
                        if z == 0:
                            kb.op("dve", lambda: V.tensor_copy(out=dst, in_=src), writes=[rb])
                        else:
                            kb.op("dve", lambda: V.tensor_copy(out=dst[:, 0:1], in_=src[:, 1:2]), writes=[rb])
                            kb.op("dve", lambda: V.tensor_copy(out=dst[:, 1:2], in_=src[:, 0:1]), writes=[rb])
                            kb.op("dve", lambda: V.tensor_copy(out=dst[:, 2:NCH], in_=src[:, 2:NCH][:, ::-1]), writes=[rb])

                    to_proc(Gend, Gend_nat)
                    to_proc(maxA, maxA_nat)
                    kb.op("dve", lambda: V.memset(Gprev[:, 0:1], 0.0), writes=[rb])
                    kb.op("dve", lambda: V.tensor_copy(out=Gprev[:, 1:NCH], in_=Gend[:, 0:NCH - 1]), writes=[rb])
                    kb.op("dve", lambda: V.tensor_tensor(out=btot, in0=Gend, in1=Gprev, op=ALU.subtract), writes=[rb])
                    kb.op("dve", lambda: V.tensor_tensor(out=mloc, in0=Gend, in1=maxA, op=ALU.add), writes=[rb])
                    kb.op("dve", lambda: V.tensor_tensor_scan(out=mq, data0=btot, data1=mloc, initial=0.0, op0=ALU.add, op1=ALU.max), writes=[rb])
                    kb.op("dve", lambda: V.memset(mprev[:, 0:1], 0.0), writes=[rb])
                    kb.op("dve", lambda: V.tensor_copy(out=mprev[:, 1:NCH], in_=mq[:, 0:NCH - 1]), writes=[rb])
                    kb.op("dve", lambda: V.tensor_tensor(out=Pq, in0=mprev, in1=Gprev, op=ALU.subtract), writes=[rb])
                    kb.op("dve", lambda: V.tensor_tensor(out=sp_, in0=btot, in1=mprev, op=ALU.add), writes=[rb])
                    kb.op("dve", lambda: V.tensor_tensor(out=sp_, in0=sp_, in1=mq, op=ALU.subtract), writes=[rb])
                    kb.op("act", lambda: nc.scalar.activation(out=sp_, in_=sp_, func=AF.Exp), writes=[rb])
                    kb.op("dve", lambda: V.tensor_tensor(out=sl_, in0=mloc, in1=mq, op=ALU.subtract), writes=[rb])
                    kb.op("act", lambda: nc.scalar.activation(out=sl_, in_=sl_, func=AF.Exp), writes=[rb])
                    to_proc(Pn, Pq)
                    bcc = lambda a: a.unsqueeze(2).to_broadcast([8, NCH, 128])
                    kb.op("dve", lambda: V.tensor_tensor(out=c3(Mx), in0=c3(cm), in1=bcc(Pn), op=ALU.max), writes=[rb])
                    kb.op("dve", lambda: V.tensor_tensor(out=c3(E1), in0=c3(A), in1=bcc(maxA_nat), op=ALU.subtract), writes=[rb])
                    kb.op("act", lambda: nc.scalar.activation(out=E1, in_=E1, func=AF.Exp), writes=[rb])
                    kb.op("dve", lambda: V.tensor_tensor(out=c3(F_), in0=bcc(maxA_nat), in1=c3(Mx), op=ALU.subtract), writes=[rb])
                    kb.op("act", lambda: nc.scalar.activation(out=F_, in_=F_, func=AF.Exp), writes=[rb])
                    kb.op("dve", lambda: V.tensor_tensor(out=c3(inter), in0=bcc(Pn), in1=c3(Mx), op=ALU.subtract), writes=[rb])
                    kb.op("act", lambda: nc.scalar.activation(out=inter, in_=inter, func=AF.Exp), writes=[rb])
                    kb.op("dve", lambda: V.tensor_tensor(out=edm, in0=G, in1=Mx, op=ALU.add), writes=[rb])
                    kb.op("act", lambda: nc.scalar.activation(out=edm, in_=edm, func=AF.Exp, scale=-1.0), writes=[rb])
                    for c in range(NCH):
                        ps, psb = kb.ps()
                        for ai, arr in enumerate((E1, F_, inter, edm)):
                            kb.op("pe", lambda: nc.tensor.matmul(ps[:, ai * 8:(ai + 1) * 8], arr[:, c * 128:(c + 1) * 128], self.ident[:8, :8], start=True, stop=True),
                                  reads=[rb, self.identb], writes=[psb], signal=(ai == 3))
                        kb.op("act", lambda: nc.scalar.copy(out=colz[:, c, :], in_=ps[:, 0:32]), reads=[psb], writes=[colb])
                    idb = self.ident[:8, :8].unsqueeze(2).to_broadcast([8, 8, NCH])
                    for (src, dd_, dst) in ((sp_, spd, spb), (sl_, sld, slb)):
                        kb.op("dve", lambda: V.tensor_tensor(out=dd_, in0=idb, in1=src.unsqueeze(1).to_broadcast([8, 8, NCH]), op=ALU.mult),
                              reads=[self.identb], writes=[rb])
                        ps, psb = kb.ps()
                        kb.op("pe", lambda: nc.tensor.matmul(ps[:, 0:8 * NCH], self.ones[:8, :], dd_.rearrange("p h q -> p (h q)"), start=True, stop=True),
                              reads=[rb, self.onesb], writes=[psb])
                        kb.op("act", lambda: nc.scalar.copy(out=dst.rearrange("p h q -> p (h q)"), in_=ps[:, 0:8 * NCH]), reads=[psb], writes=[spbb])
                    kb.barrier()
                pkt = Pool(kb, es, 2, [128, 8, 128], F32R, "kt")
                pqt = Pool(kb, es, 2, [128, 8, 128], F32R, "qt")
                pktok = Pool(kb, es, 2, [128, 1024], F32R, "ktok")
                pvx = Pool(kb, es, 2, [128, 8, 258], F32R, "vext")
                pvw = Pool(kb, es, 2, [128, 8, 258], F32R, "vw")
                for t_, b_ in zip(pvx.t, pvx.b):
                    kb.op("dve", lambda: V.tensor_copy(out=t_[:, :, 256:257], in_=self.ones[:, 0:8].unsqueeze(2)), reads=[self.onesb], writes=[b_])
                    kb.op("dve", lambda: V.tensor_scalar(out=t_[:, :, 257:258], in0=self.ones[:, 0:8].unsqueeze(2), scalar1=0.0, scalar2=None, op0=ALU.mult),
                          reads=[self.onesb], writes=[b_])
                cn = kb.tile(es, [128, 8, 258], F32, "cn")
                cnr = kb.tile(es, [128, 8, 258], F32R, "cnr")
                cnb = [Buf() for _ in range(8)]
                cnrb = [Buf() for _ in range(8)]
                pst = Pool(kb, es, 3, [128, 128], F32R, "sT")
                pt1 = Pool(kb, es, 3, [128, 258], F32, "t1")
                ptc = Pool(kb, es, 3, [128, 258], F32, "tc")
                pdd = Pool(kb, es, 4, [128, 2], F32, "dd")
                phc = Pool(kb, es, 2, [128, 8, 256], F32, "hch")
                if z == 1:
                    phf = Pool(kb, es, 2, [128, 8, 256], F32, "hf")
                    pstt = Pool(kb, es, 2, [128, 8, 6], F32, "bst")
                    pmv = Pool(kb, es, 2, [128, 8, 2], F32, "bmv")
                    prs = Pool(kb, es, 2, [128, 8], F32, "brs")
                    phnt = Pool(kb, es, 2, [128, 16, 128], F32, "hnt")
                    epst = kb.tile(es, [128, 1], F32, "eps")
                    epsb = Buf()
                    kb.op("dve", lambda: V.memset(epst, LN_EPS), writes=[epsb])
                for q, c in enumerate(order):
                    sl = slice(c * 128, (c + 1) * 128)
                    kt, ktb = pkt.get()
                    qt, qtb = pqt.get()
                    ktok, ktokb = pktok.get()
                    vx, vxb = pvx.get()
                    vw, vwb = pvw.get()
                    kb.dma("pool", kt, KTv[:, :, sl], writes=[ktb])
                    kb.dma("pool", qt, QTv[:, :, sl], writes=[qtb])
                    kb.dma("pool", ktok, KTOK[sl, :], writes=[ktokb])
                    kb.dma("pool", vx[:, :, 0:256], VTOK[sl, :].rearrange("s (h v) -> s h v", h=8), writes=[vxb])
                    kb.op("dve", lambda: V.tensor_tensor(out=vw, in0=vx, in1=colz[:, c, 0:8].unsqueeze(2).to_broadcast([128, 8, 258]), op=ALU.mult),
                          reads=[vxb, colb], writes=[vwb])
                    hch, hcb = phc.get()
                    for h in range(8):
                        E1c = colz[:, c, h:h + 1]
                        Fc = colz[:, c, 8 + h:9 + h]
                        inc = colz[:, c, 16 + h:17 + h]
                        edc = colz[:, c, 24 + h:25 + h]
                        psS, psSb = kb.ps()
                        kb.op("pe", lambda: nc.tensor.matmul(psS[:, 0:128], kt[:, h, :], qt[:, h, :], start=True, stop=True), reads=[ktb, qtb], writes=[psSb])
                        sT, sTb = pst.get()
                        kb.op("dve", lambda: V.scalar_tensor_tensor(out=sT, in0=psS[:, 0:128], scalar=E1c, in1=mask, op0=ALU.mult, op1=ALU.mult),
                              reads=[psSb, colb, maskb], writes=[sTb])
                        psN, psNb = kb.ps()
                        kb.op("pe", lambda: nc.tensor.matmul(psN[:, 0:258], sT, vx[:, h, :], start=True, stop=True), reads=[sTb, vxb], writes=[psNb])
                        t1, t1b = pt1.get()
                        if q > 0:
                            psI, psIb = kb.ps()
                            kb.op("pe", lambda: nc.tensor.matmul(psI[:, 0:258], qt[:, h, :], cnr[:, h, :], start=True, stop=True), reads=[qtb, cnrb[h]], writes=[psIb])
                            kb.op("act", lambda: nc.scalar.activation(out=t1, in_=psI[:, 0:258], func=AF.Copy, scale=inc), reads=[psIb, colb], writes=[t1b])
                            kb.op("dve", lambda: V.scalar_tensor_tensor(out=t1, in0=psN[:, 0:258], scalar=Fc, in1=t1, op0=ALU.mult, op1=ALU.add),
                                  reads=[psNb, colb], writes=[t1b])
                        else:
                            kb.op("act", lambda: nc.scalar.activation(out=t1, in_=psN[:, 0:258], func=AF.Copy, scale=Fc), reads=[psNb, colb], writes=[t1b])
                        dd, ddb = pdd.get()
                        kb.op("act", lambda: nc.scalar.activation(out=dd[:, 0:1], in_=t1[:, 256:257], func=AF.Abs), reads=[t1b], writes=[ddb])
                        kb.op("dve", lambda: V.tensor_scalar(out=dd[:, 0:1], in0=dd[:, 0:1], scalar1=edc, scalar2=None, op0=ALU.max),
                              reads=[colb], writes=[ddb])
                        kb.op("dve", lambda: V.reciprocal(out=dd[:, 1:2], in_=dd[:, 0:1]), writes=[ddb])
                        kb.op("pool", lambda: nc.gpsimd.tensor_scalar(out=hch[:, h, :], in0=t1[:, 0:256], scalar1=dd[:, 1:2], scalar2=None, op0=ALU.mult),
                              reads=[t1b, ddb], writes=[hcb])
                        if q < NCH - 1:
                            psC, psCb = kb.ps()
                            kb.op("pe", lambda: nc.tensor.matmul(psC[:, 0:258], ktok[:, h * 128:(h + 1) * 128], vw[:, h, :], start=True, stop=True),
                                  reads=[ktokb, vwb], writes=[psCb])
                            if q == 0:
                                kb.op("act", lambda: nc.scalar.activation(out=cn[:, h, :], in_=psC[:, 0:258], func=AF.Copy, scale=slb[:, h, q:q + 1]),
                                      reads=[psCb, spbb], writes=[cnb[h]])
                            else:
                                tc_, tcb = ptc.get()
                                kb.op("act", lambda: nc.scalar.activation(out=tc_, in_=psC[:, 0:258], func=AF.Copy, scale=slb[:, h, q:q + 1]),
                                      reads=[psCb, spbb], writes=[tcb])
                                kb.op("dve", lambda: V.scalar_tensor_tensor(out=cn[:, h, :], in0=cn[:, h, :], scalar=spb[:, h, q:q + 1], in1=tc_,
                                                                            op0=ALU.mult, op1=ALU.add), reads=[tcb, spbb], writes=[cnb[h]])
                            kb.op("pool", lambda: nc.gpsimd.tensor_copy(out=cnr[:, h, :], in_=cn[:, h, :]), reads=[cnb[h]], writes=[cnrb[h]])
                    if z == 0:
                        kb.dma("sp", HSF[sl, :], hch.rearrange("p h v -> p (h v)"), reads=[hcb])
                    else:
                        hf, hfb = phf.get()
                        kb.dma("sp", hf.rearrange("p h v -> p (h v)"), HSF[sl, :], writes=[hfb])
                        kb.op("pool", lambda: nc.gpsimd.tensor_tensor(out=hch, in0=hch, in1=hf, op=ALU.add), reads=[hfb], writes=[hcb])
                        st, stb = pstt.get()
                        mv, mvb = pmv.get()
                        rs, rsb = prs.get()
                        for h in range(8):
                            kb.op("dve", lambda: V.bn_stats(out=st[:, h, :], in_=hch[:, h, :]), reads=[hcb], writes=[stb])
                            kb.op("dve", lambda: V.bn_aggr(out=mv[:, h, :], in_=st[:, h, :]), reads=[stb], writes=[mvb])
                        kb.op("act", lambda: nc.scalar.activation(out=rs, in_=mv[:, :, 1], func=AF.Ln, bias=epst[:, 0:1]), reads=[mvb, epsb], writes=[rsb])
                        kb.op("act", lambda: nc.scalar.activation(out=rs, in_=rs, func=AF.Exp, scale=-0.5), writes=[rsb])
                        for h in range(8):
                            kb.op("dve", lambda: V.tensor_scalar(out=hch[:, h, :], in0=hch[:, h, :], scalar1=mv[:, h, 0:1], scalar2=rs[:, h:h + 1],
                                                                 op0=ALU.subtract, op1=ALU.mult), reads=[mvb, rsb], writes=[hcb])
                        hnt, hntb = phnt.get()
                        h2 = hch.rearrange("p h v -> p (h v)")
                        for g4 in range(4):
                            ps, psb = kb.ps()
                            for j in range(4):
                                fc = g4 * 4 + j
                                kb.op("pe", lambda: nc.tensor.transpose(out=ps[:, j * 128:(j + 1) * 128], in_=h2[:, fc * 128:(fc + 1) * 128], identity=self.ident),
                                      reads=[hcb, self.identb], writes=[psb], signal=(j == 3))
                            kb.op("act", lambda: nc.scalar.copy(out=hnt[:, g4 * 4:(g4 + 1) * 4, :], in_=ps.rearrange("p (c t) -> p c t", c=4)), reads=[psb], writes=[hntb])
                        kb.dma("sp", HNTv[:, :, sl], hnt, reads=[hntb])
            kb.barrier()

    def ml_combine_stage(self, HNT, OT, XC, YPT):
        kb, nc = self.kb, self.nc
        with ExitStack() as es:
            vec = kb.tile(es, [128, 16, 7], F32, "mlvec")
            vecb = Buf()
            kb.dma("sp", vec, self.ins["mlvec"], writes=[vecb])
            p1 = Pool(kb, es, 2, [128, T], F32, "cb1")
            p2 = Pool(kb, es, 2, [128, T], F32, "cb2")
            p3 = Pool(kb, es, 2, [128, T], F32, "cb3")
            for c in range(16):
                rows = slice(c * 128, (c + 1) * 128)
                a, ab = p1.get()
                o, ob = p2.get()
                x, xb = p3.get()
                kb.dma("sp", a, HNT[rows, :], writes=[ab])
                kb.dma("sp", o, OT[rows, :], writes=[ob])
                kb.dma("sp", x, XC[rows, :], writes=[xb])
                kb.op("dve", lambda: nc.vector.scalar_tensor_tensor(out=a, in0=a, scalar=vec[:, c, 5:6], in1=o, op0=ALU.mult, op1=ALU.mult),
                      reads=[ob, vecb], writes=[ab])
                kb.op("dve", lambda: nc.vector.scalar_tensor_tensor(out=a, in0=x, scalar=vec[:, c, 6:7], in1=a, op0=ALU.mult, op1=ALU.add),
                      reads=[xb, vecb], writes=[ab])
                kb.dma("act", YPT[rows, :], a, reads=[ab])
        kb.barrier()

    def mlstm_layer(self, HT, YT, S):
        kb, nc = self.kb, self.nc
        I = self.ins
        XM, XC, QT, KT, KTOK, VTOK, OT, GT, HSF, HNT, YPT = (S[k] for k in ("XM", "XC", "QT", "KT", "KTOK", "VTOK", "OT", "GT", "HSF", "HNT", "YPT"))
        self.linear_stage(HT, I["ml_w_up"][0], XM, 128, TT_ALL, mb=512, mod=(0, 1))
        self.ml_conv_stage(XM, XC)
        self.linear_stage(XC, I["ml_w_q"][0], QT, 128, TT_ALL)
        kscale = 128.0 ** -0.5
        self.linear_stage(XC, I["ml_w_k"][0], KT, 128, TT_ALL,
                          evac=lambda ps, o, mc, col, rd, wr: kb.op("act", lambda: nc.scalar.mul(out=o, in_=ps, mul=kscale), reads=rd, writes=wr))
        self.linear_tok_stage(XC, I["ml_w_k"][0], KTOK,
                              evac=lambda ps, o, rd, wr: kb.op("act", lambda: nc.scalar.mul(out=o, in_=ps, mul=kscale), reads=rd, writes=wr))
        self.linear_tok_stage(XM, I["ml_w_v"][0], VTOK)
        with ExitStack() as esg:
            wif = kb.tile(esg, [128, 16, 2, 16], F32R, "wif")
            wifb = Buf()
            for z in range(2):
                kb.dma("pool", wif[:, :, z, :], I["ml_w_if"][0, z].rearrange("(c p) g -> p c g", p=128), writes=[wifb])
            bif = kb.tile(esg, [8, 4], F32, "bif")
            bifb = Buf()
            kb.dma("sp", bif, I["mlbif"], writes=[bifb])
            pg = Pool(kb, esg, 2, [8, 4, 512], F32, "gout")

            def extra(x, xb, t0, n):
                g, gb = pg.get()
                for z in range(2):
                    for gi in range(2):
                        ps, psb = kb.ps()
                        for c in range(16):
                            kb.op("pe", lambda: nc.tensor.matmul(ps[:8, :n], wif[:, c, z, gi * 8:(gi + 1) * 8], x[:, c, :n], start=(c == 0), stop=(c == 15)),
                                  reads=[wifb, xb], writes=[psb], signal=(c == 15))
                        kb.op("act", lambda: nc.scalar.activation(out=g[:, z * 2 + gi, :n], in_=ps[:8, :n], func=AF.Identity, bias=bif[:, z * 2 + gi:z * 2 + gi + 1]),
                              reads=[psb, bifb], writes=[gb])
                kb.dma("sp", GT.rearrange("z g h t -> h (z g) t")[:, :, t0:t0 + n], g[:, :, :n], reads=[gb])

            self.linear_stage(XM, I["ml_w_o"][0], OT, 128, TT_ALL, extra=extra,
                              evac=lambda ps, o, mc, col, rd, wr: kb.op("act", lambda: nc.scalar.activation(out=o, in_=ps, func=AF.Sigmoid), reads=rd, writes=wr))
        self.ml_core_stage(GT, QT, KT, KTOK, VTOK, HSF, HNT)
        self.ml_combine_stage(HNT, OT, XC, YPT)
        self.linear_stage(YPT, I["ml_w_down"][0], YT, 128, TT_ALL)

    def to_tok_stage(self, XT, XTOK, C, t_lo=0, t_hi=T):
        kb, nc = self.kb, self.nc
        nc_ = C // 128
        XTv = XT.rearrange("(c p) t -> p c t", p=128)
        with ExitStack() as es:
            pin = Pool(kb, es, 3, [128, nc_, 128], F32, "tti")
            pout = Pool(kb, es, 3, [128, C], F32, "tto")
            for t0 in range(t_lo, t_hi, 128):
                a, ab = pin.get()
                kb.dma("sp", a, XTv[:, :, t0:t0 + 128], writes=[ab])
                o, ob = pout.get()
                for g4 in range(nc_ // 4):
                    ps, psb = kb.ps()
                    for j in range(4):
                        c = g4 * 4 + j
                        kb.op("pe", lambda: nc.tensor.transpose(out=ps[:, j * 128:(j + 1) * 128], in_=a[:, c, :], identity=self.ident),
                              reads=[ab, self.identb], writes=[psb], signal=(j == 3))
                    kb.op("act", lambda: nc.scalar.copy(out=o[:, g4 * 512:(g4 + 1) * 512], in_=ps), reads=[psb], writes=[ob])
                kb.dma("act", XTOK[t0:t0 + 128, :], o, reads=[ob])
        kb.barrier()

    def hy_conv_stage(self, UT, UC):
        kb, nc = self.kb, self.nc
        with ExitStack() as es:
            vec = kb.tile(es, [128, 24, 5], F32, "hyvec")
            vecb = Buf()
            kb.dma("sp", vec, self.ins["hyvec"], writes=[vecb])
            pi = Pool(kb, es, 2, [128, T], F32, "hci")
            po = Pool(kb, es, 2, [128, T], F32, "hco")
            for c in range(24):
                x, xb = pi.get()
                kb.dma("sp", x, UT[c * 128:(c + 1) * 128, :], writes=[xb])
                o, ob = po.get()
                self.dwconv(o, ob, x, xb, vec[:, c, 1:5], vecb, 3, 1, 3)
                kb.dma("act", UC[c * 128:(c + 1) * 128, :], o, reads=[ob])
        kb.barrier()

    def hy_filter_stage(self, L, zT, win, FILT):
        kb, nc = self.kb, self.nc
        V = nc.vector
        I = self.ins
        MAGIC = 12582912.0
        with ExitStack() as es:
            zt = kb.tile(es, [33, L], F32, "zt")
            w1 = kb.tile(es, [33, 64], F32, "fw1")
            w2 = kb.tile(es, [64, 64], F32, "fw2")
            w3 = kb.tile(es, [64, 2 * D], F32, "fw3")
            hyf = kb.tile(es, [64, 4], F32, "hyf")
            cb = Buf()
            kb.dma("sp", zt, zT, writes=[cb])
            kb.dma("sp", w1, I["hy_f_w1"][0], writes=[cb])
            kb.dma("sp", w2, I["hy_f_w2"][0], writes=[cb])
            kb.dma("sp", w3, I["hy_f_w3"][0], writes=[cb])
            kb.dma("sp", hyf, I["hyf"], writes=[cb])
            hd1 = kb.tile(es, [64, L], F32, "hd1")
            hd2 = kb.tile(es, [64, L], F32, "hd2")
            h1b, h2b = Buf(), Buf()
            pa = Pool(kb, es, 2, [64, 512], F32, "farg")
            pk = Pool(kb, es, 2, [64, 512], F32, "fk")
            for (lhs, rhs_t, rhsb, dst, dstb, bc, fc) in ((w1, zt, cb, hd1, h1b, 0, 1), (w2, hd1, h1b, hd2, h2b, 2, 3)):
                for t0 in range(0, L, 512):
                    n = min(512, L - t0)
                    ps, psb = kb.ps()
                    kb.op("pe", lambda: nc.tensor.matmul(ps[:64, :n], lhs, rhs_t[:, t0:t0 + n], start=True, stop=True), reads=[cb, rhsb], writes=[psb])
                    a, ab = pa.get()
                    k, kbf = pk.get()
                    kb.op("dve", lambda: V.tensor_scalar(out=a[:, :n], in0=ps[:64, :n], scalar1=hyf[:, bc:bc + 1], scalar2=hyf[:, fc:fc + 1], op0=ALU.add, op1=ALU.mult),
                          reads=[psb, cb], writes=[ab])
                    kb.op("dve", lambda: V.tensor_scalar(out=k[:, :n], in0=a[:, :n], scalar1=1.0 / (2.0 * math.pi), scalar2=MAGIC, op0=ALU.mult, op1=ALU.add),
                          reads=[ab], writes=[kbf])
                    kb.op("dve", lambda: V.tensor_scalar(out=k[:, :n], in0=k[:, :n], scalar1=MAGIC, scalar2=None, op0=ALU.subtract), writes=[kbf])
                    kb.op("dve", lambda: V.scalar_tensor_tensor(out=a[:, :n], in0=k[:, :n], scalar=-2.0 * math.pi, in1=a[:, :n], op0=ALU.mult, op1=ALU.add),
                          reads=[kbf], writes=[ab])
                    kb.op("dve", lambda: V.tensor_scalar(out=a[:, :n], in0=a[:, :n], scalar1=3.1415925, scalar2=-3.1415925, op0=ALU.min, op1=ALU.max), writes=[ab])
                    kb.op("act", lambda: nc.scalar.activation(out=dst[:, t0:t0 + n], in_=a[:, :n], func=AF.Sin), reads=[ab], writes=[dstb])
            pw = Pool(kb, es, 2, [128, D], F32, "fwin")
            po = Pool(kb, es, 2, [128, 2 * D], F32, "fout")
            for sc in range(L // 128):
                wn, wnb = pw.get()
                kb.dma("sp", wn, win[sc * 128:(sc + 1) * 128, :], writes=[wnb])
                o, ob = po.get()
                for cbk in range(4):
                    ps, psb = kb.ps()
                    kb.op("pe", lambda: nc.tensor.matmul(ps, hd2[:, sc * 128:(sc + 1) * 128], w3[:, cbk * 512:(cbk + 1) * 512], start=True, stop=True),
                          reads=[h2b, cb], writes=[psb])
                    kb.op("dve", lambda: V.tensor_tensor(out=o[:, cbk * 512:(cbk + 1) * 512], in0=ps, in1=wn[:, (cbk % 2) * 512:(cbk % 2 + 1) * 512], op=ALU.mult),
                          reads=[psb, wnb], writes=[ob])
                kb.dma("act", FILT[sc * 128:(sc + 1) * 128, :], o, reads=[ob])
        kb.barrier()

    def dft_fwd_stage(self, X, L, Fre, Fim, SPEC):
        kb, nc = self.kb, self.nc
        ns = L // 128
        Xv = X.rearrange("(c p) d -> p c d", p=128)
        with ExitStack() as es:
            xt = kb.tile(es, [128, ns, D], F32R, "dfx")
            xb = Buf()
            for c in range(ns):
                kb.dma("pool", xt[:, c, :], Xv[:, c, :], writes=[xb])
            pf = Pool(kb, es, 2, [128, 2, ns, 128], F32R, "dff")
            po = Pool(kb, es, 2, [128, 2, D], F32, "dfo")
            for fc in range(ns):
                f, fb = pf.get()
                for ri, Fm in enumerate((Fre, Fim)):
                    kb.dma("pool", f[:, ri], Fm[:, fc * 128:(fc + 1) * 128].rearrange("(c p) f -> p c f", p=128), writes=[fb])
                o, ob = po.get()
                for ri in range(2):
                    for cbk in range(2):
                        ps, psb = kb.ps()
                        for c in range(ns):
                            kb.op("pe", lambda: nc.tensor.matmul(ps, f[:, ri, c, :], xt[:, c, cbk * 512:(cbk + 1) * 512], start=(c == 0), stop=(c == ns - 1)),
                                  reads=[fb, xb], writes=[psb], signal=(c == ns - 1))
                        kb.op("act", lambda: nc.scalar.copy(out=o[:, ri, cbk * 512:(cbk + 1) * 512], in_=ps), reads=[psb], writes=[ob])
                kb.dma("sp", SPEC[:, fc * 128:(fc + 1) * 128, :].rearrange("r f d -> f r d"), o, reads=[ob])
        kb.barrier()

    def dft_inv_stage(self, US, HS, L, Cre, Cim, YT, t_off):
        kb, nc = self.kb, self.nc
        V = nc.vector
        ns = L // 128
        TB = 256
        with ExitStack() as es:
            yre = kb.tile(es, [128, ns, 512], F32R, "yre")
            yim = kb.tile(es, [128, ns, 512], F32R, "yim")
            pl = Pool(kb, es, 2, [128, 4, 512], F32, "spl")
            pt = Pool(kb, es, 2, [128, 4, 512], F32, "spt")
            pc = Pool(kb, es, 2, [128, 2, ns, TB], F32R, "cmat")
            po = Pool(kb, es, 2, [128, 4, TB], F32, "ivo")
            YTv = YT.rearrange("(c p) t -> p c t", p=128)
            for half in range(2):
                cs = slice(half * 512, (half + 1) * 512)
                yb = Buf()
                for fc in range(ns):
                    fs = slice(fc * 128, (fc + 1) * 128)
                    l, lb = pl.get()
                    kb.dma("sp", l[:, 0:2, :], US[:, fs, cs].rearrange("r f d -> f r d"), writes=[lb])
                    kb.dma("sp", l[:, 2:4, :], HS[:, fs, cs].rearrange("r f d -> f r d"), writes=[lb])
                    t, tb = pt.get()
                    kb.op("dve", lambda: V.tensor_tensor(out=t[:, 0, :], in0=l[:, 0, :], in1=l[:, 2, :], op=ALU.mult), reads=[lb], writes=[tb])
                    kb.op("dve", lambda: V.tensor_tensor(out=t[:, 1, :], in0=l[:, 1, :], in1=l[:, 3, :], op=ALU.mult), reads=[lb], writes=[tb])
                    kb.op("pool", lambda: nc.gpsimd.tensor_tensor(out=t[:, 2, :], in0=l[:, 0, :], in1=l[:, 3, :], op=ALU.mult), reads=[lb], writes=[tb])
                    kb.op("pool", lambda: nc.gpsimd.tensor_tensor(out=t[:, 3, :], in0=l[:, 1, :], in1=l[:, 2, :], op=ALU.mult), reads=[lb], writes=[tb])
                    kb.op("dve", lambda: V.tensor_tensor(out=yre[:, fc, :], in0=t[:, 0, :], in1=t[:, 1, :], op=ALU.subtract), reads=[tb], writes=[yb])
                    kb.op("dve", lambda: V.tensor_tensor(out=yim[:, fc, :], in0=t[:, 2, :], in1=t[:, 3, :], op=ALU.add), reads=[tb], writes=[yb])
                    if fc == 0:
                        kb.op("dve", lambda: V.tensor_copy(out=yre[0:1, 0, :], in_=t[0:1, 0, :]), reads=[tb], writes=[yb])
                        kb.op("dve", lambda: V.tensor_copy(out=yim[0:1, 0, :], in_=t[0:1, 1, :]), reads=[tb], writes=[yb])
                for tb0 in range(0, L, TB):
                    cm, cmb = pc.get()
                    for ri, Cm in enumerate((Cre, Cim)):
                        kb.dma("pool", cm[:, ri], Cm[:, tb0:tb0 + TB].rearrange("(c p) t -> p c t", p=128), writes=[cmb])
                    o, ob = po.get()
                    for cc in range(4):
                        ps, psb = kb.ps()
                        for fc in range(ns):
                            kb.op("pe", lambda: nc.tensor.matmul(ps[:, :TB], yre[:, fc, cc * 128:(cc + 1) * 128], cm[:, 0, fc, :], start=(fc == 0), stop=False),
                                  reads=[yb, cmb], writes=[psb], signal=False)
                            kb.op("pe", lambda: nc.tensor.matmul(ps[:, :TB], yim[:, fc, cc * 128:(cc + 1) * 128], cm[:, 1, fc, :], start=False, stop=(fc == ns - 1)),
                                  reads=[yb, cmb], writes=[psb], signal=(fc == ns - 1))
                        kb.op("act", lambda: nc.scalar.copy(out=o[:, cc, :], in_=ps[:, :TB]), reads=[psb], writes=[ob])
                    kb.dma("sp", YTv[:, half * 4:(half + 1) * 4, t_off + tb0:t_off + tb0 + TB], o, reads=[ob])
        kb.barrier()

    def hy_combine_stage(self, YT_, VT, GT_, ZT, skip_idx):
        kb, nc = self.kb, self.nc
        with ExitStack() as es:
            sk = kb.tile(es, [128, KC, 2], F32, "hyskip")
            skb = Buf()
            kb.dma("sp", sk, self.ins["hyskip"], writes=[skb])
            p1 = Pool(kb, es, 2, [128, T], F32, "hb1")
            p2 = Pool(kb, es, 2, [128, T], F32, "hb2")
            p3 = Pool(kb, es, 2, [128, T], F32, "hb3")
            for c in range(KC):
                rows = slice(c * 128, (c + 1) * 128)
                y, yb = p1.get()
                v, vb = p2.get()
                g, gb = p3.get()
                kb.dma("sp", y, YT_[rows, :], writes=[yb])
                kb.dma("sp", v, VT[rows, :], writes=[vb])
                kb.dma("sp", g, GT_[rows, :], writes=[gb])
                kb.op("dve", lambda: nc.vector.scalar_tensor_tensor(out=y, in0=v, scalar=sk[:, c, skip_idx:skip_idx + 1], in1=y, op0=ALU.mult, op1=ALU.add),
                      reads=[vb, skb], writes=[yb])
                kb.op("pool", lambda: nc.gpsimd.tensor_tensor(out=y, in0=y, in1=g, op=ALU.mult), reads=[gb], writes=[yb])
                kb.dma("act", ZT[rows, :], y, reads=[yb])
        kb.barrier()

    def hyena_layer(self, HT, YT, S):
        kb, nc = self.kb, self.nc
        I = self.ins
        UT, UC, VTOK, Y1T, Z1T, Z1TOK, Y2T, Z2T, US = (S[k] for k in ("hUT", "hUC", "hVTOK", "hY1T", "hZ1T", "hZ1TOK", "hY2T", "hZ2T", "hUS"))
        vec_bias = {}
        with ExitStack() as esb:
            vec = kb.tile(esb, [128, 24, 5], F32, "hyvecb")
            vecb = Buf()
            kb.dma("sp", vec, I["hyvec"], writes=[vecb])
            self.linear_stage(HT, I["hy_w_in"][0], UT, 128, TT_ALL, mb=512, mod=(0, 1),
                              evac=lambda ps, o, mc, col, rd, wr: kb.op("act", lambda: nc.scalar.activation(out=o, in_=ps, func=AF.Identity, bias=vec[:, mc, 0:1]),
                                                                        reads=rd + [vecb], writes=wr))
        steps = []
        steps.append(lambda: self.hy_conv_stage(UT, UC))
        steps.append(lambda: self.to_tok_stage(UC[0:D, :], VTOK, D))
        cfg = {}
        for L in (CTX, SEQ):
            FILT = S["hFILT%d" % L]
            steps.append(lambda L=L, FILT=FILT: self.hy_filter_stage(L, I["hzT%d" % L], I["hwin%d" % L], FILT))
            HSs = []
            for k in range(2):
                HS = S["hHS%d_%d" % (L, k)]
                steps.append(lambda L=L, FILT=FILT, k=k, HS=HS: self.dft_fwd_stage(FILT[:, k * D:(k + 1) * D], L, I["Fre%d" % L], I["Fim%d" % L], HS))
                HSs.append(HS)
            cfg[L] = HSs
        for (L, off) in ((CTX, 0), (SEQ, CTX)):
            steps.append(lambda L=L, off=off: self.dft_fwd_stage(VTOK[off:off + L, :], L, I["Fre%d" % L], I["Fim%d" % L], US[:, 0:L, :]))
            steps.append(lambda L=L, off=off: self.dft_inv_stage(US[:, 0:L, :], cfg[L][0], L, I["Cre%d" % L], I["Cim%d" % L], Y1T, off))
        steps.append(lambda: self.hy_combine_stage(Y1T, UC[0:D, :], UC[D:2 * D, :], Z1T, 0))
        steps.append(lambda: self.to_tok_stage(Z1T, Z1TOK, D))
        for (L, off) in ((CTX, 0), (SEQ, CTX)):
            steps.append(lambda L=L, off=off: self.dft_fwd_stage(Z1TOK[off:off + L, :], L, I["Fre%d" % L], I["Fim%d" % L], US[:, 0:L, :]))
            steps.append(lambda L=L, off=off: self.dft_inv_stage(US[:, 0:L, :], cfg[L][1], L, I["Cre%d" % L], I["Cim%d" % L], Y2T, off))
        steps.append(lambda: self.hy_combine_stage(Y2T, Z1T, UC[2 * D:3 * D, :], Z2T, 1))
        steps.append(lambda: self.linear_stage(Z2T, I["hy_w_out"][0], YT, 128, TT_ALL, mb=1024))
        for st in steps[:getattr(self, "hy_stop", 999)]:
            st()

    def decl_hyena(self):
        for nm, shp in (("hy_w_in", [1, D, 3 * D]), ("hyvec", [128, 24, 5]), ("hy_f_w1", [1, 33, 64]), ("hy_f_w2", [1, 64, 64]), ("hy_f_w3", [1, 64, 2 * D]),
                        ("hyf", [64, 4]), ("hyskip", [128, KC, 2]), ("hy_w_out", [1, D, D])):
            self.inp(nm, shp)
        for L in (CTX, SEQ):
            self.inp("hzT%d" % L, [33, L])
            self.inp("hwin%d" % L, [L, D])
            for nm in ("Fre", "Fim", "Cre", "Cim"):
                self.inp("%s%d" % (nm, L), [L, L])
        S = {}
        for nm, shp in (("hUT", [3 * D, T]), ("hUC", [3 * D, T]), ("hVTOK", [T, D]), ("hY1T", [D, T]), ("hZ1T", [D, T]), ("hZ1TOK", [T, D]),
                        ("hY2T", [D, T]), ("hZ2T", [D, T]), ("hUS", [2, SEQ, D])):
            S[nm] = self.scratch(nm, shp)
        for L in (CTX, SEQ):
            S["hFILT%d" % L] = self.scratch("hFILT%d" % L, [L, 2 * D])
            for k in range(2):
                S["hHS%d_%d" % (L, k)] = self.scratch("hHS%d_%d" % (L, k), [2, L, D])
        return S

    def decl_all(self):
        for nm, shp in (("x", [SEQ, D]), ("ctx", [CTX, D]), ("pos", [SEQ, D]), ("ident", [128, 128]), ("cc", [128, KC, 2]),
                        ("ada_w", [DEPTH, D, 6 * D]), ("adab", [DEPTH, 128, 48]), ("lng", [128, DEPTH * 2 * KC]), ("lnb", [128, DEPTH * 2 * KC]),
                        ("routw", [128, KC, NE]), ("routb", [128, NE]), ("selE", [NE, NE, 128]),
                        ("moe_w_gate0", [DEPTH * NE * 128, 2048]), ("moe_w_gate1", [DEPTH * NE * 128, 2048]), ("moe_w_up0", [DEPTH * NE * 128, 2048]), ("moe_w_up1", [DEPTH * NE * 128, 2048]), ("moe_w_down0", [DEPTH * NE * 128, 2048]), ("moe_w_down1", [DEPTH * NE * 128, 2048]),
                        ("rg_w_in", [2, D, 2 * D_RNN]), ("rgv", [2, RG_BS, 16, 11]), ("rg_gate_a_w", [2, 2, 16, RG_BS, RG_BS]),
                        ("rg_gate_x_w", [2, 2, 16, RG_BS, RG_BS]), ("rg_w_out", [2, D_RNN, D]),
                        ("ml_w_up", [1, D, 2048]), ("ml_w_q", [1, 2048, D]), ("ml_w_k", [1, 2048, D]), ("ml_w_v", [1, 2048, 2048]),
                        ("ml_w_o", [1, 2048, 2048]), ("ml_w_if", [1, 2, 2048, 16]), ("ml_w_down", [1, 2048, D]),
                        ("mlvec", [128, 16, 7]), ("mlbif", [8, 4]), ("maskF", [128, 128]), ("maskB", [128, 128])):
            self.inp(nm, shp)
        self.inp("iota64", [NE, 64])
        self.inp("pidx", [128, 1])
        self.inp("triL", [NE, NE])
        self.XS = self.scratch("XS", [34 * 256, D])
        self.YS = self.scratch("YS", [34 * 256, D])
        S = self.decl_hyena()
        for nm, shp in (("XM", [2048, T]), ("XC", [2048, T]), ("QT", [D, T]), ("KT", [D, T]), ("KTOK", [T, D]), ("VTOK", [T, 2048]),
                        ("OT", [2048, T]), ("GT", [2, 2, 8, T]), ("HSF", [T, 2048]), ("HNT", [2048, T]), ("YPT", [2048, T]), ("MT", [D_RNN, T])):
            S[nm] = self.scratch(nm, shp)
        return S

    def mixer(self, layer, HT, YT, S):
        kind, jl = layer % 3, layer // 3
        if kind == 0:
            self.rglru_stage(HT, S["MT"], jl)
            self.linear_stage(S["MT"], self.ins["rg_w_out"][jl], YT, RG_BS, TT_ALL, mb=1024)
        elif kind == 1:
            self.mlstm_layer(HT, YT, S)
        else:
            self.hyena_layer(HT, YT, S)

    def build(self, stages=None):
        kb = self.kb
        self.es_global = ExitStack()
        S = self.decl_all()
        out = self.nc.dram_tensor("out", [SEQ, D], F32, kind="ExternalOutput").ap()
        HT = self.scratch("HT", [D, T])
        YT = self.scratch("YT", [D, T])
        self.persist()
        self.zero_scratch(self.XS, 34 * 256)
        self.prep(HT)
        if stages is None:
            for layer in range(self.n_layers):
                last = layer == DEPTH - 1
                self.mod_stage(layer)
                self.mixer(layer, HT, YT, S)
                self.ln_stage(HT, YT, layer, 0, TT_ALL)
                self.moe_sparse_stage(HT, YT, layer, with_ctx=not last)
                self.ln_stage(HT, YT, layer, 1, TT_X if last else TT_ALL)
        elif stages == "ln_test":
            self.mod_stage(1)
            self.prep(YT)
            self.ln_stage(HT, YT, 1, 1, TT_ALL)
        elif stages == "rg_test":
            self.mod_stage(0)
            self.mixer(0, HT, YT, S)
        elif stages == "ml_test":
            self.mod_stage(1)
            self.mixer(1, HT, YT, S)
        elif stages == "hy_test":
            self.mod_stage(2)
            self.mixer(2, HT, YT, S)
        elif stages == "moe_test":
            self.mod_stage(0)
            self.moe_stage(HT, YT, 0, True)
        elif stages == "moes_test":
            self.mod_stage(0)
            self.moe_sparse_stage(HT, YT, 0, True)
        elif stages == "moes_test_x":
            self.mod_stage(0)
            self.moe_sparse_stage(HT, YT, 0, False)
        self.final(HT, out)
        kb.barrier()
        self.es_global.close()
        return self.nc


def host_consts():
    c = {}
    c["ident"] = np.eye(128, dtype=np.float32)
    rows = SEQ // 64
    quarter = D // 4
    omega = (1.0 / (10000.0 ** (np.arange(quarter, dtype=np.float32) / np.float32(quarter)))).astype(np.float32)
    ar = np.arange(rows, dtype=np.float32)[:, None] * omega
    ac = np.arange(64, dtype=np.float32)[:, None] * omega
    er = np.concatenate([np.sin(ar), np.cos(ar)], -1)
    ec = np.concatenate([np.sin(ac), np.cos(ac)], -1)
    half = D // 2
    pos = np.concatenate([np.broadcast_to(er[:, None], (rows, 64, half)), np.broadcast_to(ec[None], (rows, 64, half))], -1)
    c["pos"] = np.ascontiguousarray(pos.reshape(SEQ, D).astype(np.float32))
    sel = np.zeros((NE, NE, 128), np.float32)
    for e in range(NE):
        sel[e, e, :] = 1.0
    c["selE"] = sel
    c["maskF"] = np.ascontiguousarray(np.triu(np.ones((128, 128), np.float32)))
    c["maskB"] = np.ascontiguousarray(np.tril(np.ones((128, 128), np.float32)))
    c["pidx"] = np.arange(128, dtype=np.float32).reshape(128, 1)
    c["iota64"] = np.ascontiguousarray(np.broadcast_to(np.arange(64, dtype=np.float32)[None, :], (NE, 64)))
    c["triL"] = np.ascontiguousarray(np.triu(np.ones((NE, NE), np.float32), 1))
    for L in (CTX, SEQ):
        n = 2 * L
        t01 = np.linspace(0.0, 1.0, L, dtype=np.float32)
        bands = np.linspace(1e-4, 15.0, 16, dtype=np.float32)
        ang = (np.float32(2.0 * math.pi / L) * np.arange(L, dtype=np.float32)[:, None]) * bands[None, :]
        z = np.concatenate([t01[:, None], np.cos(ang), -np.sin(ang)], -1).astype(np.float32)
        c["hzT%d" % L] = np.ascontiguousarray(z.T)
        dist = np.abs(np.arange(L) - L // 2).astype(np.float32) * np.float32(2.0 / L)
        d_max = math.log(1e-2) / 0.3
        d_min = math.log(1e-2) / 1.5
        deltas = np.abs(np.linspace(d_min, d_max, D, dtype=np.float32))
        c["hwin%d" % L] = np.ascontiguousarray(np.exp(-dist[:, None] * deltas[None, :]).astype(np.float32))
        sidx = np.arange(L, dtype=np.int64)
        fidx = np.arange(L, dtype=np.int64)
        th = 2.0 * np.pi * ((sidx[:, None] * fidx[None, :]) % n).astype(np.float64) / n
        Fre = np.cos(th)
        Fim = -np.sin(th)
        Fim[:, 0] = (-1.0) ** sidx
        tau = sidx + L // 2
        th2 = 2.0 * np.pi * ((fidx[:, None] * tau[None, :]) % n).astype(np.float64) / n
        Cre = (2.0 / n) * np.cos(th2)
        Cim = -(2.0 / n) * np.sin(th2)
        Cre[0, :] = 1.0 / n
        Cim[0, :] = (1.0 / n) * ((-1.0) ** tau)
        for nm, a in (("Fre", Fre), ("Fim", Fim), ("Cre", Cre), ("Cim", Cim)):
            c["%s%d" % (nm, L)] = np.ascontiguousarray(a.astype(np.float32))
    return c


def host_inputs(inputs, b, consts):
    f = lambda a: np.ascontiguousarray(np.asarray(a, dtype=np.float32))
    m = {}
    m["x"] = f(inputs["x"][b])
    m["ctx"] = f(inputs["ctx"][b])
    m["pos"] = consts["pos"]
    m["ident"] = consts["ident"]
    cc = np.stack([np.asarray(inputs["c"][b]), np.asarray(inputs["c_ctx"])], -1)
    m["cc"] = f(cc.reshape(KC, 128, 2).transpose(1, 0, 2))
    m["ada_w"] = f(inputs["ada_w"])
    m["adab"] = f(np.asarray(inputs["ada_b"]).reshape(DEPTH, 48, 128).transpose(0, 2, 1))
    m["lng"] = f(np.asarray(inputs["ln_g"]).reshape(DEPTH * 2 * KC, 128).T)
    m["lnb"] = f(np.asarray(inputs["ln_b"]).reshape(DEPTH * 2 * KC, 128).T)
    m["routw"] = f(np.asarray(inputs["router_w"]).reshape(KC, 128, NE).transpose(1, 0, 2))
    m["routb"] = f(np.broadcast_to(np.asarray(inputs["router_b"])[None, :], (128, NE)))
    m["selE"] = consts["selE"]
    for k in ("rg_w_in", "rg_gate_a_w", "rg_gate_x_w", "rg_w_out"):
        m[k] = f(inputs[k])
    for k in ("moe_w_gate", "moe_w_up"):
        a = f(inputs[k]).reshape(DEPTH * NE, 2, 4, 128, DEXP)
        for hh in range(2):
            m[k + str(hh)] = np.ascontiguousarray(a[:, hh].transpose(0, 2, 1, 3)).reshape(DEPTH * NE * 128, 4 * DEXP)
    a = f(inputs["moe_w_down"]).reshape(DEPTH * NE, 2, 2, 128, D)
    for hh in range(2):
        m["moe_w_down" + str(hh)] = np.ascontiguousarray(a[:, hh].transpose(0, 2, 1, 3)).reshape(DEPTH * NE * 128, 2 * D)
    A = np.asarray
    na = A(inputs["rg_conv_w"]).shape[0]
    vecs = [A(inputs["rg_conv_w"])[:, k] for k in range(4)] + [A(inputs["rg_conv_b"])]
    vecs += [A(inputs["rg_gate_a_b"])[:, z] for z in range(2)] + [A(inputs["rg_gate_x_b"])[:, z] for z in range(2)]
    vecs += [A(inputs["rg_lambda"])[:, z] for z in range(2)]
    rgv = np.stack(vecs, -1)
    m["rgv"] = f(rgv.reshape(na, 16, RG_BS, 11).transpose(0, 2, 1, 3))
    for k in ("ml_w_up", "ml_w_q", "ml_w_k", "ml_w_v", "ml_w_o", "ml_w_if", "ml_w_down"):
        m[k] = f(inputs[k])
    mv = [A(inputs["ml_conv_w"])[0, k] for k in range(4)] + [A(inputs["ml_conv_b"])[0], A(inputs["ml_norm_g"])[0], A(inputs["ml_skip"])[0]]
    m["mlvec"] = f(np.stack(mv, -1).reshape(16, 128, 7).transpose(1, 0, 2))
    bif = A(inputs["ml_b_if"])[0]
    m["mlbif"] = f(bif.reshape(2, 2, 8).transpose(2, 0, 1).reshape(8, 4))
    m["maskF"] = consts["maskF"]
    m["maskB"] = consts["maskB"]
    m["iota64"] = consts["iota64"]
    m["pidx"] = consts["pidx"]
    m["triL"] = consts["triL"]
    for k in ("hy_w_in", "hy_f_w1", "hy_f_w2", "hy_f_w3", "hy_w_out"):
        m[k] = f(inputs[k])
    hv = [A(inputs["hy_b_in"])[0]] + [A(inputs["hy_conv_w"])[0, k] for k in range(3)] + [A(inputs["hy_conv_b"])[0]]
    m["hyvec"] = f(np.stack(hv, -1).reshape(24, 128, 5).transpose(1, 0, 2))
    m["hyf"] = f(np.stack([A(inputs["hy_f_b1"])[0], A(inputs["hy_f_freq1"])[0], A(inputs["hy_f_b2"])[0], A(inputs["hy_f_freq2"])[0]], -1))
    m["hyskip"] = f(A(inputs["hy_skip"])[0].reshape(2, KC, 128).transpose(2, 1, 0))
    for k, v in consts.items():
        if k[0] in "hFC" and k not in m:
            m[k] = v
    return m


_CACHE = {}


def kernel(**inputs):
    consts = host_consts()
    prog = Prog()
    nc = prog.build()
    shared = None
    in_maps = []
    for b in range(8):
        m = host_inputs(inputs, b, consts) if shared is None else dict(shared)
        if shared is None:
            shared = m
        else:
            m["x"] = np.ascontiguousarray(np.asarray(inputs["x"][b], dtype=np.float32))
            m["ctx"] = np.ascontiguousarray(np.asarray(inputs["ctx"][b], dtype=np.float32))
            cc = np.stack([np.asarray(inputs["c"][b]), np.asarray(inputs["c_ctx"])], -1)
            m["cc"] = np.ascontiguousarray(cc.reshape(KC, 128, 2).transpose(1, 0, 2).astype(np.float32))
        in_maps.append({k: v for k, v in m.items() if k in prog.ins})
    res = run_bass_kernel_spmd(nc, in_maps, core_ids=list(range(8)))
    return np.stack([np.asarray(r["out"], dtype=np.float32) for r in res.results], 0)
```

```python
import math
from contextlib import ExitStack
import numpy as np
import concourse.bass as bass
import concourse.mybir as mybir
from concourse.bass_utils import run_bass_kernel_spmd

F32 = mybir.dt.float32
F32R = mybir.dt.float32r
I32 = mybir.dt.int32
AF = mybir.ActivationFunctionType
ALU = mybir.AluOpType
AX = mybir.AxisListType

D = 1024
KC = 8
SEQ = 2048
CTX = 256
T = SEQ + CTX
DEPTH = 4
ALPHA = (2.0 * DEPTH) ** 0.25
LN_EPS = 1e-6
D_RNN = 1408
RG_BS = 88
NE = 16
DEXP = 512
TT_ALL = [(0, 256), (256, 512), (768, 512), (1280, 512), (1792, 512)]
TT_X = [(256, 512), (768, 512), (1280, 512), (1792, 512)]


class Buf:
    __slots__ = ("w", "r")

    def __init__(self):
        self.w = {}
        self.r = {}


class KB:
    NRING = 12

    def __init__(self):
        nc = self.nc = bass.Bass("TRN2", target_bir_lowering=False)
        self.eng = {"pe": nc.tensor, "act": nc.scalar, "dve": nc.vector, "pool": nc.gpsimd, "sp": nc.sync}
        self.sems = {}
        self.esem = {}
        self.cnt = {}
        for e in ("pe", "act", "dve", "pool"):
            s = nc.alloc_semaphore("s_" + e)
            self.sems[s.num] = s
            self.esem[e] = s.num
            self.cnt[e] = 0
        self.dring = {}
        self.dcnt = {}
        self.dnext = {}
        for q in ("sp", "act", "pool"):
            self.dring[q] = []
            for i in range(self.NRING):
                s = nc.alloc_semaphore("d_%s%d" % (q, i))
                self.sems[s.num] = s
                self.dring[q].append(s.num)
                self.dcnt[s.num] = 0
            self.dnext[q] = 0
        self.seen = {e: {} for e in self.eng}
        self.pool_inflight = []
        self.uid = 0
        self.psum = [nc.alloc_psum_tensor("ps%d" % i, [128, 512], F32).ap() for i in range(8)]
        self.psb = [Buf() for _ in range(8)]
        self.pnext = 0

    def name(self, p):
        self.uid += 1
        return "%s_%d" % (p, self.uid)

    POOL_DESC_LIMIT = 8192

    def _pool_budget(self, need, shape):
        nd = 1
        for d in list(shape)[:-1]:
            nd *= int(d)
        tot = sum(x[2] for x in self.pool_inflight)
        while self.pool_inflight and tot + nd > self.POOL_DESC_LIMIT:
            s0, c0, n0 = self.pool_inflight.pop(0)
            if need.get(s0, 0) < c0:
                need[s0] = c0
            tot -= n0
        return nd

    def _wait(self, e, need):
        seen = self.seen[e]
        own = self.esem.get(e)
        for s, c in need.items():
            if c <= 0:
                continue
            if e == "pe" and s == own:
                continue
            if seen.get(s, 0) >= c:
                continue
            self.eng[e].wait_ge(self.sems[s], c)
            seen[s] = c

    @staticmethod
    def _merge(dst, src):
        for s, c in src.items():
            if dst.get(s, 0) < c:
                dst[s] = c

    def _deps(self, reads, writes):
        need = {}
        for b in reads:
            self._merge(need, b.w)
        for b in writes:
            self._merge(need, b.w)
            self._merge(need, b.r)
        return need

    def op(self, e, fn, reads=(), writes=(), signal=True):
        self._wait(e, self._deps(reads, writes))
        inst = fn()
        s = self.esem[e]
        if signal:
            inst.then_inc(self.sems[s], 1)
            self.cnt[e] += 1
            ev = self.cnt[e]
        else:
            ev = self.cnt[e] + 1
        for b in writes:
            b.w = {s: ev}
            b.r = {}
        for b in reads:
            if b.r.get(s, 0) < ev:
                b.r[s] = ev
        return inst

    def dma(self, q, out, in_, reads=(), writes=(), **kw):
        i = self.dnext[q]
        self.dnext[q] = (i + 1) % self.NRING
        s = self.dring[q][i]
        need = self._deps(reads, writes)
        if need.get(s, 0) < self.dcnt[s]:
            need[s] = self.dcnt[s]
        nd = self._pool_budget(need, out.shape) if q == "pool" else 0
        self._wait(q, need)
        self.eng[q].dma_start(out=out, in_=in_, **kw).then_inc(self.sems[s], 16)
        self.dcnt[s] += 16
        ev = self.dcnt[s]
        if q == "pool":
            self.pool_inflight.append((s, ev, nd))
        for b in writes:
            b.w = {s: ev}
            b.r = {}
        for b in reads:
            if b.r.get(s, 0) < ev:
                b.r[s] = ev

    def idma(self, out, out_offset, in_, in_offset, reads=(), writes=()):
        q = "pool"
        i = self.dnext[q]
        self.dnext[q] = (i + 1) % self.NRING
        s = self.dring[q][i]
        need = self._deps(reads, writes)
        if need.get(s, 0) < self.dcnt[s]:
            need[s] = self.dcnt[s]
        nd = self._pool_budget(need, [256, 1])
        self._wait(q, need)
        self.nc.gpsimd.indirect_dma_start(out=out, out_offset=out_offset, in_=in_, in_offset=in_offset).then_inc(self.sems[s], 16)
        self.dcnt[s] += 16
        ev = self.dcnt[s]
        self.pool_inflight.append((s, ev, nd))
        for b in writes:
            b.w = {s: ev}
            b.r = {}
        for b in reads:
            if b.r.get(s, 0) < ev:
                b.r[s] = ev

    def barrier(self):
        need = {}
        for e, s in self.esem.items():
            need[s] = self.cnt[e]
        for s, c in self.dcnt.items():
            need[s] = c
        for e in self.eng:
            self._wait(e, need)

    def ps(self):
        i = self.pnext
        self.pnext = (i + 1) % 8
        return self.psum[i], self.psb[i]

    def tile(self, es, shape, dtype=F32, name="t"):
        t = es.enter_context(self.nc.sbuf_tensor(self.name(name), list(shape), dtype))
        return t.ap()


class Pool:
    def __init__(self, kb, es, n, shape, dtype=F32, name="p"):
        self.t = [kb.tile(es, shape, dtype, name) for _ in range(n)]
        self.b = [Buf() for _ in range(n)]
        self.i = 0
        self.n = n

    def get(self):
        i = self.i
        self.i = (i + 1) % self.n
        return self.t[i], self.b[i]


class Prog:
    def __init__(self, n_layers=DEPTH, dbg=()):
        self.kb = KB()
        self.nc = self.kb.nc
        self.n_layers = n_layers
        self.ins = {}
        self.dbg = {}
        self.dbg_want = set(dbg)

    def inp(self, name, shape):
        t = self.nc.dram_tensor(name, list(shape), F32, kind="ExternalInput").ap()
        self.ins[name] = t
        return t

    def scratch(self, name, shape):
        kind = "ExternalOutput" if name in self.dbg_want else "Internal"
        t = self.nc.dram_tensor(name, list(shape), F32, kind=kind).ap()
        if name in self.dbg_want:
            self.dbg[name] = t
        return t

    def prep(self, HT):
        kb, nc = self.kb, self.nc
        x, ctx, pos, ident = self.ins["x"], self.ins["ctx"], self.ins["pos"], self.ins["ident"]
        HTv = HT.rearrange("(c p) t -> p c t", p=128)
        with ExitStack() as es:
            idt = kb.tile(es, [128, 128], F32, "ident")
            idb = Buf()
            kb.dma("sp", idt, ident, writes=[idb])
            pin = Pool(kb, es, 3, [128, D], F32, "pin")
            ppos = Pool(kb, es, 3, [128, D], F32, "ppos")
            pout = Pool(kb, es, 3, [128, KC, 128], F32, "pout")
            for ti in range(T // 128):
                t0 = ti * 128
                a, ab = pin.get()
                if t0 < CTX:
                    kb.dma("sp", a, ctx[t0:t0 + 128, :], writes=[ab])
                else:
                    kb.dma("sp", a, x[t0 - CTX:t0 - CTX + 128, :], writes=[ab])
                    p_, pb = ppos.get()
                    kb.dma("sp", p_, pos[t0 - CTX:t0 - CTX + 128, :], writes=[pb])
                    kb.op("dve", lambda: nc.vector.tensor_tensor(out=a, in0=a, in1=p_, op=ALU.add), reads=[pb], writes=[ab])
                o, ob = pout.get()
                for half in range(2):
                    ps, psb = kb.ps()
                    for j in range(4):
                        c = half * 4 + j
                        kb.op("pe", lambda: nc.tensor.transpose(out=ps[:, j * 128:(j + 1) * 128], in_=a[:, c * 128:(c + 1) * 128], identity=idt),
                              reads=[ab, idb], writes=[psb], signal=(j == 3))
                    kb.op("act", lambda: nc.scalar.copy(out=o[:, half * 4:half * 4 + 4, :], in_=ps.rearrange("p (c t) -> p c t", c=4)),
                          reads=[psb], writes=[ob])
                kb.dma("act", HTv[:, :, t0:t0 + 128], o, reads=[ob])
        kb.barrier()

    def final(self, HT, out):
        kb, nc = self.kb, self.nc
        ident = self.ins["ident"]
        HTv = HT.rearrange("(c p) t -> p c t", p=128)
        with ExitStack() as es:
            idt = kb.tile(es, [128, 128], F32, "ident")
            idb = Buf()
            kb.dma("sp", idt, ident, writes=[idb])
            pin = Pool(kb, es, 3, [128, KC, 128], F32, "fin")
            pout = Pool(kb, es, 3, [128, D], F32, "fout")
            for ti in range(SEQ // 128):
                t0 = CTX + ti * 128
                a, ab = pin.get()
                kb.dma("sp", a, HTv[:, :, t0:t0 + 128], writes=[ab])
                o, ob = pout.get()
                for half in range(2):
                    ps, psb = kb.ps()
                    for j in range(4):
                        c = half * 4 + j
                        kb.op("pe", lambda: nc.tensor.transpose(out=ps[:, j * 128:(j + 1) * 128], in_=a[:, c, :], identity=idt),
                              reads=[ab, idb], writes=[psb], signal=(j == 3))
                    kb.op("act", lambda: nc.scalar.copy(out=o[:, half * 512:(half + 1) * 512], in_=ps), reads=[psb], writes=[ob])
                kb.dma("act", out[ti * 128:(ti + 1) * 128, :], o, reads=[ob])
        kb.barrier()

    def persist(self):
        kb, nc = self.kb, self.nc
        es = self.es_global
        self.ident = kb.tile(es, [128, 128], F32, "identp")
        self.identb = Buf()
        kb.dma("sp", self.ident, self.ins["ident"], writes=[self.identb])
        self.ones = kb.tile(es, [128, 128], F32, "ones")
        self.onesb = Buf()
        kb.op("dve", lambda: nc.vector.memset(self.ones, 1.0), writes=[self.onesb])
        self.lng = kb.tile(es, [128, DEPTH * 2 * KC], F32, "lng")
        self.lnb = kb.tile(es, [128, DEPTH * 2 * KC], F32, "lnb")
        self.lnbuf = Buf()
        kb.dma("sp", self.lng, self.ins["lng"], writes=[self.lnbuf])
        kb.dma("sp", self.lnb, self.ins["lnb"], writes=[self.lnbuf])
        self.routw = kb.tile(es, [128, KC, NE], F32, "routw")
        self.routb = kb.tile(es, [128, NE], F32, "routb")
        self.sel = kb.tile(es, [NE, NE, 128], F32, "selE")
        self.routbuf = Buf()
        kb.dma("sp", self.routw, self.ins["routw"], writes=[self.routbuf])
        kb.dma("sp", self.routb, self.ins["routb"], writes=[self.routbuf])
        kb.dma("sp", self.sel, self.ins["selE"], writes=[self.routbuf])
        self.modT = kb.tile(es, [128, 48, 2], F32, "modT")
        self.modb = Buf()
        kb.barrier()

    def mod_stage(self, layer):
        kb, nc = self.kb, self.nc
        ada_w = self.ins["ada_w"]
        modT = self.modT
        with ExitStack() as es:
            cc = kb.tile(es, [128, KC, 2], F32, "cc")
            ccb = Buf()
            kb.dma("sp", cc, self.ins["cc"], writes=[ccb])
            sc = kb.tile(es, [128, KC, 2], F32, "sc")
            scb = Buf()
            kb.op("act", lambda: nc.scalar.activation(out=sc, in_=cc, func=AF.Silu), reads=[ccb], writes=[scb])
            ab = kb.tile(es, [128, 48], F32, "adab")
            abb = Buf()
            kb.dma("sp", ab, self.ins["adab"][layer], writes=[abb])
            wp = Pool(kb, es, 2, [128, KC, 1024], F32, "adaw")
            ps, psb = kb.ps()
            for s in range(6):
                w, wb = wp.get()
                kb.dma("sp", w, ada_w[layer, :, s * 1024:(s + 1) * 1024].rearrange("(c p) f -> p c f", p=128), writes=[wb])
                for mm in range(8):
                    m = s * 8 + mm
                    for kc in range(KC):
                        kb.op("pe", lambda: nc.tensor.matmul(ps[:, 2 * m:2 * m + 2], w[:, kc, mm * 128:(mm + 1) * 128], sc[:, kc, :],
                                                             start=(kc == 0), stop=(kc == KC - 1)),
                              reads=[wb, scb], writes=[psb], signal=(kc == KC - 1 and mm == 7))
            psv = ps[:, 0:96].rearrange("p (m j) -> p m j", j=2)
            for j in range(2):
                kb.op("dve", lambda: nc.vector.tensor_tensor(out=modT[:, :, j], in0=psv[:, :, j], in1=ab, op=ALU.add),
                      reads=[psb, abb], writes=[self.modb])
            for s in (1, 4):
                kb.op("dve", lambda: nc.vector.tensor_scalar(out=modT[:, s * 8:(s + 1) * 8, :], in0=modT[:, s * 8:(s + 1) * 8, :],
                                                             scalar1=1.0, scalar2=None, op0=ALU.add), writes=[self.modb])
            for s in (2, 5):
                kb.op("dve", lambda: nc.vector.tensor_scalar(out=modT[:, s * 8:(s + 1) * 8, :], in0=modT[:, s * 8:(s + 1) * 8, :],
                                                             scalar1=1.0 / ALPHA, scalar2=None, op0=ALU.mult), writes=[self.modb])
        kb.barrier()

    def ln_stage(self, HT, YT, layer, j, tiles):
        kb, nc = self.kb, self.nc
        g_slot = 2 if j == 0 else 5
        modT = self.modT
        HTv = HT.rearrange("(c p) t -> p c t", p=128)
        YTv = YT.rearrange("(c p) t -> p c t", p=128)
        lcol = (layer * 2 + j) * KC
        eps = LN_EPS / (ALPHA * ALPHA)
        with ExitStack() as es:
            ph = Pool(kb, es, 2, [128, KC, 512], F32, "lnh")
            py = Pool(kb, es, 2, [128, KC, 512], F32, "lny")
            pq = Pool(kb, es, 1, [128, KC, 512], F32, "lnq")
            po = Pool(kb, es, 2, [128, KC, 512], F32, "lno")
            pm = Pool(kb, es, 2, [128, 512], F32, "lnm")
            pv = Pool(kb, es, 2, [128, 512], F32, "lnv")
            epst = kb.tile(es, [128, 1], F32, "eps")
            epsb = Buf()
            kb.op("dve", lambda: nc.vector.memset(epst, eps), writes=[epsb])
            for (t0, n) in tiles:
                col = 1 if t0 < CTX else 0
                h, hb = ph.get()
                y, yb = py.get()
                kb.dma("sp", h[:, :, :n], HTv[:, :, t0:t0 + n], writes=[hb])
                kb.dma("sp", y[:, :, :n], YTv[:, :, t0:t0 + n], writes=[yb])
                for c in range(KC):
                    m = g_slot * 8 + c
                    kb.op("dve", lambda: nc.vector.scalar_tensor_tensor(out=h[:, c, :n], in0=y[:, c, :n], scalar=modT[:, m, col:col + 1],
                                                                        in1=h[:, c, :n], op0=ALU.mult, op1=ALU.add),
                          reads=[yb, self.modb], writes=[hb])
                q, qb = pq.get()
                kb.op("act", lambda: nc.scalar.activation(out=q[:, :, :n], in_=h[:, :, :n], func=AF.Square), reads=[hb], writes=[qb])
                ps1, ps1b = kb.ps()
                ps2, ps2b = kb.ps()
                for c in range(KC):
                    kb.op("pe", lambda: nc.tensor.matmul(ps1[:, :n], self.ones, h[:, c, :n], start=(c == 0), stop=(c == KC - 1)),
                          reads=[hb, self.onesb], writes=[ps1b], signal=(c == KC - 1))
                for c in range(KC):
                    kb.op("pe", lambda: nc.tensor.matmul(ps2[:, :n], self.ones, q[:, c, :n], start=(c == 0), stop=(c == KC - 1)),
                          reads=[qb, self.onesb], writes=[ps2b], signal=(c == KC - 1))
                mean, mb = pm.get()
                var, vb = pv.get()
                kb.op("act", lambda: nc.scalar.mul(out=mean[:, :n], in_=ps1[:, :n], mul=1.0 / D), reads=[ps1b], writes=[mb])
                kb.op("dve", lambda: nc.vector.tensor_tensor(out=var[:, :n], in0=mean[:, :n], in1=mean[:, :n], op=ALU.mult), reads=[mb], writes=[vb])
                kb.op("dve", lambda: nc.vector.scalar_tensor_tensor(out=var[:, :n], in0=ps2[:, :n], scalar=1.0 / D, in1=var[:, :n],
                                                                    op0=ALU.mult, op1=ALU.subtract), reads=[ps2b], writes=[vb])
                kb.op("act", lambda: nc.scalar.activation(out=var[:, :n], in_=var[:, :n], func=AF.Ln, bias=epst[:, 0:1]), reads=[epsb], writes=[vb])
                kb.op("act", lambda: nc.scalar.activation(out=var[:, :n], in_=var[:, :n], func=AF.Exp, scale=-0.5), writes=[vb])
                o, ob = po.get()
                for c in range(KC):
                    kb.op("dve", lambda: nc.vector.tensor_tensor(out=h[:, c, :n], in0=h[:, c, :n], in1=mean[:, :n], op=ALU.subtract),
                          reads=[mb], writes=[hb])
                    kb.op("pool", lambda: nc.gpsimd.tensor_tensor(out=h[:, c, :n], in0=h[:, c, :n], in1=var[:, :n], op=ALU.mult),
                          reads=[vb], writes=[hb])
                    kb.op("act", lambda: nc.scalar.activation(out=o[:, c, :n], in_=h[:, c, :n], func=AF.Identity,
                                                              scale=self.lng[:, lcol + c:lcol + c + 1], bias=self.lnb[:, lcol + c:lcol + c + 1]),
                          reads=[hb, self.lnbuf], writes=[ob])
                kb.dma("act", HTv[:, :, t0:t0 + n], o[:, :, :n], reads=[ob])
        kb.barrier()

    def moe_stage(self, HT, YT, layer, with_ctx):
        kb, nc = self.kb, self.nc
        modT = self.modT
        HTv = HT.rearrange("(c p) t -> p c t", p=128)
        YTv = YT.rearrange("(c p) t -> p c t", p=128)
        wg_d, wu_d, wd_d = self.ins.get("moe_w_gate"), self.ins.get("moe_w_up"), self.ins.get("moe_w_down")
        if with_ctx:
            groups = [[(0, 256), (256, 512)], [(768, 512), (1280, 256)], [(1536, 512), (2048, 256)]]
        else:
            groups = [[(256, 512), (768, 256)], [(1024, 512), (1536, 256)], [(1792, 512)]]
        GMAX = 768
        with ExitStack() as es:
            B = kb.tile(es, [128, KC, GMAX], F32R, "moeB")
            Bb = Buf()
            acc = kb.tile(es, [128, KC, GMAX], F32, "moeacc")
            accb = [Buf() for _ in range(2)]
            ar = kb.tile(es, [128, 2, GMAX], F32R, "moea")
            arb = [Buf() for _ in range(2)]
            gT = kb.tile(es, [NE, GMAX], F32, "gatesT")
            gTb = Buf()
            pfx = Pool(kb, es, 2, [128, KC, 128], F32, "fx32")
            pgbc = Pool(kb, es, 2, [128, GMAX], F32, "gbc")
            psil = Pool(kb, es, 2, [128, 512], F32, "sil")
            ptmp = Pool(kb, es, 2, [128, 512], F32, "tmp")
            pwg = Pool(kb, es, 2, [128, KC, 256], F32R, "wg")
            pwu = Pool(kb, es, 2, [128, KC, 256], F32R, "wu")
            pwd = Pool(kb, es, 2, [128, 2, D], F32R, "wd")
            prt = Pool(kb, es, 2, [128, 160], F32, "rt")
            for grp in groups:
                g0 = grp[0][0]
                G = sum(n for _, n in grp)
                tiles = []
                l0 = 0
                for (t0, n) in grp:
                    tiles.append((t0, n, l0))
                    l0 += n
                for si in range(G // 128):
                    t0 = g0 + si * 128
                    col = 1 if t0 < CTX else 0
                    fx, fxb = pfx.get()
                    kb.dma("sp", fx, HTv[:, :, t0:t0 + 128], writes=[fxb])
                    for c in range(KC):
                        kb.op("act", lambda: nc.scalar.activation(out=fx[:, c, :], in_=fx[:, c, :], func=AF.Identity,
                                                                  scale=modT[:, 4 * 8 + c, col:col + 1], bias=modT[:, 3 * 8 + c, col:col + 1]),
                              reads=[self.modb], writes=[fxb])
                    kb.op("dve", lambda: nc.vector.tensor_copy(out=B[:, :, si * 128:(si + 1) * 128], in_=fx), reads=[fxb], writes=[Bb])
                    ps, psb = kb.ps()
                    for c in range(KC):
                        kb.op("pe", lambda: nc.tensor.matmul(ps[:, 0:NE], fx[:, c, :], self.routw[:, c, :], start=(c == 0), stop=(c == KC - 1)),
                              reads=[fxb, self.routbuf], writes=[psb], signal=(c == KC - 1))
                    r, rb = prt.get()
                    sc = r[:, 0:16]
                    sel = r[:, 16:32]
                    eq = r[:, 32:48]
                    s2 = r[:, 48:64]
                    m1 = r[:, 64:68]
                    m2 = r[:, 68:72]
                    gs = r[:, 72:76]
                    og = r[:, 76:80]
                    t4 = r[:, 80:84]
                    gmax = r[:, 84:85]
                    m2b = r[:, 85:86]
                    wsum = r[:, 86:87]
                    selm = r[:, 96:112]
                    ch = r[:, 112:128]
                    w = r[:, 128:144]
                    gts = r[:, 144:160]
                    v3 = lambda a: a.rearrange("p (g k) -> p g k", k=4)
                    bc = lambda a: a.unsqueeze(2).to_broadcast([128, 4, 4])
                    V = nc.vector
                    kb.op("act", lambda: nc.scalar.activation(out=sc, in_=ps[:, 0:NE], func=AF.Sigmoid), reads=[psb], writes=[rb])
                    kb.op("dve", lambda: V.tensor_tensor(out=sel, in0=sc, in1=self.routb, op=ALU.add), reads=[self.routbuf], writes=[rb])
                    kb.op("dve", lambda: V.tensor_reduce(out=m1, in_=v3(sel), axis=AX.X, op=ALU.max), writes=[rb])
                    kb.op("dve", lambda: V.tensor_tensor(out=v3(eq), in0=v3(sel), in1=bc(m1), op=ALU.is_equal), writes=[rb])
                    kb.op("dve", lambda: V.scalar_tensor_tensor(out=s2, in0=eq, scalar=-1e30, in1=sel, op0=ALU.mult, op1=ALU.add), writes=[rb])
                    kb.op("dve", lambda: V.tensor_reduce(out=m2, in_=v3(s2), axis=AX.X, op=ALU.max), writes=[rb])
                    kb.op("dve", lambda: V.tensor_tensor(out=gs, in0=m1, in1=m2, op=ALU.add), writes=[rb])
                    kb.op("dve", lambda: V.tensor_reduce(out=gmax, in_=gs, axis=AX.X, op=ALU.max), writes=[rb])
                    kb.op("dve", lambda: V.tensor_scalar(out=og, in0=gs, scalar1=gmax, scalar2=None, op0=ALU.is_equal), writes=[rb])
                    kb.op("dve", lambda: V.tensor_tensor(out=t4, in0=og, in1=m2, op=ALU.mult), writes=[rb])
                    kb.op("dve", lambda: V.tensor_reduce(out=m2b, in_=t4, axis=AX.X, op=ALU.add), writes=[rb])
                    kb.op("dve", lambda: V.tensor_scalar(out=t4, in0=og, scalar1=-1.0, scalar2=1e30, op0=ALU.add, op1=ALU.mult), writes=[rb])
                    kb.op("dve", lambda: V.tensor_tensor(out=v3(selm), in0=v3(sel), in1=bc(t4), op=ALU.add), writes=[rb])
                    kb.op("dve", lambda: V.tensor_scalar(out=ch, in0=selm, scalar1=m2b, scalar2=None, op0=ALU.is_ge), writes=[rb])
                    kb.op("dve", lambda: V.tensor_tensor(out=w, in0=sc, in1=ch, op=ALU.mult), writes=[rb])
                    kb.op("dve", lambda: V.tensor_reduce(out=wsum, in_=w, axis=AX.X, op=ALU.add), writes=[rb])
                    kb.op("dve", lambda: V.reciprocal(out=wsum, in_=wsum), writes=[rb])
                    kb.op("dve", lambda: V.tensor_scalar(out=gts, in0=w, scalar1=wsum, scalar2=None, op0=ALU.mult), writes=[rb])
                    pst, pstb = kb.ps()
                    kb.op("pe", lambda: nc.tensor.transpose(out=pst[0:NE, 0:128], in_=gts, identity=self.ident), reads=[rb, self.identb], writes=[pstb])
                    kb.op("act", lambda: nc.scalar.copy(out=gT[:, si * 128:(si + 1) * 128], in_=pst[0:NE, 0:128]), reads=[pstb], writes=[gTb])
                first = True
                for e in range(NE):
                    gbc, gbcb = pgbc.get()
                    for (t0, n, l0) in tiles:
                        psg, psgb = kb.ps()
                        kb.op("pe", lambda: nc.tensor.matmul(psg[:, :n], self.sel[:, e, :], gT[:, l0:l0 + n], start=True, stop=True),
                              reads=[gTb, self.routbuf], writes=[psgb])
                        kb.op("act", lambda: nc.scalar.copy(out=gbc[:, l0:l0 + n], in_=psg[:, :n]), reads=[psgb], writes=[gbcb])
                    for jh in range(2):
                        wg, wgb = pwg.get()
                        wu, wub = pwu.get()
                        wd, wdb = pwd.get()
                        kb.dma("pool", wg, wg_d[layer * NE + e, :, jh * 256:(jh + 1) * 256].rearrange("(c p) f -> p c f", p=128), writes=[wgb])
                        kb.dma("pool", wu, wu_d[layer * NE + e, :, jh * 256:(jh + 1) * 256].rearrange("(c p) f -> p c f", p=128), writes=[wub])
                        kb.dma("pool", wd, wd_d[layer * NE + e, jh * 256:(jh + 1) * 256, :].rearrange("(j p) d -> p j d", p=128), writes=[wdb])
                        for (t0, n, l0) in tiles:
                            for jj in range(2):
                                pg, pgb = kb.ps()
                                pu, pub = kb.ps()
                                for c in range(KC):
                                    kb.op("pe", lambda: nc.tensor.matmul(pg[:, :n], wg[:, c, jj * 128:(jj + 1) * 128], B[:, c, l0:l0 + n],
                                                                         start=(c == 0), stop=(c == KC - 1)),
                                          reads=[wgb, Bb], writes=[pgb], signal=(c == KC - 1))
                                for c in range(KC):
                                    kb.op("pe", lambda: nc.tensor.matmul(pu[:, :n], wu[:, c, jj * 128:(jj + 1) * 128], B[:, c, l0:l0 + n],
                                                                         start=(c == 0), stop=(c == KC - 1)),
                                          reads=[wub, Bb], writes=[pub], signal=(c == KC - 1))
                                sl, slb = psil.get()
                                tm, tmb = ptmp.get()
                                kb.op("act", lambda: nc.scalar.activation(out=sl[:, :n], in_=pg[:, :n], func=AF.Silu), reads=[pgb], writes=[slb])
                                kb.op("dve", lambda: nc.vector.tensor_tensor(out=tm[:, :n], in0=sl[:, :n], in1=pu[:, :n], op=ALU.mult),
                                      reads=[slb, pub], writes=[tmb])
                                kb.op("pool", lambda: nc.gpsimd.tensor_tensor(out=ar[:, jj, l0:l0 + n], in0=tm[:, :n], in1=gbc[:, l0:l0 + n], op=ALU.mult),
                                      reads=[tmb, gbcb], writes=[arb[jj]])
                        for (t0, n, l0) in tiles:
                            for dc in range(KC):
                                po, pob = kb.ps()
                                for jj in range(2):
                                    kb.op("pe", lambda: nc.tensor.matmul(po[:, :n], wd[:, jj, dc * 128:(dc + 1) * 128], ar[:, jj, l0:l0 + n],
                                                                         start=(jj == 0), stop=(jj == 1)),
                                          reads=[wdb, arb[jj]], writes=[pob], signal=(jj == 1))
                                ab_ = accb[dc % 2]
                                if first:
                                    kb.op("dve", lambda: nc.vector.tensor_copy(out=acc[:, dc, l0:l0 + n], in_=po[:, :n]), reads=[pob], writes=[ab_])
                                else:
                                    kb.op("dve", lambda: nc.vector.tensor_tensor(out=acc[:, dc, l0:l0 + n], in0=acc[:, dc, l0:l0 + n], in1=po[:, :n], op=ALU.add),
                                          reads=[pob], writes=[ab_])
                        first = False
                kb.dma("act", YTv[:, :, g0:g0 + G], acc[:, :, :G], reads=accb)
        kb.barrier()

    def moe_sparse_stage(self, HT, YT, layer, with_ctx):
        kb, nc = self.kb, self.nc
        V = nc.vector
        modT = self.modT
        I = self.ins
        XS, YS = self.XS, self.YS
        HTv = HT.rearrange("(c p) t -> p c t", p=128)
        YTv = YT.rearrange("(c p) t -> p c t", p=128)
        wg_d = [I["moe_w_gate0"], I["moe_w_gate1"]]
        wu_d = [I["moe_w_up0"], I["moe_w_up1"]]
        wd_d = [I["moe_w_down0"], I["moe_w_down1"]]
        t_lo = 0 if with_ctx else CTX
        Tn = T - t_lo
        NTK = Tn // 128
        NT = (2 * Tn + NE * 255) // 256
        MAGIC = 12582912.0
        with ExitStack() as es:
            gsel = kb.tile(es, [128, NTK, 2], F32, "gsel")
            idxa = kb.tile(es, [128, NTK, 2], I32, "idxa")
            selb = Buf()
            widx = kb.tile(es, [128, 64], I32, "widx")
            widxf = kb.tile(es, [128, 64], F32, "widxf")
            pcol = kb.tile(es, [128, 1], F32, "pcol")
            widxb = Buf()
            with ExitStack() as es1:
                fxtok = kb.tile(es1, [128, NTK, D], F32, "fxtok")
                fxtb = [Buf() for _ in range(NTK)]
                gall = kb.tile(es1, [128, NTK, NE], F32, "gall")
                gallb = Buf()
                gT = kb.tile(es1, [NE, Tn], F32, "gTs")
                gTb = Buf()
                pos = kb.tile(es1, [NE, Tn], F32, "pos")
                one16 = kb.tile(es1, [NE, Tn], F32, "one16")
                rb = Buf()
                sm = kb.tile(es1, [NE, 8], F32, "smallr")
                cmp_ = kb.tile(es1, [NE, 64], F32, "cmp")
                iot = kb.tile(es1, [NE, 64], F32, "iota")
                tril = kb.tile(es1, [NE, NE], F32, "tril")
                eidf = kb.tile(es1, [1, 64], F32, "eidf")
                kb.dma("sp", iot, I["iota64"], writes=[rb])
                kb.dma("sp", pcol, I["pidx"], writes=[rb])
                kb.dma("sp", tril, I["triL"], writes=[rb])
                kb.op("dve", lambda: V.memset(one16, 1.0), writes=[rb])
                pfx = Pool(kb, es1, 2, [128, KC, 128], F32, "fx32s")
                mk3 = lambda nm, k: kb.tile(es1, [128, NTK, k], F32, nm)
                scA, selA, eqA, s2A = mk3("scA", NE), mk3("selA", NE), mk3("eqA", NE), mk3("s2A", NE)
                m1A, m2A, gsA, ogA, t4A = mk3("m1A", 4), mk3("m2A", 4), mk3("gsA", 4), mk3("ogA", 4), mk3("t4A", 4)
                gmaxA = kb.tile(es1, [128, NTK], F32, "gmaxA")
                m2bA = kb.tile(es1, [128, NTK], F32, "m2bA")
                scb_, wkb = Buf(), Buf()
                for si in range(NTK):
                    t0 = t_lo + si * 128
                    col = 1 if t0 < CTX else 0
                    fx, fxb = pfx.get()
                    kb.dma("sp", fx, HTv[:, :, t0:t0 + 128], writes=[fxb])
                    for c in range(KC):
                        kb.op("act", lambda: nc.scalar.activation(out=fx[:, c, :], in_=fx[:, c, :], func=AF.Identity,
                                                                  scale=modT[:, 4 * 8 + c, col:col + 1], bias=modT[:, 3 * 8 + c, col:col + 1]),
                              reads=[self.modb], writes=[fxb])
                    ps, psb = kb.ps()
                    for c in range(KC):
                        kb.op("pe", lambda: nc.tensor.matmul(ps[:, 0:NE], fx[:, c, :], self.routw[:, c, :], start=(c == 0), stop=(c == KC - 1)),
                              reads=[fxb, self.routbuf], writes=[psb], signal=(c == KC - 1))
                    for half in range(2):
                        pst2, pst2b = kb.ps()
                        for j in range(4):
                            c = half * 4 + j
                            kb.op("pe", lambda: nc.tensor.transpose(out=pst2[:, j * 128:(j + 1) * 128], in_=fx[:, c, :], identity=self.ident),
                                  reads=[fxb, self.identb], writes=[pst2b], signal=(j == 3))
                        kb.op("act" if half == 0 else "dve",
                              (lambda: nc.scalar.copy(out=fxtok[:, si, half * 512:(half + 1) * 512], in_=pst2)) if half == 0 else
                              (lambda: V.tensor_copy(out=fxtok[:, si, half * 512:(half + 1) * 512], in_=pst2)),
                              reads=[pst2b], writes=[fxtb[si]])
                    kb.op("act", lambda: nc.scalar.activation(out=scA[:, si, :], in_=ps[:, 0:NE], func=AF.Sigmoid), reads=[psb], writes=[scb_])
                f2 = lambda a: a.rearrange("p n e -> p (n e)")
                v4 = lambda a: a.rearrange("p n (g k) -> p n g k", k=4)
                b4 = lambda a: a.unsqueeze(3).to_broadcast([128, NTK, 4, 4])
                b16 = lambda a: a.unsqueeze(2).to_broadcast([128, NTK, NE])
                bg = lambda a: a.unsqueeze(2).to_broadcast([128, NTK, 4])
                kb.op("dve", lambda: V.tensor_tensor(out=selA, in0=scA, in1=self.routb.unsqueeze(1).to_broadcast([128, NTK, NE]), op=ALU.add),
                      reads=[scb_, self.routbuf], writes=[wkb])
                kb.op("dve", lambda: V.tensor_reduce(out=m1A, in_=v4(selA), axis=AX.X, op=ALU.max), writes=[wkb])
                kb.op("dve", lambda: V.tensor_tensor(out=v4(eqA), in0=v4(selA), in1=b4(m1A), op=ALU.is_equal), writes=[wkb])
                kb.op("dve", lambda: V.scalar_tensor_tensor(out=f2(s2A), in0=f2(eqA), scalar=-1e30, in1=f2(selA), op0=ALU.mult, op1=ALU.add), writes=[wkb])
                kb.op("dve", lambda: V.tensor_reduce(out=m2A, in_=v4(s2A), axis=AX.X, op=ALU.max), writes=[wkb])
                kb.op("dve", lambda: V.tensor_tensor(out=gsA, in0=m1A, in1=m2A, op=ALU.add), writes=[wkb])
                kb.op("dve", lambda: V.tensor_reduce(out=gmaxA, in_=gsA, axis=AX.X, op=ALU.max), writes=[wkb])
                kb.op("dve", lambda: V.tensor_tensor(out=ogA, in0=gsA, in1=bg(gmaxA), op=ALU.is_equal), writes=[wkb])
                kb.op("dve", lambda: V.tensor_tensor(out=t4A, in0=ogA, in1=m2A, op=ALU.mult), writes=[wkb])
                kb.op("dve", lambda: V.tensor_reduce(out=m2bA, in_=t4A, axis=AX.X, op=ALU.add), writes=[wkb])
                kb.op("dve", lambda: V.tensor_scalar(out=t4A, in0=ogA, scalar1=-1.0, scalar2=1e30, op0=ALU.add, op1=ALU.mult), writes=[wkb])
                kb.op("dve", lambda: V.tensor_tensor(out=v4(eqA), in0=v4(selA), in1=b4(t4A), op=ALU.add), writes=[wkb])
                kb.op("dve", lambda: V.tensor_tensor(out=s2A, in0=eqA, in1=b16(m2bA), op=ALU.is_ge), writes=[wkb])
                kb.op("dve", lambda: V.tensor_tensor(out=selA, in0=scA, in1=s2A, op=ALU.mult), writes=[wkb])
                kb.op("dve", lambda: V.tensor_reduce(out=gmaxA, in_=selA, axis=AX.X, op=ALU.add), writes=[wkb])
                kb.op("dve", lambda: V.reciprocal(out=gmaxA, in_=gmaxA), writes=[wkb])
                kb.op("dve", lambda: V.tensor_tensor(out=gall, in0=selA, in1=b16(gmaxA), op=ALU.mult), reads=[wkb], writes=[gallb])
                for si in range(NTK):
                    pst, pstb = kb.ps()
                    kb.op("pe", lambda: nc.tensor.transpose(out=pst[0:NE, 0:128], in_=gall[:, si, :], identity=self.ident), reads=[gallb, self.identb], writes=[pstb])
                    kb.op("act", lambda: nc.scalar.copy(out=gT[:, si * 128:(si + 1) * 128], in_=pst[0:NE, 0:128]), reads=[pstb], writes=[gTb])
                cnt, nt_, toff, tend, roff = (sm[:, k:k + 1] for k in range(5))
                kb.op("dve", lambda: V.tensor_scalar(out=gT, in0=gT, scalar1=0.0, scalar2=None, op0=ALU.is_gt), writes=[gTb])
                kb.op("dve", lambda: V.tensor_tensor_scan(out=pos, data0=one16, data1=gT, initial=0.0, op0=ALU.mult, op1=ALU.add), reads=[gTb], writes=[rb])
                kb.op("dve", lambda: V.tensor_scalar(out=nt_, in0=pos[:, Tn - 1:Tn], scalar1=1.0 / 256.0, scalar2=255.0 / 512.0, op0=ALU.mult, op1=ALU.add), writes=[rb])
                kb.op("dve", lambda: V.tensor_scalar(out=nt_, in0=nt_, scalar1=MAGIC, scalar2=None, op0=ALU.add), writes=[rb])
                kb.op("dve", lambda: V.tensor_scalar(out=nt_, in0=nt_, scalar1=MAGIC, scalar2=None, op0=ALU.subtract), writes=[rb])
                ps, psb = kb.ps()
                kb.op("pe", lambda: nc.tensor.matmul(ps[0:NE, 0:1], tril, nt_, start=True, stop=True), reads=[rb], writes=[psb])
                kb.op("dve", lambda: V.tensor_copy(out=toff, in_=ps[0:NE, 0:1]), reads=[psb], writes=[rb])
                kb.op("dve", lambda: V.tensor_tensor(out=tend, in0=toff, in1=nt_, op=ALU.add), writes=[rb])
                kb.op("dve", lambda: V.tensor_scalar(out=roff, in0=toff, scalar1=256.0, scalar2=None, op0=ALU.mult), writes=[rb])
                kb.op("dve", lambda: V.tensor_scalar(out=pos, in0=pos, scalar1=roff, scalar2=None, op0=ALU.add), writes=[rb])
                kb.op("dve", lambda: V.tensor_tensor(out=pos, in0=pos, in1=gT, op=ALU.mult), reads=[gTb], writes=[rb])
                kb.op("dve", lambda: V.tensor_scalar(out=cmp_, in0=iot, scalar1=tend, scalar2=None, op0=ALU.is_ge), writes=[rb])
                ps, psb = kb.ps()
                kb.op("pe", lambda: nc.tensor.matmul(ps[0:1, 0:64], self.ones[:NE, 0:1], cmp_, start=True, stop=True), reads=[rb, self.onesb], writes=[psb])
                kb.op("dve", lambda: V.tensor_scalar(out=eidf, in0=ps[0:1, 0:64], scalar1=float(NE - 1), scalar2=float(layer * NE), op0=ALU.min, op1=ALU.add),
                      reads=[psb], writes=[rb])
                psw, pswb = kb.ps()
                kb.op("pe", lambda: nc.tensor.matmul(psw[:, 0:64], self.ones[0:1, :], eidf, start=True, stop=True), reads=[rb, self.onesb], writes=[pswb])
                kb.op("dve", lambda: V.tensor_scalar(out=widxf, in0=psw[:, 0:64], scalar1=128.0, scalar2=pcol[:, 0:1], op0=ALU.mult, op1=ALU.add),
                      reads=[pswb, rb], writes=[widxb])
                kb.op("dve", lambda: V.tensor_copy(out=widx, in_=widxf), writes=[widxb])
                for si in range(NTK):
                    ps, psb = kb.ps()
                    kb.op("pe", lambda: nc.tensor.transpose(out=ps[:, 0:NE], in_=pos[:, si * 128:(si + 1) * 128], identity=self.ident[:NE, :NE]),
                          reads=[rb, self.identb], writes=[psb])
                    kb.op("act", lambda: nc.scalar.copy(out=scA[:, si, :], in_=ps[:, 0:NE]), reads=[psb, wkb], writes=[scb_])
                r1A, r2A = gmaxA, m2bA
                kb.op("dve", lambda: V.tensor_reduce(out=r1A, in_=scA, axis=AX.X, op=ALU.max), reads=[scb_], writes=[wkb])
                kb.op("dve", lambda: V.tensor_tensor(out=eqA, in0=scA, in1=b16(r1A), op=ALU.is_equal), writes=[wkb])
                kb.op("dve", lambda: V.tensor_tensor(out=selA, in0=eqA, in1=scA, op=ALU.mult), writes=[wkb])
                kb.op("dve", lambda: V.tensor_tensor(out=selA, in0=scA, in1=selA, op=ALU.subtract), writes=[wkb])
                kb.op("dve", lambda: V.tensor_reduce(out=r2A, in_=selA, axis=AX.X, op=ALU.max), writes=[wkb])
                kb.op("dve", lambda: V.tensor_tensor(out=s2A, in0=selA, in1=b16(r2A), op=ALU.is_equal), writes=[wkb])
                kb.op("dve", lambda: V.tensor_tensor(out=eqA, in0=eqA, in1=gall, op=ALU.mult), reads=[gallb], writes=[wkb])
                kb.op("dve", lambda: V.tensor_tensor(out=s2A, in0=s2A, in1=gall, op=ALU.mult), reads=[gallb], writes=[wkb])
                kb.op("dve", lambda: V.tensor_reduce(out=gsel[:, :, 0], in_=eqA, axis=AX.X, op=ALU.add), reads=[wkb], writes=[selb])
                kb.op("dve", lambda: V.tensor_reduce(out=gsel[:, :, 1], in_=s2A, axis=AX.X, op=ALU.add), reads=[wkb], writes=[selb])
                kb.op("dve", lambda: V.tensor_scalar(out=idxa[:, :, 0], in0=r1A, scalar1=-1.0, scalar2=None, op0=ALU.add), reads=[wkb], writes=[selb])
                kb.op("dve", lambda: V.tensor_scalar(out=idxa[:, :, 1], in0=r2A, scalar1=-1.0, scalar2=None, op0=ALU.add), reads=[wkb], writes=[selb])
                for si in range(NTK):
                    for j in range(2):
                        kb.idma(XS, bass.IndirectOffsetOnAxis(ap=idxa[:, si, j:j + 1], axis=0), fxtok[:, si, :], None, reads=[selb, fxtb[si]])
                kb.barrier()
            if getattr(self, "moe_stop", 9) < 2:
                return
            with ExitStack() as es2:
                pwg = Pool(kb, es2, 2, [128, KC, DEXP], F32R, "swg")
                pwu = Pool(kb, es2, 2, [128, KC, DEXP], F32R, "swu")
                pwd = Pool(kb, es2, 2, [128, 4, D], F32R, "swd")
                pxt = Pool(kb, es2, 4, [128, D], F32, "sxt")
                pxT = Pool(kb, es2, 2, [128, KC, 256], F32R, "sxT")
                psl = Pool(kb, es2, 3, [128, 256], F32, "ssl")
                pa = Pool(kb, es2, 2, [128, 4, 256], F32R, "sa")
                pys = Pool(kb, es2, 4, [128, D], F32, "sys")
                for i in range(NT):
                    wg, wgb = pwg.get()
                    wu, wub = pwu.get()
                    wd, wdb = pwd.get()
                    off = bass.IndirectOffsetOnAxis(ap=widx[:, i:i + 1], axis=0)
                    for hh in range(2):
                        kb.idma(wg[:, hh * 4:(hh + 1) * 4, :].rearrange("p c f -> p (c f)"), None, wg_d[hh], off, reads=[widxb], writes=[wgb])
                        kb.idma(wu[:, hh * 4:(hh + 1) * 4, :].rearrange("p c f -> p (c f)"), None, wu_d[hh], off, reads=[widxb], writes=[wub])
                        kb.idma(wd[:, hh * 2:(hh + 1) * 2, :].rearrange("p j d -> p (j d)"), None, wd_d[hh], off, reads=[widxb], writes=[wdb])
                    xT, xTb = pxT.get()
                    for h in range(2):
                        xt, xtb = pxt.get()
                        kb.dma("sp", xt, XS[i * 256 + h * 128:i * 256 + (h + 1) * 128, :], writes=[xtb])
                        for half in range(2):
                            ps, psb = kb.ps()
                            for j in range(4):
                                c = half * 4 + j
                                kb.op("pe", lambda: nc.tensor.transpose(out=ps[:, j * 128:(j + 1) * 128], in_=xt[:, c * 128:(c + 1) * 128], identity=self.ident),
                                      reads=[xtb, self.identb], writes=[psb], signal=(j == 3))
                            src = ps.rearrange("p (c t) -> p c t", c=4)
                            dst = xT[:, half * 4:(half + 1) * 4, h * 128:(h + 1) * 128]
                            if half == 0:
                                kb.op("act", lambda: nc.scalar.copy(out=dst, in_=src), reads=[psb], writes=[xTb])
                            else:
                                kb.op("dve", lambda: V.tensor_copy(out=dst, in_=src), reads=[psb], writes=[xTb])
                    a, ab = pa.get()
                    for j in range(4):
                        pg, pgb = kb.ps()
                        pu, pub = kb.ps()
                        for c in range(KC):
                            kb.op("pe", lambda: nc.tensor.matmul(pg[:, 0:256], wg[:, c, j * 128:(j + 1) * 128], xT[:, c, :], start=(c == 0), stop=(c == KC - 1)),
                                  reads=[wgb, xTb], writes=[pgb], signal=(c == KC - 1))
                        for c in range(KC):
                            kb.op("pe", lambda: nc.tensor.matmul(pu[:, 0:256], wu[:, c, j * 128:(j + 1) * 128], xT[:, c, :], start=(c == 0), stop=(c == KC - 1)),
                                  reads=[wub, xTb], writes=[pub], signal=(c == KC - 1))
                        sl, slb = psl.get()
                        kb.op("act", lambda: nc.scalar.activation(out=sl, in_=pg[:, 0:256], func=AF.Silu), reads=[pgb], writes=[slb])
                        kb.op("dve", lambda: V.tensor_tensor(out=a[:, j, :], in0=sl, in1=pu[:, 0:256], op=ALU.mult), reads=[slb, pub], writes=[ab])
                    for th in range(2):
                        ys, ysb = pys.get()
                        for dh in range(2):
                            po, pob = kb.ps()
                            for j in range(4):
                                kb.op("pe", lambda: nc.tensor.matmul(po, a[:, j, th * 128:(th + 1) * 128], wd[:, j, dh * 512:(dh + 1) * 512], start=(j == 0), stop=(j == 3)),
                                      reads=[ab, wdb], writes=[pob], signal=(j == 3))
                            if dh == 0:
                                kb.op("act", lambda: nc.scalar.copy(out=ys[:, 0:512], in_=po), reads=[pob], writes=[ysb])
                            else:
                                kb.op("dve", lambda: V.tensor_copy(out=ys[:, 512:1024], in_=po), reads=[pob], writes=[ysb])
                        kb.dma("act", YS[i * 256 + th * 128:i * 256 + (th + 1) * 128, :], ys, reads=[ysb])
                kb.barrier()
            if getattr(self, "moe_stop", 9) < 3:
                return
            with ExitStack() as es3:
                pya = Pool(kb, es3, 2, [128, D], F32, "cya")
                pyb = Pool(kb, es3, 2, [128, D], F32, "cyb")
                pot = Pool(kb, es3, 2, [128, KC, 128], F32, "cot")
                for si in range(NTK):
                    t0 = t_lo + si * 128
                    ya, yab = pya.get()
                    yb, ybb = pyb.get()
                    kb.idma(ya, None, YS, bass.IndirectOffsetOnAxis(ap=idxa[:, si, 0:1], axis=0), reads=[selb], writes=[yab])
                    kb.idma(yb, None, YS, bass.IndirectOffsetOnAxis(ap=idxa[:, si, 1:2], axis=0), reads=[selb], writes=[ybb])
                    kb.op("act", lambda: nc.scalar.activation(out=ya, in_=ya, func=AF.Copy, scale=gsel[:, si, 0:1]), reads=[selb], writes=[yab])
                    kb.op("dve", lambda: V.scalar_tensor_tensor(out=ya, in0=yb, scalar=gsel[:, si, 1:2], in1=ya, op0=ALU.mult, op1=ALU.add),
                          reads=[ybb, selb], writes=[yab])
                    o, ob = pot.get()
                    for half in range(2):
                        ps, psb = kb.ps()
                        for j in range(4):
                            c = half * 4 + j
                            kb.op("pe", lambda: nc.tensor.transpose(out=ps[:, j * 128:(j + 1) * 128], in_=ya[:, c * 128:(c + 1) * 128], identity=self.ident),
                                  reads=[yab, self.identb], writes=[psb], signal=(j == 3))
                        kb.op("act" if half == 0 else "dve",
                              (lambda: nc.scalar.copy(out=o[:, half * 4:half * 4 + 4, :], in_=ps.rearrange("p (c t) -> p c t", c=4))) if half == 0 else
                              (lambda: V.tensor_copy(out=o[:, half * 4:half * 4 + 4, :], in_=ps.rearrange("p (c t) -> p c t", c=4))),
                              reads=[psb], writes=[ob])
                    kb.dma("sp", YTv[:, :, t0:t0 + 128], o, reads=[ob])
                kb.barrier()

    def zero_scratch(self, X, rows):
        kb, nc = self.kb, self.nc
        with ExitStack() as es:
            z = kb.tile(es, [128, D], F32, "zeros")
            zb = Buf()
            kb.op("dve", lambda: nc.vector.memset(z, 0.0), writes=[zb])
            for r0 in range(0, rows, 128):
                kb.dma("sp" if (r0 // 128) % 2 == 0 else "act", X[r0:r0 + 128, :], z, reads=[zb])
            kb.barrier()

    def linear_stage(self, XT, W, YT, kp, tiles, mb=512, evac=None, mod=None, extra=None):
        kb, nc = self.kb, self.nc
        K_, M = W.shape
        nk = K_ // kp
        XTv = XT.rearrange("(c p) t -> p c t", p=kp)
        Wv = W.rearrange("(c p) m -> p c m", p=kp)
        YTv = YT.rearrange("(c p) t -> p c t", p=128)
        mb = min(mb, M)
        with ExitStack() as es:
            if mod is not None:
                U, Ub = self.load_mod(es, XT, mod[0], mod[1], tiles)
            pw = Pool(kb, es, 1 if M <= mb else 2, [kp, nk, mb], F32R, "linw")
            if mod is None:
                px = Pool(kb, es, 2, [kp, nk, 512], F32R, "linx")
            po = Pool(kb, es, 2, [128, mb // 128, 512], F32, "lino")
            for m0 in range(0, M, mb):
                w, wb = pw.get()
                kb.dma("pool", w, Wv[:, :, m0:m0 + mb], writes=[wb])
                for (t0, n) in tiles:
                    col = 1 if t0 < CTX else 0
                    if mod is None:
                        x, xb = px.get()
                        kb.dma("pool", x[:, :, :n], XTv[:, :, t0:t0 + n], writes=[xb])
                    else:
                        x, xb = U[:, :, t0:t0 + n], Ub[t0]
                    if extra is not None and m0 == 0:
                        extra(x, xb, t0, n)
                    o, ob = po.get()
                    for mc in range(mb // 128):
                        ps, psb = kb.ps()
                        for c in range(nk):
                            kb.op("pe", lambda: nc.tensor.matmul(ps[:, :n], w[:, c, mc * 128:(mc + 1) * 128], x[:, c, :n], start=(c == 0), stop=(c == nk - 1)),
                                  reads=[wb, xb], writes=[psb], signal=(c == nk - 1))
                        if evac is None:
                            kb.op("act", lambda: nc.scalar.copy(out=o[:, mc, :n], in_=ps[:, :n]), reads=[psb], writes=[ob])
                        else:
                            evac(ps[:, :n], o[:, mc, :n], m0 // 128 + mc, col, [psb], [ob])
                    kb.dma("sp", YTv[:, m0 // 128:(m0 + mb) // 128, t0:t0 + n], o[:, :, :n], reads=[ob])
        kb.barrier()

    def load_mod(self, es, HT, sh_slot, sc_slot, tiles):
        kb, nc = self.kb, self.nc
        HTv = HT.rearrange("(c p) t -> p c t", p=128)
        U = kb.tile(es, [128, KC, T], F32R, "U")
        Ub = {}
        with ExitStack() as es2:
            ph = Pool(kb, es2, 2, [128, KC, 512], F32, "uh")
            for (t0, n) in tiles:
                col = 1 if t0 < CTX else 0
                h, hb = ph.get()
                kb.dma("sp", h[:, :, :n], HTv[:, :, t0:t0 + n], writes=[hb])
                Ub[t0] = Buf()
                for c in range(KC):
                    kb.op("act", lambda: nc.scalar.activation(out=U[:, c, t0:t0 + n], in_=h[:, c, :n], func=AF.Identity,
                                                              scale=self.modT[:, sc_slot * 8 + c, col:col + 1], bias=self.modT[:, sh_slot * 8 + c, col:col + 1]),
                          reads=[hb, self.modb], writes=[Ub[t0]])
            kb.barrier()
        return U, Ub

    def rglru_stage(self, HT, MT, jl, RG):
        kb, nc = self.kb, self.nc
        NB = 16
        P = RG_BS
        self.linear_stage(HT, self.ins["rg_w_in"][jl], RG, 128, TT_ALL, mb=256, mod=(0, 1),
                          evac=lambda ps, o, mc, col, rd, wr: kb.op("act", lambda: nc.scalar.activation(
                              out=o, in_=ps, func=(AF.Gelu_apprx_tanh if mc < D_RNN // 128 else AF.Copy)), reads=rd, writes=wr))
        with ExitStack() as es:
            rgv = kb.tile(es, [P, NB, 11], F32, "rgv")
            rgvb = Buf()
            kb.dma("sp", rgv, self.ins["rgv"][jl], writes=[rgvb])
            pgw = Pool(kb, es, 2, [P, 2, 2, P], F32R, "rggw")
            coef = kb.tile(es, [P, 2, NB], F32, "coef")
            coef2 = kb.tile(es, [P, 2, NB], F32, "coef2")
            cb = Buf()
            for z in range(2):
                kb.op("act", lambda: nc.scalar.activation(out=coef[:, z, :], in_=rgv[:, :, 9 + z], func=AF.Exp, scale=-1.0), reads=[rgvb], writes=[cb])
            kb.op("act", lambda: nc.scalar.activation(out=coef, in_=coef, func=AF.Ln, bias=self.ones[:P, 0:1]), reads=[self.onesb], writes=[cb])
            kb.op("dve", lambda: nc.vector.tensor_scalar(out=coef2, in0=coef, scalar1=-16.0, scalar2=None, op0=ALU.mult), writes=[cb])
            kb.op("dve", lambda: nc.vector.tensor_scalar(out=coef, in0=coef, scalar1=-8.0, scalar2=None, op0=ALU.mult), writes=[cb])
            mkp = lambda nm, dt=F32: Pool(kb, es, 2, [P, T], dt, nm)
            prec, pgate, pxc, pxcr, pr, pi, pa, pw, phs = (mkp("rec"), mkp("gate"), mkp("xc"), mkp("xcr", F32R), mkp("r"), mkp("i"), mkp("a"), mkp("w"), mkp("hs"))
            segs = [(0, CTX), (CTX, T)]
            for n in range(NB):
                gw4, gwb = pgw.get()
                kb.dma("pool", gw4[:, 0], self.ins["rg_gate_a_w"][jl, :, n].rearrange("z k j -> k z j"), writes=[gwb])
                kb.dma("pool", gw4[:, 1], self.ins["rg_gate_x_w"][jl, :, n].rearrange("z k j -> k z j"), writes=[gwb])
                rec, recb = prec.get()
                gate, gateb = pgate.get()
                kb.dma("sp", rec, RG[D_RNN + n * P:D_RNN + (n + 1) * P, :], writes=[recb])
                kb.dma("sp", gate, RG[n * P:(n + 1) * P, :], writes=[gateb])
                xc, xcb = pxc.get()
                xcr, xcrb = pxcr.get()
                hs, hsb = phs.get()
                kb.op("dve", lambda: nc.vector.tensor_scalar(out=xc, in0=rec, scalar1=rgv[:, n, 2:3], scalar2=rgv[:, n, 4:5], op0=ALU.mult, op1=ALU.add),
                      reads=[recb, rgvb], writes=[xcb])
                for k in (0, 1, 3):
                    d = k - 2
                    for (s0, s1) in segs:
                        ta, tb = max(s0, s0 - d), min(s1, s1 - d)
                        kb.op("dve", lambda: nc.vector.scalar_tensor_tensor(out=xc[:, ta:tb], in0=rec[:, ta + d:tb + d], scalar=rgv[:, n, k:k + 1],
                                                                            in1=xc[:, ta:tb], op0=ALU.mult, op1=ALU.add),
                              reads=[recb, rgvb], writes=[xcb])
                kb.op("act", lambda: nc.scalar.copy(out=xcr, in_=xc), reads=[xcb], writes=[xcrb])
                for z in range(2):
                    r, rb = pr.get()
                    ig, igb = pi.get()
                    a_, ab_ = pa.get()
                    w_, wb_ = pw.get()
                    for gi, (gt, gtb, bcol) in enumerate(((r, rb, 5 + z), (ig, igb, 7 + z))):
                        for (t0, nn) in TT_ALL:
                            ps, psb = kb.ps()
                            kb.op("pe", lambda: nc.tensor.matmul(ps[:P, :nn], gw4[:, gi, z, :], xcr[:, t0:t0 + nn], start=True, stop=True),
                                  reads=[gwb, xcrb], writes=[psb])
                            kb.op("act", lambda: nc.scalar.activation(out=gt[:, t0:t0 + nn], in_=ps[:P, :nn], func=AF.Sigmoid, bias=rgv[:, n, bcol:bcol + 1]),
                                  reads=[psb, rgvb], writes=[gtb])
                    kb.op("act", lambda: nc.scalar.activation(out=a_, in_=r, func=AF.Exp, scale=coef[:, z, n:n + 1]), reads=[rb, cb], writes=[ab_])
                    kb.op("act", lambda: nc.scalar.activation(out=w_, in_=r, func=AF.Exp, scale=coef2[:, z, n:n + 1]), reads=[rb, cb], writes=[wb_])
                    kb.op("act", lambda: nc.scalar.activation(out=w_, in_=w_, func=AF.Ln, scale=-1.0, bias=self.ones[:P, 0:1]), reads=[self.onesb], writes=[wb_])
                    kb.op("act", lambda: nc.scalar.activation(out=w_, in_=w_, func=AF.Exp, scale=0.5), writes=[wb_])
                    kb.op("dve", lambda: nc.vector.tensor_tensor(out=ig, in0=ig, in1=xc, op=ALU.mult), reads=[xcb], writes=[igb])
                    kb.op("pool", lambda: nc.gpsimd.tensor_tensor(out=ig, in0=ig, in1=w_, op=ALU.mult), reads=[wb_], writes=[igb])
                    if z == 0:
                        kb.op("dve", lambda: nc.vector.tensor_tensor_scan(out=hs, data0=a_, data1=ig, initial=0.0, op0=ALU.mult, op1=ALU.add),
                              reads=[ab_, igb], writes=[hsb])
                    else:
                        kb.op("dve", lambda: nc.vector.tensor_tensor_scan(out=r[:, 0:CTX][:, ::-1], data0=a_[:, 0:CTX][:, ::-1], data1=ig[:, 0:CTX][:, ::-1],
                                                                          initial=0.0, op0=ALU.mult, op1=ALU.add),
                              reads=[ab_, igb], writes=[rb])
                        kb.op("dve", lambda: nc.vector.tensor_tensor_scan(out=r[:, CTX:T][:, ::-1], data0=a_[:, CTX:T][:, ::-1], data1=ig[:, CTX:T][:, ::-1],
                                                                          initial=r[:, 0:1], op0=ALU.mult, op1=ALU.add),
                              reads=[ab_, igb], writes=[rb])
                        kb.op("pool", lambda: nc.gpsimd.tensor_tensor(out=hs, in0=hs, in1=r, op=ALU.add), reads=[rb], writes=[hsb])
                kb.op("dve", lambda: nc.vector.tensor_tensor(out=hs, in0=hs, in1=gate, op=ALU.mult), reads=[gateb], writes=[hsb])
                kb.dma("act", MT[n * P:(n + 1) * P, :], hs, reads=[hsb])
        kb.barrier()

    def linear_tok_stage(self, XT, W, Y, evac=None):
        kb, nc = self.kb, self.nc
        K_, M = W.shape
        nk = K_ // 128
        XTv = XT.rearrange("(c p) t -> p c t", p=128)
        Wv = W.rearrange("(c p) m -> p c m", p=128)
        mb = 512
        with ExitStack() as es:
            pw = Pool(kb, es, 2, [128, nk, mb], F32R, "ltw")
            px = Pool(kb, es, 3, [128, nk, 128], F32R, "ltx")
            po = Pool(kb, es, 3, [128, mb], F32, "lto")
            for m0 in range(0, M, mb):
                w, wb = pw.get()
                kb.dma("pool", w, Wv[:, :, m0:m0 + mb], writes=[wb])
                for ti in range(T // 128):
                    t0 = ti * 128
                    x, xb = px.get()
                    kb.dma("pool", x, XTv[:, :, t0:t0 + 128], writes=[xb])
                    ps, psb = kb.ps()
                    for c in range(nk):
                        kb.op("pe", lambda: nc.tensor.matmul(ps, x[:, c, :], w[:, c, :], start=(c == 0), stop=(c == nk - 1)),
                              reads=[wb, xb], writes=[psb], signal=(c == nk - 1))
                    o, ob = po.get()
                    if evac is None:
                        kb.op("act", lambda: nc.scalar.copy(out=o, in_=ps), reads=[psb], writes=[ob])
                    else:
                        evac(ps, o, [psb], [ob])
                    kb.dma("sp", Y[t0:t0 + 128, m0:m0 + mb], o, reads=[ob])
        kb.barrier()

    def dwconv(self, out, outb, x, xb, vec, vecb, ntap, left, bias_col, P=128, eng="dve"):
        kb, nc = self.kb, self.nc
        kb.op("dve", lambda: nc.vector.tensor_scalar(out=out, in0=x, scalar1=vec[:, left:left + 1], scalar2=vec[:, bias_col:bias_col + 1],
                                                     op0=ALU.mult, op1=ALU.add), reads=[xb, vecb], writes=[outb])
        for k in range(ntap):
            d = k - left
            if d == 0:
                continue
            for (s0, s1) in ((0, CTX), (CTX, T)):
                ta, tb = max(s0, s0 - d), min(s1, s1 - d)
                kb.op("dve", lambda: nc.vector.scalar_tensor_tensor(out=out[:, ta:tb], in0=x[:, ta + d:tb + d], scalar=vec[:, k:k + 1],
                                                                    in1=out[:, ta:tb], op0=ALU.mult, op1=ALU.add),
                      reads=[xb, vecb], writes=[outb])

    def ml_conv_stage(self, XM, XC):
        kb, nc = self.kb, self.nc
        with ExitStack() as es:
            vec = kb.tile(es, [128, 16, 7], F32, "mlvec")
            vecb = Buf()
            kb.dma("sp", vec, self.ins["mlvec"], writes=[vecb])
            pi = Pool(kb, es, 2, [128, T], F32, "mci")
            po = Pool(kb, es, 2, [128, T], F32, "mco")
            for c in range(16):
                x, xb = pi.get()
                kb.dma("sp", x, XM[c * 128:(c + 1) * 128, :], writes=[xb])
                o, ob = po.get()
                self.dwconv(o, ob, x, xb, vec[:, c, :], vecb, 4, 2, 4)
                kb.op("act", lambda: nc.scalar.activation(out=o, in_=o, func=AF.Silu), writes=[ob])
                kb.dma("act", XC[c * 128:(c + 1) * 128, :], o, reads=[ob])
        kb.barrier()

    def ml_core_stage(self, GT, QT, KT, KTOK, VTOK, HSF, HNT):
        kb, nc = self.kb, self.nc
        NCH = T // 128
        V = nc.vector
        QTv = QT.rearrange("(h p) t -> p h t", p=128)
        KTv = KT.rearrange("(h p) t -> p h t", p=128)
        HNTv = HNT.rearrange("(c p) t -> p c t", p=128)
        for z in range(2):
            order = list(range(NCH)) if z == 0 else [1, 0] + list(range(NCH - 1, 1, -1))
            li = 127 if z == 0 else 0
            with ExitStack() as es:
                colz = kb.tile(es, [128, NCH, 32], F32, "colz")
                colb = Buf()
                spb = kb.tile(es, [128, 8, NCH], F32, "spb")
                slb = kb.tile(es, [128, 8, NCH], F32, "slb")
                spbb = Buf()
                mask = kb.tile(es, [128, 128], F32, "mask")
                maskb = Buf()
                kb.dma("sp", mask, self.ins["maskF" if z == 0 else "maskB"], writes=[maskb])
                with ExitStack() as es2:
                    rt = lambda nm: kb.tile(es2, [8, T], F32, nm)
                    ig, fg, G, A, cm, Mx, E1, F_, inter, edm, one8 = [rt(nm) for nm in ("ig", "fg", "G", "A", "cm", "Mx", "E1", "F", "inter", "edm", "one8")]
                    rb = Buf()
                    ch = lambda nm: kb.tile(es2, [8, NCH], F32, nm)
                    Gend, Gprev, maxA, btot, mloc, mq, mprev, Pq, Pn, sp_, sl_ = [ch(nm) for nm in ("Gend", "Gprev", "maxA", "btot", "mloc", "mq", "mprev", "Pq", "Pn", "sp", "sl")]
                    spd = kb.tile(es2, [8, 8, NCH], F32, "spd")
                    sld = kb.tile(es2, [8, 8, NCH], F32, "sld")
                    bif = kb.tile(es2, [8, 4], F32, "bif")
                    kb.dma("sp", ig, GT[z, 0], writes=[rb])
                    kb.dma("sp", fg, GT[z, 1], writes=[rb])
                    kb.op("dve", lambda: V.memset(one8, 1.0), writes=[rb])
                    kb.op("act", lambda: nc.scalar.activation(out=fg, in_=fg, func=AF.Exp, scale=-1.0), writes=[rb])
                    kb.op("act", lambda: nc.scalar.activation(out=fg, in_=fg, func=AF.Ln, bias=self.ones[:8, 0:1]), reads=[self.onesb], writes=[rb])
                    if z == 0:
                        kb.op("dve", lambda: V.tensor_tensor_scan(out=G, data0=one8, data1=fg, initial=0.0, op0=ALU.mult, op1=ALU.subtract), writes=[rb])
                    else:
                        kb.op("dve", lambda: V.tensor_tensor_scan(out=G[:, 0:CTX][:, ::-1], data0=one8[:, 0:CTX], data1=fg[:, 0:CTX][:, ::-1], initial=0.0,
                                                                  op0=ALU.mult, op1=ALU.subtract), writes=[rb])
                        kb.op("dve", lambda: V.tensor_tensor_scan(out=G[:, CTX:T][:, ::-1], data0=one8[:, CTX:T], data1=fg[:, CTX:T][:, ::-1], initial=G[:, 0:1],
                                                                  op0=ALU.mult, op1=ALU.subtract), writes=[rb])
                    kb.op("dve", lambda: V.tensor_tensor(out=A, in0=ig, in1=G, op=ALU.subtract), writes=[rb])
                    for c in range(NCH):
                        sl = slice(c * 128, (c + 1) * 128)
                        rv = (lambda a: a[:, sl][:, ::-1]) if z == 1 else (lambda a: a[:, sl])
                        kb.op("dve", lambda: V.tensor_tensor_scan(out=rv(cm), data0=one8[:, sl], data1=rv(A), initial=-1e30, op0=ALU.mult, op1=ALU.max), writes=[rb])
                    c3 = lambda a: a.rearrange("p (c i) -> p c i", i=128)
                    maxA_nat = c3(cm)[:, :, li]
                    Gend_nat = c3(G)[:, :, li]

                    def to_proc(dst, src):
                        if z == 0:
                            kb.op("dve", lambda: V.tensor_copy(out=dst, in_=src), writes=[rb])
                        else:
                            kb.op("dve", lambda: V.tensor_copy(out=dst[:, 0:1], in_=src[:, 1:2]), writes=[rb])
                            kb.op("dve", lambda: V.tensor_copy(out=dst[:, 1:2], in_=src[:, 0:1]), writes=[rb])
                            kb.op("dve", lambda: V.tensor_copy(out=dst[:, 2:NCH], in_=src[:, 2:NCH][:, ::-1]), writes=[rb])

                    to_proc(Gend, Gend_nat)
                    to_proc(maxA, maxA_nat)
                    kb.op("dve", lambda: V.memset(Gprev[:, 0:1], 0.0), writes=[rb])
                    kb.op("dve", lambda: V.tensor_copy(out=Gprev[:, 1:NCH], in_=Gend[:, 0:NCH - 1]), writes=[rb])
                    kb.op("dve", lambda: V.tensor_tensor(out=btot, in0=Gend, in1=Gprev, op=ALU.subtract), writes=[rb])
                    kb.op("dve", lambda: V.tensor_tensor(out=mloc, in0=Gend, in1=maxA, op=ALU.add), writes=[rb])
                    kb.op("dve", lambda: V.tensor_tensor_scan(out=mq, data0=btot, data1=mloc, initial=0.0, op0=ALU.add, op1=ALU.max), writes=[rb])
                    kb.op("dve", lambda: V.memset(mprev[:, 0:1], 0.0), writes=[rb])
                    kb.op("dve", lambda: V.tensor_copy(out=mprev[:, 1:NCH], in_=mq[:, 0:NCH - 1]), writes=[rb])
                    kb.op("dve", lambda: V.tensor_tensor(out=Pq, in0=mprev, in1=Gprev, op=ALU.subtract), writes=[rb])
                    kb.op("dve", lambda: V.tensor_tensor(out=sp_, in0=btot, in1=mprev, op=ALU.add), writes=[rb])
                    kb.op("dve", lambda: V.tensor_tensor(out=sp_, in0=sp_, in1=mq, op=ALU.subtract), writes=[rb])
                    kb.op("act", lambda: nc.scalar.activation(out=sp_, in_=sp_, func=AF.Exp), writes=[rb])
                    kb.op("dve", lambda: V.tensor_tensor(out=sl_, in0=mloc, in1=mq, op=ALU.subtract), writes=[rb])
                    kb.op("act", lambda: nc.scalar.activation(out=sl_, in_=sl_, func=AF.Exp), writes=[rb])
                    to_proc(Pn, Pq)
                    bcc = lambda a: a.unsqueeze(2).to_broadcast([8, NCH, 128])
                    kb.op("dve", lambda: V.tensor_tensor(out=c3(Mx), in0=c3(cm), in1=bcc(Pn), op=ALU.max), writes=[rb])
                    kb.op("dve", lambda: V.tensor_tensor(out=c3(E1), in0=c3(A), in1=bcc(maxA_nat), op=ALU.subtract), writes=[rb])
                    kb.op("act", lambda: nc.scalar.activation(out=E1, in_=E1, func=AF.Exp), writes=[rb])
                    kb.op("dve", lambda: V.tensor_tensor(out=c3(F_), in0=bcc(maxA_nat), in1=c3(Mx), op=ALU.subtract), writes=[rb])
                    kb.op("act", lambda: nc.scalar.activation(out=F_, in_=F_, func=AF.Exp), writes=[rb])
                    kb.op("dve", lambda: V.tensor_tensor(out=c3(inter), in0=bcc(Pn), in1=c3(Mx), op=ALU.subtract), writes=[rb])
                    kb.op("act", lambda: nc.scalar.activation(out=inter, in_=inter, func=AF.Exp), writes=[rb])
                    kb.op("dve", lambda: V.tensor_tensor(out=edm, in0=G, in1=Mx, op=ALU.add), writes=[rb])
                    kb.op("act", lambda: nc.scalar.activation(out=edm, in_=edm, func=AF.Exp, scale=-1.0), writes=[rb])
                    for c in range(NCH):
                        ps, psb = kb.ps()
                        for ai, arr in enumerate((E1, F_, inter, edm)):
                            kb.op("pe", lambda: nc.tensor.matmul(ps[:, ai * 8:(ai + 1) * 8], arr[:, c * 128:(c + 1) * 128], self.ident[:8, :8], start=True, stop=True),
                                  reads=[rb, self.identb], writes=[psb], signal=(ai == 3))
                        kb.op("act", lambda: nc.scalar.copy(out=colz[:, c, :], in_=ps[:, 0:32]), reads=[psb], writes=[colb])
                    idb = self.ident[:8, :8].unsqueeze(2).to_broadcast([8, 8, NCH])
                    for (src, dd_, dst) in ((sp_, spd, spb), (sl_, sld, slb)):
                        kb.op("dve", lambda: V.tensor_tensor(out=dd_, in0=idb, in1=src.unsqueeze(1).to_broadcast([8, 8, NCH]), op=ALU.mult),
                              reads=[self.identb], writes=[rb])
                        ps, psb = kb.ps()
                        kb.op("pe", lambda: nc.tensor.matmul(ps[:, 0:8 * NCH], self.ones[:8, :], dd_.rearrange("p h q -> p (h q)"), start=True, stop=True),
                              reads=[rb, self.onesb], writes=[psb])
                        kb.op("act", lambda: nc.scalar.copy(out=dst.rearrange("p h q -> p (h q)"), in_=ps[:, 0:8 * NCH]), reads=[psb], writes=[spbb])
                    kb.barrier()
                pkt = Pool(kb, es, 2, [128, 8, 128], F32R, "kt")
                pqt = Pool(kb, es, 2, [128, 8, 128], F32R, "qt")
                pktok = Pool(kb, es, 2, [128, 1024], F32R, "ktok")
                pvx = Pool(kb, es, 2, [128, 8, 258], F32R, "vext")
                pvw = Pool(kb, es, 2, [128, 8, 258], F32R, "vw")
                for t_, b_ in zip(pvx.t, pvx.b):
                    kb.op("dve", lambda: V.tensor_copy(out=t_[:, :, 256:257], in_=self.ones[:, 0:8].unsqueeze(2)), reads=[self.onesb], writes=[b_])
                    kb.op("dve", lambda: V.tensor_scalar(out=t_[:, :, 257:258], in0=self.ones[:, 0:8].unsqueeze(2), scalar1=0.0, scalar2=None, op0=ALU.mult),
                          reads=[self.onesb], writes=[b_])
                cn = kb.tile(es, [128, 8, 258], F32, "cn")
                cnr = kb.tile(es, [128, 8, 258], F32R, "cnr")
                cnb = [Buf() for _ in range(8)]
                cnrb = [Buf() for _ in range(8)]
                pst = Pool(kb, es, 3, [128, 128], F32R, "sT")
                pt1 = Pool(kb, es, 3, [128, 258], F32, "t1")
                ptc = Pool(kb, es, 3, [128, 258], F32, "tc")
                pdd = Pool(kb, es, 4, [128, 2], F32, "dd")
                phc = Pool(kb, es, 2, [128, 8, 256], F32, "hch")
                if z == 1:
                    phf = Pool(kb, es, 2, [128, 8, 256], F32, "hf")
                    pstt = Pool(kb, es, 2, [128, 8, 6], F32, "bst")
                    pmv = Pool(kb, es, 2, [128, 8, 2], F32, "bmv")
                    prs = Pool(kb, es, 2, [128, 8], F32, "brs")
                    phnt = Pool(kb, es, 2, [128, 16, 128], F32, "hnt")
                    epst = kb.tile(es, [128, 1], F32, "eps")
                    epsb = Buf()
                    kb.op("dve", lambda: V.memset(epst, LN_EPS), writes=[epsb])
                for q, c in enumerate(order):
                    sl = slice(c * 128, (c + 1) * 128)
                    kt, ktb = pkt.get()
                    qt, qtb = pqt.get()
                    ktok, ktokb = pktok.get()
                    vx, vxb = pvx.get()
                    vw, vwb = pvw.get()
                    kb.dma("pool", kt, KTv[:, :, sl], writes=[ktb])
                    kb.dma("pool", qt, QTv[:, :, sl], writes=[qtb])
                    kb.dma("pool", ktok, KTOK[sl, :], writes=[ktokb])
                    kb.dma("pool", vx[:, :, 0:256], VTOK[sl, :].rearrange("s (h v) -> s h v", h=8), writes=[vxb])
                    kb.op("dve", lambda: V.tensor_tensor(out=vw, in0=vx, in1=colz[:, c, 0:8].unsqueeze(2).to_broadcast([128, 8, 258]), op=ALU.mult),
                          reads=[vxb, colb], writes=[vwb])
                    hch, hcb = phc.get()
                    for h in range(8):
                        E1c = colz[:, c, h:h + 1]
                        Fc = colz[:, c, 8 + h:9 + h]
                        inc = colz[:, c, 16 + h:17 + h]
                        edc = colz[:, c, 24 + h:25 + h]
                        psS, psSb = kb.ps()
                        kb.op("pe", lambda: nc.tensor.matmul(psS[:, 0:128], kt[:, h, :], qt[:, h, :], start=True, stop=True), reads=[ktb, qtb], writes=[psSb])
                        sT, sTb = pst.get()
                        kb.op("dve", lambda: V.scalar_tensor_tensor(out=sT, in0=psS[:, 0:128], scalar=E1c, in1=mask, op0=ALU.mult, op1=ALU.mult),
                              reads=[psSb, colb, maskb], writes=[sTb])
                        psN, psNb = kb.ps()
                        kb.op("pe", lambda: nc.tensor.matmul(psN[:, 0:258], sT, vx[:, h, :], start=True, stop=True), reads=[sTb, vxb], writes=[psNb])
                        t1, t1b = pt1.get()
                        if q > 0:
                            psI, psIb = kb.ps()
                            kb.op("pe", lambda: nc.tensor.matmul(psI[:, 0:258], qt[:, h, :], cnr[:, h, :], start=True, stop=True), reads=[qtb, cnrb[h]], writes=[psIb])
                            kb.op("act", lambda: nc.scalar.activation(out=t1, in_=psI[:, 0:258], func=AF.Copy, scale=inc), reads=[psIb, colb], writes=[t1b])
                            kb.op("dve", lambda: V.scalar_tensor_tensor(out=t1, in0=psN[:, 0:258], scalar=Fc, in1=t1, op0=ALU.mult, op1=ALU.add),
                                  reads=[psNb, colb], writes=[t1b])
                        else:
                            kb.op("act", lambda: nc.scalar.activation(out=t1, in_=psN[:, 0:258], func=AF.Copy, scale=Fc), reads=[psNb, colb], writes=[t1b])
                        dd, ddb = pdd.get()
                        kb.op("act", lambda: nc.scalar.activation(out=dd[:, 0:1], in_=t1[:, 256:257], func=AF.Abs), reads=[t1b], writes=[ddb])
                        kb.op("dve", lambda: V.tensor_scalar(out=dd[:, 0:1], in0=dd[:, 0:1], scalar1=edc, scalar2=None, op0=ALU.max),
                              reads=[colb], writes=[ddb])
                        kb.op("dve", lambda: V.reciprocal(out=dd[:, 1:2], in_=dd[:, 0:1]), writes=[ddb])
                        kb.op("pool", lambda: nc.gpsimd.tensor_scalar(out=hch[:, h, :], in0=t1[:, 0:256], scalar1=dd[:, 1:2], scalar2=None, op0=ALU.mult),
                              reads=[t1b, ddb], writes=[hcb])
                        if q < NCH - 1:
                            psC, psCb = kb.ps()
                            kb.op("pe", lambda: nc.tensor.matmul(psC[:, 0:258], ktok[:, h * 128:(h + 1) * 128], vw[:, h, :], start=True, stop=True),
                                  reads=[ktokb, vwb], writes=[psCb])
                            if q == 0:
                                kb.op("act", lambda: nc.scalar.activation(out=cn[:, h, :], in_=psC[:, 0:258], func=AF.Copy, scale=slb[:, h, q:q + 1]),
                                      reads=[psCb, spbb], writes=[cnb[h]])
                            else:
                                tc_, tcb = ptc.get()
                                kb.op("act", lambda: nc.scalar.activation(out=tc_, in_=psC[:, 0:258], func=AF.Copy, scale=slb[:, h, q:q + 1]),
                                      reads=[psCb, spbb], writes=[tcb])
                                kb.op("dve", lambda: V.scalar_tensor_tensor(out=cn[:, h, :], in0=cn[:, h, :], scalar=spb[:, h, q:q + 1], in1=tc_,
                                                                            op0=ALU.mult, op1=ALU.add), reads=[tcb, spbb], writes=[cnb[h]])
                            kb.op("pool", lambda: nc.gpsimd.tensor_copy(out=cnr[:, h, :], in_=cn[:, h, :]), reads=[cnb[h]], writes=[cnrb[h]])
                    if z == 0:
                        kb.dma("sp", HSF[sl, :], hch.rearrange("p h v -> p (h v)"), reads=[hcb])
                    else:
                        hf, hfb = phf.get()
                        kb.dma("sp", hf.rearrange("p h v -> p (h v)"), HSF[sl, :], writes=[hfb])
                        kb.op("pool", lambda: nc.gpsimd.tensor_tensor(out=hch, in0=hch, in1=hf, op=ALU.add), reads=[hfb], writes=[hcb])
                        st, stb = pstt.get()
                        mv, mvb = pmv.get()
                        rs, rsb = prs.get()
                        for h in range(8):
                            kb.op("dve", lambda: V.bn_stats(out=st[:, h, :], in_=hch[:, h, :]), reads=[hcb], writes=[stb])
                            kb.op("dve", lambda: V.bn_aggr(out=mv[:, h, :], in_=st[:, h, :]), reads=[stb], writes=[mvb])
                        kb.op("act", lambda: nc.scalar.activation(out=rs, in_=mv[:, :, 1], func=AF.Ln, bias=epst[:, 0:1]), reads=[mvb, epsb], writes=[rsb])
                        kb.op("act", lambda: nc.scalar.activation(out=rs, in_=rs, func=AF.Exp, scale=-0.5), writes=[rsb])
                        for h in range(8):
                            kb.op("dve", lambda: V.tensor_scalar(out=hch[:, h, :], in0=hch[:, h, :], scalar1=mv[:, h, 0:1], scalar2=rs[:, h:h + 1],
                                                                 op0=ALU.subtract, op1=ALU.mult), reads=[mvb, rsb], writes=[hcb])
                        hnt, hntb = phnt.get()
                        h2 = hch.rearrange("p h v -> p (h v)")
                        for g4 in range(4):
                            ps, psb = kb.ps()
                            for j in range(4):
                                fc = g4 * 4 + j
                                kb.op("pe", lambda: nc.tensor.transpose(out=ps[:, j * 128:(j + 1) * 128], in_=h2[:, fc * 128:(fc + 1) * 128], identity=self.ident),
                                      reads=[hcb, self.identb], writes=[psb], signal=(j == 3))
                            kb.op("act", lambda: nc.scalar.copy(out=hnt[:, g4 * 4:(g4 + 1) * 4, :], in_=ps.rearrange("p (c t) -> p c t", c=4)), reads=[psb], writes=[hntb])
                        kb.dma("sp", HNTv[:, :, sl], hnt, reads=[hntb])
            kb.barrier()

    def ml_combine_stage(self, HNT, OT, XC, YPT):
        kb, nc = self.kb, self.nc
        with ExitStack() as es:
            vec = kb.tile(es, [128, 16, 7], F32, "mlvec")
            vecb = Buf()
            kb.dma("sp", vec, self.ins["mlvec"], writes=[vecb])
            p1 = Pool(kb, es, 2, [128, T], F32, "cb1")
            p2 = Pool(kb, es, 2, [128, T], F32, "cb2")
            p3 = Pool(kb, es, 2, [128, T], F32, "cb3")
            for c in range(16):
                rows = slice(c * 128, (c + 1) * 128)
                a, ab = p1.get()
                o, ob = p2.get()
                x, xb = p3.get()
                kb.dma("sp", a, HNT[rows, :], writes=[ab])
                kb.dma("sp", o, OT[rows, :], writes=[ob])
                kb.dma("sp", x, XC[rows, :], writes=[xb])
                kb.op("dve", lambda: nc.vector.scalar_tensor_tensor(out=a, in0=a, scalar=vec[:, c, 5:6], in1=o, op0=ALU.mult, op1=ALU.mult),
                      reads=[ob, vecb], writes=[ab])
                kb.op("dve", lambda: nc.vector.scalar_tensor_tensor(out=a, in0=x, scalar=vec[:, c, 6:7], in1=a, op0=ALU.mult, op1=ALU.add),
                      reads=[xb, vecb], writes=[ab])
                kb.dma("act", YPT[rows, :], a, reads=[ab])
        kb.barrier()

    def mlstm_layer(self, HT, YT, S):
        kb, nc = self.kb, self.nc
        I = self.ins
        XM, XC, QT, KT, KTOK, VTOK, OT, GT, HSF, HNT, YPT = (S[k] for k in ("XM", "XC", "QT", "KT", "KTOK", "VTOK", "OT", "GT", "HSF", "HNT", "YPT"))
        self.linear_stage(HT, I["ml_w_up"][0], XM, 128, TT_ALL, mb=512, mod=(0, 1))
        self.ml_conv_stage(XM, XC)
        self.linear_stage(XC, I["ml_w_q"][0], QT, 128, TT_ALL)
        kscale = 128.0 ** -0.5
        self.linear_stage(XC, I["ml_w_k"][0], KT, 128, TT_ALL,
                          evac=lambda ps, o, mc, col, rd, wr: kb.op("act", lambda: nc.scalar.mul(out=o, in_=ps, mul=kscale), reads=rd, writes=wr))
        self.linear_tok_stage(XC, I["ml_w_k"][0], KTOK,
                              evac=lambda ps, o, rd, wr: kb.op("act", lambda: nc.scalar.mul(out=o, in_=ps, mul=kscale), reads=rd, writes=wr))
        self.linear_tok_stage(XM, I["ml_w_v"][0], VTOK)
        with ExitStack() as esg:
            wif = kb.tile(esg, [128, 16, 2, 16], F32R, "wif")
            wifb = Buf()
            for z in range(2):
                kb.dma("pool", wif[:, :, z, :], I["ml_w_if"][0, z].rearrange("(c p) g -> p c g", p=128), writes=[wifb])
            bif = kb.tile(esg, [8, 4], F32, "bif")
            bifb = Buf()
            kb.dma("sp", bif, I["mlbif"], writes=[bifb])
            pg = Pool(kb, esg, 2, [8, 4, 512], F32, "gout")

            def extra(x, xb, t0, n):
                g, gb = pg.get()
                for z in range(2):
                    for gi in range(2):
                        ps, psb = kb.ps()
                        for c in range(16):
                            kb.op("pe", lambda: nc.tensor.matmul(ps[:8, :n], wif[:, c, z, gi * 8:(gi + 1) * 8], x[:, c, :n], start=(c == 0), stop=(c == 15)),
                                  reads=[wifb, xb], writes=[psb], signal=(c == 15))
                        kb.op("act", lambda: nc.scalar.activation(out=g[:, z * 2 + gi, :n], in_=ps[:8, :n], func=AF.Identity, bias=bif[:, z * 2 + gi:z * 2 + gi + 1]),
                              reads=[psb, bifb], writes=[gb])
                kb.dma("sp", GT.rearrange("z g h t -> h (z g) t")[:, :, t0:t0 + n], g[:, :, :n], reads=[gb])

            self.linear_stage(XM, I["ml_w_o"][0], OT, 128, TT_ALL, extra=extra,
                              evac=lambda ps, o, mc, col, rd, wr: kb.op("act", lambda: nc.scalar.activation(out=o, in_=ps, func=AF.Sigmoid), reads=rd, writes=wr))
        self.ml_core_stage(GT, QT, KT, KTOK, VTOK, HSF, HNT)
        self.ml_combine_stage(HNT, OT, XC, YPT)
        self.linear_stage(YPT, I["ml_w_down"][0], YT, 128, TT_ALL)

    def to_tok_stage(self, XT, XTOK, C, t_lo=0, t_hi=T):
        kb, nc = self.kb, self.nc
        nc_ = C // 128
        XTv = XT.rearrange("(c p) t -> p c t", p=128)
        with ExitStack() as es:
            pin = Pool(kb, es, 3, [128, nc_, 128], F32, "tti")
            pout = Pool(kb, es, 3, [128, C], F32, "tto")
            for t0 in range(t_lo, t_hi, 128):
                a, ab = pin.get()
                kb.dma("sp", a, XTv[:, :, t0:t0 + 128], writes=[ab])
                o, ob = pout.get()
                for g4 in range(nc_ // 4):
                    ps, psb = kb.ps()
                    for j in range(4):
                        c = g4 * 4 + j
                        kb.op("pe", lambda: nc.tensor.transpose(out=ps[:, j * 128:(j + 1) * 128], in_=a[:, c, :], identity=self.ident),
                              reads=[ab, self.identb], writes=[psb], signal=(j == 3))
                    kb.op("act", lambda: nc.scalar.copy(out=o[:, g4 * 512:(g4 + 1) * 512], in_=ps), reads=[psb], writes=[ob])
                kb.dma("act", XTOK[t0:t0 + 128, :], o, reads=[ob])
        kb.barrier()

    def hy_conv_stage(self, UT, UC):
        kb, nc = self.kb, self.nc
        with ExitStack() as es:
            vec = kb.tile(es, [128, 24, 5], F32, "hyvec")
            vecb = Buf()
            kb.dma("sp", vec, self.ins["hyvec"], writes=[vecb])
            pi = Pool(kb, es, 2, [128, T], F32, "hci")
            po = Pool(kb, es, 2, [128, T], F32, "hco")
            for c in range(24):
                x, xb = pi.get()
                kb.dma("sp", x, UT[c * 128:(c + 1) * 128, :], writes=[xb])
                o, ob = po.get()
                self.dwconv(o, ob, x, xb, vec[:, c, 1:5], vecb, 3, 1, 3)
                kb.dma("act", UC[c * 128:(c + 1) * 128, :], o, reads=[ob])
        kb.barrier()

    def hy_filter_stage(self, L, zT, win, FILT):
        kb, nc = self.kb, self.nc
        V = nc.vector
        I = self.ins
        MAGIC = 12582912.0
        with ExitStack() as es:
            zt = kb.tile(es, [33, L], F32, "zt")
            w1 = kb.tile(es, [33, 64], F32, "fw1")
            w2 = kb.tile(es, [64, 64], F32, "fw2")
            w3 = kb.tile(es, [64, 2 * D], F32, "fw3")
            hyf = kb.tile(es, [64, 4], F32, "hyf")
            cb = Buf()
            kb.dma("sp", zt, zT, writes=[cb])
            kb.dma("sp", w1, I["hy_f_w1"][0], writes=[cb])
            kb.dma("sp", w2, I["hy_f_w2"][0], writes=[cb])
            kb.dma("sp", w3, I["hy_f_w3"][0], writes=[cb])
            kb.dma("sp", hyf, I["hyf"], writes=[cb])
            hd1 = kb.tile(es, [64, L], F32, "hd1")
            hd2 = kb.tile(es, [64, L], F32, "hd2")
            h1b, h2b = Buf(), Buf()
            pa = Pool(kb, es, 2, [64, 512], F32, "farg")
            pk = Pool(kb, es, 2, [64, 512], F32, "fk")
            for (lhs, rhs_t, rhsb, dst, dstb, bc, fc) in ((w1, zt, cb, hd1, h1b, 0, 1), (w2, hd1, h1b, hd2, h2b, 2, 3)):
                for t0 in range(0, L, 512):
                    n = min(512, L - t0)
                    ps, psb = kb.ps()
                    kb.op("pe", lambda: nc.tensor.matmul(ps[:64, :n], lhs, rhs_t[:, t0:t0 + n], start=True, stop=True), reads=[cb, rhsb], writes=[psb])
                    a, ab = pa.get()
                    k, kbf = pk.get()
                    kb.op("dve", lambda: V.tensor_scalar(out=a[:, :n], in0=ps[:64, :n], scalar1=hyf[:, bc:bc + 1], scalar2=hyf[:, fc:fc + 1], op0=ALU.add, op1=ALU.mult),
                          reads=[psb, cb], writes=[ab])
                    kb.op("dve", lambda: V.tensor_scalar(out=k[:, :n], in0=a[:, :n], scalar1=1.0 / (2.0 * math.pi), scalar2=MAGIC, op0=ALU.mult, op1=ALU.add),
                          reads=[ab], writes=[kbf])
                    kb.op("dve", lambda: V.tensor_scalar(out=k[:, :n], in0=k[:, :n], scalar1=MAGIC, scalar2=None, op0=ALU.subtract), writes=[kbf])
                    kb.op("dve", lambda: V.scalar_tensor_tensor(out=a[:, :n], in0=k[:, :n], scalar=-2.0 * math.pi, in1=a[:, :n], op0=ALU.mult, op1=ALU.add),
                          reads=[kbf], writes=[ab])
                    kb.op("dve", lambda: V.tensor_scalar(out=a[:, :n], in0=a[:, :n], scalar1=3.1415925, scalar2=-3.1415925, op0=ALU.min, op1=ALU.max), writes=[ab])
                    kb.op("act", lambda: nc.scalar.activation(out=dst[:, t0:t0 + n], in_=a[:, :n], func=AF.Sin), reads=[ab], writes=[dstb])
            pw = Pool(kb, es, 2, [128, D], F32, "fwin")
            po = Pool(kb, es, 2, [128, 2 * D], F32, "fout")
            for sc in range(L // 128):
                wn, wnb = pw.get()
                kb.dma("sp", wn, win[sc * 128:(sc + 1) * 128, :], writes=[wnb])
                o, ob = po.get()
                for cbk in range(4):
                    ps, psb = kb.ps()
                    kb.op("pe", lambda: nc.tensor.matmul(ps, hd2[:, sc * 128:(sc + 1) * 128], w3[:, cbk * 512:(cbk + 1) * 512], start=True, stop=True),
                          reads=[h2b, cb], writes=[psb])
                    kb.op("dve", lambda: V.tensor_tensor(out=o[:, cbk * 512:(cbk + 1) * 512], in0=ps, in1=wn[:, (cbk % 2) * 512:(cbk % 2 + 1) * 512], op=ALU.mult),
                          reads=[psb, wnb], writes=[ob])
                kb.dma("act", FILT[sc * 128:(sc + 1) * 128, :], o, reads=[ob])
        kb.barrier()

    def dft_fwd_stage(self, X, L, Fre, Fim, SPEC):
        kb, nc = self.kb, self.nc
        ns = L // 128
        Xv = X.rearrange("(c p) d -> p c d", p=128)
        with ExitStack() as es:
            xt = kb.tile(es, [128, ns, D], F32R, "dfx")
            xb = Buf()
            for c in range(ns):
                kb.dma("pool", xt[:, c, :], Xv[:, c, :], writes=[xb])
            pf = Pool(kb, es, 2, [128, 2, ns, 128], F32R, "dff")
            po = Pool(kb, es, 2, [128, 2, D], F32, "dfo")
            for fc in range(ns):
                f, fb = pf.get()
                for ri, Fm in enumerate((Fre, Fim)):
                    kb.dma("pool", f[:, ri], Fm[:, fc * 128:(fc + 1) * 128].rearrange("(c p) f -> p c f", p=128), writes=[fb])
                o, ob = po.get()
                for ri in range(2):
                    for cbk in range(2):
                        ps, psb = kb.ps()
                        for c in range(ns):
                            kb.op("pe", lambda: nc.tensor.matmul(ps, f[:, ri, c, :], xt[:, c, cbk * 512:(cbk + 1) * 512], start=(c == 0), stop=(c == ns - 1)),
                                  reads=[fb, xb], writes=[psb], signal=(c == ns - 1))
                        kb.op("act", lambda: nc.scalar.copy(out=o[:, ri, cbk * 512:(cbk + 1) * 512], in_=ps), reads=[psb], writes=[ob])
                kb.dma("sp", SPEC[:, fc * 128:(fc + 1) * 128, :].rearrange("r f d -> f r d"), o, reads=[ob])
        kb.barrier()

    def dft_inv_stage(self, US, HS, L, Cre, Cim, YT, t_off):
        kb, nc = self.kb, self.nc
        V = nc.vector
        ns = L // 128
        TB = 256
        with ExitStack() as es:
            yre = kb.tile(es, [128, ns, 512], F32R, "yre")
            yim = kb.tile(es, [128, ns, 512], F32R, "yim")
            pl = Pool(kb, es, 2, [128, 4, 512], F32, "spl")
            pt = Pool(kb, es, 2, [128, 4, 512], F32, "spt")
            pc = Pool(kb, es, 2, [128, 2, ns, TB], F32R, "cmat")
            po = Pool(kb, es, 2, [128, 4, TB], F32, "ivo")
            YTv = YT.rearrange("(c p) t -> p c t", p=128)
            for half in range(2):
                cs = slice(half * 512, (half + 1) * 512)
                yb = Buf()
                for fc in range(ns):
                    fs = slice(fc * 128, (fc + 1) * 128)
                    l, lb = pl.get()
                    kb.dma("sp", l[:, 0:2, :], US[:, fs, cs].rearrange("r f d -> f r d"), writes=[lb])
                    kb.dma("sp", l[:, 2:4, :], HS[:, fs, cs].rearrange("r f d -> f r d"), writes=[lb])
                    t, tb = pt.get()
                    kb.op("dve", lambda: V.tensor_tensor(out=t[:, 0, :], in0=l[:, 0, :], in1=l[:, 2, :], op=ALU.mult), reads=[lb], writes=[tb])
                    kb.op("dve", lambda: V.tensor_tensor(out=t[:, 1, :], in0=l[:, 1, :], in1=l[:, 3, :], op=ALU.mult), reads=[lb], writes=[tb])
                    kb.op("pool", lambda: nc.gpsimd.tensor_tensor(out=t[:, 2, :], in0=l[:, 0, :], in1=l[:, 3, :], op=ALU.mult), reads=[lb], writes=[tb])
                    kb.op("pool", lambda: nc.gpsimd.tensor_tensor(out=t[:, 3, :], in0=l[:, 1, :], in1=l[:, 2, :], op=ALU.mult), reads=[lb], writes=[tb])
                    kb.op("dve", lambda: V.tensor_tensor(out=yre[:, fc, :], in0=t[:, 0, :], in1=t[:, 1, :], op=ALU.subtract), reads=[tb], writes=[yb])
                    kb.op("dve", lambda: V.tensor_tensor(out=yim[:, fc, :], in0=t[:, 2, :], in1=t[:, 3, :], op=ALU.add), reads=[tb], writes=[yb])
                    if fc == 0:
                        kb.op("dve", lambda: V.tensor_copy(out=yre[0:1, 0, :], in_=t[0:1, 0, :]), reads=[tb], writes=[yb])
                        kb.op("dve", lambda: V.tensor_copy(out=yim[0:1, 0, :], in_=t[0:1, 1, :]), reads=[tb], writes=[yb])
                for tb0 in range(0, L, TB):
                    cm, cmb = pc.get()
                    for ri, Cm in enumerate((Cre, Cim)):
                        kb.dma("pool", cm[:, ri], Cm[:, tb0:tb0 + TB].rearrange("(c p) t -> p c t", p=128), writes=[cmb])
                    o, ob = po.get()
                    for cc in range(4):
                        ps, psb = kb.ps()
                        for fc in range(ns):
                            kb.op("pe", lambda: nc.tensor.matmul(ps[:, :TB], yre[:, fc, cc * 128:(cc + 1) * 128], cm[:, 0, fc, :], start=(fc == 0), stop=False),
                                  reads=[yb, cmb], writes=[psb], signal=False)
                            kb.op("pe", lambda: nc.tensor.matmul(ps[:, :TB], yim[:, fc, cc * 128:(cc + 1) * 128], cm[:, 1, fc, :], start=False, stop=(fc == ns - 1)),
                                  reads=[yb, cmb], writes=[psb], signal=(fc == ns - 1))
                        kb.op("act", lambda: nc.scalar.copy(out=o[:, cc, :], in_=ps[:, :TB]), reads=[psb], writes=[ob])
                    kb.dma("sp", YTv[:, half * 4:(half + 1) * 4, t_off + tb0:t_off + tb0 + TB], o, reads=[ob])
        kb.barrier()

    def hy_combine_stage(self, YT_, VT, GT_, ZT, skip_idx):
        kb, nc = self.kb, self.nc
        with ExitStack() as es:
            sk = kb.tile(es, [128, KC, 2], F32, "hyskip")
            skb = Buf()
            kb.dma("sp", sk, self.ins["hyskip"], writes=[skb])
            p1 = Pool(kb, es, 2, [128, T], F32, "hb1")
            p2 = Pool(kb, es, 2, [128, T], F32, "hb2")
            p3 = Pool(kb, es, 2, [128, T], F32, "hb3")
            for c in range(KC):
                rows = slice(c * 128, (c + 1) * 128)
                y, yb = p1.get()
                v, vb = p2.get()
                g, gb = p3.get()
                kb.dma("sp", y, YT_[rows, :], writes=[yb])
                kb.dma("sp", v, VT[rows, :], writes=[vb])
                kb.dma("sp", g, GT_[rows, :], writes=[gb])
                kb.op("dve", lambda: nc.vector.scalar_tensor_tensor(out=y, in0=v, scalar=sk[:, c, skip_idx:skip_idx + 1], in1=y, op0=ALU.mult, op1=ALU.add),
                      reads=[vb, skb], writes=[yb])
                kb.op("pool", lambda: nc.gpsimd.tensor_tensor(out=y, in0=y, in1=g, op=ALU.mult), reads=[gb], writes=[yb])
                kb.dma("act", ZT[rows, :], y, reads=[yb])
        kb.barrier()

    def hyena_layer(self, HT, YT, S):
        kb, nc = self.kb, self.nc
        I = self.ins
        UT, UC, VTOK, Y1T, Z1T, Z1TOK, Y2T, Z2T, US = (S[k] for k in ("hUT", "hUC", "hVTOK", "hY1T", "hZ1T", "hZ1TOK", "hY2T", "hZ2T", "hUS"))
        vec_bias = {}
        with ExitStack() as esb:
            vec = kb.tile(esb, [128, 24, 5], F32, "hyvecb")
            vecb = Buf()
            kb.dma("sp", vec, I["hyvec"], writes=[vecb])
            self.linear_stage(HT, I["hy_w_in"][0], UT, 128, TT_ALL, mb=512, mod=(0, 1),
                              evac=lambda ps, o, mc, col, rd, wr: kb.op("act", lambda: nc.scalar.activation(out=o, in_=ps, func=AF.Identity, bias=vec[:, mc, 0:1]),
                                                                        reads=rd + [vecb], writes=wr))
        steps = []
        steps.append(lambda: self.hy_conv_stage(UT, UC))
        steps.append(lambda: self.to_tok_stage(UC[0:D, :], VTOK, D))
        cfg = {}
        for L in (CTX, SEQ):
            FILT = S["hFILT%d" % L]
            steps.append(lambda L=L, FILT=FILT: self.hy_filter_stage(L, I["hzT%d" % L], I["hwin%d" % L], FILT))
            HSs = []
            for k in range(2):
                HS = S["hHS%d_%d" % (L, k)]
                steps.append(lambda L=L, FILT=FILT, k=k, HS=HS: self.dft_fwd_stage(FILT[:, k * D:(k + 1) * D], L, I["Fre%d" % L], I["Fim%d" % L], HS))
                HSs.append(HS)
            cfg[L] = HSs
        for (L, off) in ((CTX, 0), (SEQ, CTX)):
            steps.append(lambda L=L, off=off: self.dft_fwd_stage(VTOK[off:off + L, :], L, I["Fre%d" % L], I["Fim%d" % L], US[:, 0:L, :]))
            steps.append(lambda L=L, off=off: self.dft_inv_stage(US[:, 0:L, :], cfg[L][0], L, I["Cre%d" % L], I["Cim%d" % L], Y1T, off))
        steps.append(lambda: self.hy_combine_stage(Y1T, UC[0:D, :], UC[D:2 * D, :], Z1T, 0))
        steps.append(lambda: self.to_tok_stage(Z1T, Z1TOK, D))
        for (L, off) in ((CTX, 0), (SEQ, CTX)):
            steps.append(lambda L=L, off=off: self.dft_fwd_stage(Z1TOK[off:off + L, :], L, I["Fre%d" % L], I["Fim%d" % L], US[:, 0:L, :]))
            steps.append(lambda L=L, off=off: self.dft_inv_stage(US[:, 0:L, :], cfg[L][1], L, I["Cre%d" % L], I["Cim%d" % L], Y2T, off))
        steps.append(lambda: self.hy_combine_stage(Y2T, Z1T, UC[2 * D:3 * D, :], Z2T, 1))
        steps.append(lambda: self.linear_stage(Z2T, I["hy_w_out"][0], YT, 128, TT_ALL, mb=1024))
        for st in steps[:getattr(self, "hy_stop", 999)]:
            st()

    def decl_hyena(self):
        for nm, shp in (("hy_w_in", [1, D, 3 * D]), ("hyvec", [128, 24, 5]), ("hy_f_w1", [1, 33, 64]), ("hy_f_w2", [1, 64, 64]), ("hy_f_w3", [1, 64, 2 * D]),
                        ("hyf", [64, 4]), ("hyskip", [128, KC, 2]), ("hy_w_out", [1, D, D])):
            self.inp(nm, shp)
        for L in (CTX, SEQ):
            self.inp("hzT%d" % L, [33, L])
            self.inp("hwin%d" % L, [L, D])
            for nm in ("Fre", "Fim", "Cre", "Cim"):
                self.inp("%s%d" % (nm, L), [L, L])
        S = {}
        for nm, shp in (("hUT", [3 * D, T]), ("hUC", [3 * D, T]), ("hVTOK", [T, D]), ("hY1T", [D, T]), ("hZ1T", [D, T]), ("hZ1TOK", [T, D]),
                        ("hY2T", [D, T]), ("hZ2T", [D, T]), ("hUS", [2, SEQ, D])):
            S[nm] = self.scratch(nm, shp)
        for L in (CTX, SEQ):
            S["hFILT%d" % L] = self.scratch("hFILT%d" % L, [L, 2 * D])
            for k in range(2):
                S["hHS%d_%d" % (L, k)] = self.scratch("hHS%d_%d" % (L, k), [2, L, D])
        return S

    def decl_all(self):
        for nm, shp in (("x", [SEQ, D]), ("ctx", [CTX, D]), ("pos", [SEQ, D]), ("ident", [128, 128]), ("cc", [128, KC, 2]),
                        ("ada_w", [DEPTH, D, 6 * D]), ("adab", [DEPTH, 128, 48]), ("lng", [128, DEPTH * 2 * KC]), ("lnb", [128, DEPTH * 2 * KC]),
                        ("routw", [128, KC, NE]), ("routb", [128, NE]), ("selE", [NE, NE, 128]),
                        ("moe_w_gate0", [DEPTH * NE * 128, 2048]), ("moe_w_gate1", [DEPTH * NE * 128, 2048]), ("moe_w_up0", [DEPTH * NE * 128, 2048]), ("moe_w_up1", [DEPTH * NE * 128, 2048]), ("moe_w_down0", [DEPTH * NE * 128, 2048]), ("moe_w_down1", [DEPTH * NE * 128, 2048]),
                        ("rg_w_in", [2, D, 2 * D_RNN]), ("rgv", [2, RG_BS, 16, 11]), ("rg_gate_a_w", [2, 2, 16, RG_BS, RG_BS]),
                        ("rg_gate_x_w", [2, 2, 16, RG_BS, RG_BS]), ("rg_w_out", [2, D_RNN, D]),
                        ("ml_w_up", [1, D, 2048]), ("ml_w_q", [1, 2048, D]), ("ml_w_k", [1, 2048, D]), ("ml_w_v", [1, 2048, 2048]),
                        ("ml_w_o", [1, 2048, 2048]), ("ml_w_if", [1, 2, 2048, 16]), ("ml_w_down", [1, 2048, D]),
                        ("mlvec", [128, 16, 7]), ("mlbif", [8, 4]), ("maskF", [128, 128]), ("maskB", [128, 128])):
            self.inp(nm, shp)
        self.inp("iota64", [NE, 64])
        self.inp("pidx", [128, 1])
        self.inp("triL", [NE, NE])
        self.XS = self.scratch("XS", [34 * 256, D])
        self.YS = self.scratch("YS", [34 * 256, D])
        S = self.decl_hyena()
        for nm, shp in (("XM", [2048, T]), ("XC", [2048, T]), ("QT", [D, T]), ("KT", [D, T]), ("KTOK", [T, D]), ("VTOK", [T, 2048]),
                        ("OT", [2048, T]), ("GT", [2, 2, 8, T]), ("HSF", [T, 2048]), ("HNT", [2048, T]), ("YPT", [2048, T]), ("MT", [D_RNN, T]), ("RG", [2 * D_RNN, T])):
            S[nm] = self.scratch(nm, shp)
        return S

    def mixer(self, layer, HT, YT, S):
        kind, jl = layer % 3, layer // 3
        if kind == 0:
            self.rglru_stage(HT, S["MT"], jl, S["RG"])
            self.linear_stage(S["MT"], self.ins["rg_w_out"][jl], YT, RG_BS, TT_ALL, mb=1024)
        elif kind == 1:
            self.mlstm_layer(HT, YT, S)
        else:
            self.hyena_layer(HT, YT, S)

    def build(self, stages=None):
        kb = self.kb
        self.es_global = ExitStack()
        S = self.decl_all()
        out = self.nc.dram_tensor("out", [SEQ, D], F32, kind="ExternalOutput").ap()
        HT = self.scratch("HT", [D, T])
        YT = self.scratch("YT", [D, T])
        self.persist()
        self.zero_scratch(self.XS, 34 * 256)
        self.prep(HT)
        if stages is None:
            for layer in range(self.n_layers):
                last = layer == DEPTH - 1
                self.mod_stage(layer)
                self.mixer(layer, HT, YT, S)
                self.ln_stage(HT, YT, layer, 0, TT_ALL)
                self.moe_sparse_stage(HT, YT, layer, with_ctx=not last)
                self.ln_stage(HT, YT, layer, 1, TT_X if last else TT_ALL)
        elif stages == "ln_test":
            self.mod_stage(1)
            self.prep(YT)
            self.ln_stage(HT, YT, 1, 1, TT_ALL)
        elif stages == "rg_test":
            self.mod_stage(0)
            self.mixer(0, HT, YT, S)
        elif stages == "ml_test":
            self.mod_stage(1)
            self.mixer(1, HT, YT, S)
        elif stages == "hy_test":
            self.mod_stage(2)
            self.mixer(2, HT, YT, S)
        elif stages == "moe_test":
            self.mod_stage(0)
            self.moe_stage(HT, YT, 0, True)
        elif stages == "moes_test":
            self.mod_stage(0)
            self.moe_sparse_stage(HT, YT, 0, True)
        elif stages == "moes_test_x":
            self.mod_stage(0)
            self.moe_sparse_stage(HT, YT, 0, False)
        self.final(HT, out)
        kb.barrier()
        self.es_global.close()
        return self.nc


def host_consts():
    c = {}
    c["ident"] = np.eye(128, dtype=np.float32)
    rows = SEQ // 64
    quarter = D // 4
    omega = (1.0 / (10000.0 ** (np.arange(quarter, dtype=np.float32) / np.float32(quarter)))).astype(np.float32)
    ar = np.arange(rows, dtype=np.float32)[:, None] * omega
    ac = np.arange(64, dtype=np.float32)[:, None] * omega
    er = np.concatenate([np.sin(ar), np.cos(ar)], -1)
    ec = np.concatenate([np.sin(ac), np.cos(ac)], -1)
    half = D // 2
    pos = np.concatenate([np.broadcast_to(er[:, None], (rows, 64, half)), np.broadcast_to(ec[None], (rows, 64, half))], -1)
    c["pos"] = np.ascontiguousarray(pos.reshape(SEQ, D).astype(np.float32))
    sel = np.zeros((NE, NE, 128), np.float32)
    for e in range(NE):
        sel[e, e, :] = 1.0
    c["selE"] = sel
    c["maskF"] = np.ascontiguousarray(np.triu(np.ones((128, 128), np.float32)))
    c["maskB"] = np.ascontiguousarray(np.tril(np.ones((128, 128), np.float32)))
    c["pidx"] = np.arange(128, dtype=np.float32).reshape(128, 1)
    c["iota64"] = np.ascontiguousarray(np.broadcast_to(np.arange(64, dtype=np.float32)[None, :], (NE, 64)))
    c["triL"] = np.ascontiguousarray(np.triu(np.ones((NE, NE), np.float32), 1))
    for L in (CTX, SEQ):
        n = 2 * L
        t01 = np.linspace(0.0, 1.0, L, dtype=np.float32)
        bands = np.linspace(1e-4, 15.0, 16, dtype=np.float32)
        ang = (np.float32(2.0 * math.pi / L) * np.arange(L, dtype=np.float32)[:, None]) * bands[None, :]
        z = np.concatenate([t01[:, None], np.cos(ang), -np.sin(ang)], -1).astype(np.float32)
        c["hzT%d" % L] = np.ascontiguousarray(z.T)
        dist = np.abs(np.arange(L) - L // 2).astype(np.float32) * np.float32(2.0 / L)
        d_max = math.log(1e-2) / 0.3
        d_min = math.log(1e-2) / 1.5
        deltas = np.abs(np.linspace(d_min, d_max, D, dtype=np.float32))
        c["hwin%d" % L] = np.ascontiguousarray(np.exp(-dist[:, None] * deltas[None, :]).astype(np.float32))
        sidx = np.arange(L, dtype=np.int64)
        fidx = np.arange(L, dtype=np.int64)
        th = 2.0 * np.pi * ((sidx[:, None] * fidx[None, :]) % n).astype(np.float64) / n
        Fre = np.cos(th)
        Fim = -np.sin(th)
        Fim[:, 0] = (-1.0) ** sidx
        tau = sidx + L // 2
        th2 = 2.0 * np.pi * ((fidx[:, None] * tau[None, :]) % n).astype(np.float64) / n
        Cre = (2.0 / n) * np.cos(th2)
        Cim = -(2.0 / n) * np.sin(th2)
        Cre[0, :] = 1.0 / n
        Cim[0, :] = (1.0 / n) * ((-1.0) ** tau)
        for nm, a in (("Fre", Fre), ("Fim", Fim), ("Cre", Cre), ("Cim", Cim)):
            c["%s%d" % (nm, L)] = np.ascontiguousarray(a.astype(np.float32))
    return c


def host_inputs(inputs, b, consts):
    f = lambda a: np.ascontiguousarray(np.asarray(a, dtype=np.float32))
    m = {}
    m["x"] = f(inputs["x"][b])
    m["ctx"] = f(inputs["ctx"][b])
    m["pos"] = consts["pos"]
    m["ident"] = consts["ident"]
    cc = np.stack([np.asarray(inputs["c"][b]), np.asarray(inputs["c_ctx"])], -1)
    m["cc"] = f(cc.reshape(KC, 128, 2).transpose(1, 0, 2))
    m["ada_w"] = f(inputs["ada_w"])
    m["adab"] = f(np.asarray(inputs["ada_b"]).reshape(DEPTH, 48, 128).transpose(0, 2, 1))
    m["lng"] = f(np.asarray(inputs["ln_g"]).reshape(DEPTH * 2 * KC, 128).T)
    m["lnb"] = f(np.asarray(inputs["ln_b"]).reshape(DEPTH * 2 * KC, 128).T)
    m["routw"] = f(np.asarray(inputs["router_w"]).reshape(KC, 128, NE).transpose(1, 0, 2))
    m["routb"] = f(np.broadcast_to(np.asarray(inputs["router_b"])[None, :], (128, NE)))
    m["selE"] = consts["selE"]
    for k in ("rg_w_in", "rg_gate_a_w", "rg_gate_x_w", "rg_w_out"):
        m[k] = f(inputs[k])
    for k in ("moe_w_gate", "moe_w_up"):
        a = f(inputs[k]).reshape(DEPTH * NE, 2, 4, 128, DEXP)
        for hh in range(2):
            m[k + str(hh)] = np.ascontiguousarray(a[:, hh].transpose(0, 2, 1, 3)).reshape(DEPTH * NE * 128, 4 * DEXP)
    a = f(inputs["moe_w_down"]).reshape(DEPTH * NE, 2, 2, 128, D)
    for hh in range(2):
        m["moe_w_down" + str(hh)] = np.ascontiguousarray(a[:, hh].transpose(0, 2, 1, 3)).reshape(DEPTH * NE * 128, 2 * D)
    A = np.asarray
    na = A(inputs["rg_conv_w"]).shape[0]
    vecs = [A(inputs["rg_conv_w"])[:, k] for k in range(4)] + [A(inputs["rg_conv_b"])]
    vecs += [A(inputs["rg_gate_a_b"])[:, z] for z in range(2)] + [A(inputs["rg_gate_x_b"])[:, z] for z in range(2)]
    vecs += [A(inputs["rg_lambda"])[:, z] for z in range(2)]
    rgv = np.stack(vecs, -1)
    m["rgv"] = f(rgv.reshape(na, 16, RG_BS, 11).transpose(0, 2, 1, 3))
    for k in ("ml_w_up", "ml_w_q", "ml_w_k", "ml_w_v", "ml_w_o", "ml_w_if", "ml_w_down"):
        m[k] = f(inputs[k])
    mv = [A(inputs["ml_conv_w"])[0, k] for k in range(4)] + [A(inputs["ml_conv_b"])[0], A(inputs["ml_norm_g"])[0], A(inputs["ml_skip"])[0]]
    m["mlvec"] = f(np.stack(mv, -1).reshape(16, 128, 7).transpose(1, 0, 2))
    bif = A(inputs["ml_b_if"])[0]
    m["mlbif"] = f(bif.reshape(2, 2, 8).transpose(2, 0, 1).reshape(8, 4))
    m["maskF"] = consts["maskF"]
    m["maskB"] = consts["maskB"]
    m["iota64"] = consts["iota64"]
    m["pidx"] = consts["pidx"]
    m["triL"] = consts["triL"]
    for k in ("hy_w_in", "hy_f_w1", "hy_f_w2", "hy_f_w3", "hy_w_out"):
        m[k] = f(inputs[k])
    hv = [A(inputs["hy_b_in"])[0]] + [A(inputs["hy_conv_w"])[0, k] for k in range(3)] + [A(inputs["hy_conv_b"])[0]]
    m["hyvec"] = f(np.stack(hv, -1).reshape(24, 128, 5).transpose(1, 0, 2))
    m["hyf"] = f(np.stack([A(inputs["hy_f_b1"])[0], A(inputs["hy_f_freq1"])[0], A(inputs["hy_f_b2"])[0], A(inputs["hy_f_freq2"])[0]], -1))
    m["hyskip"] = f(A(inputs["hy_skip"])[0].reshape(2, KC, 128).transpose(2, 1, 0))
    for k, v in consts.items():
        if k[0] in "hFC" and k not in m:
            m[k] = v
    return m


_CACHE = {}


def kernel(**inputs):
    consts = host_consts()
    prog = Prog()
    nc = prog.build()
    shared = None
    in_maps = []
    for b in range(8):
        m = host_inputs(inputs, b, consts) if shared is None else dict(shared)
        if shared is None:
            shared = m
        else:
            m["x"] = np.ascontiguousarray(np.asarray(inputs["x"][b], dtype=np.float32))
            m["ctx"] = np.ascontiguousarray(np.asarray(inputs["ctx"][b], dtype=np.float32))
            cc = np.stack([np.asarray(inputs["c"][b]), np.asarray(inputs["c_ctx"])], -1)
            m["cc"] = np.ascontiguousarray(cc.reshape(KC, 128, 2).transpose(1, 0, 2).astype(np.float32))
        in_maps.append({k: v for k, v in m.items() if k in prog.ins})
    res = run_bass_kernel_spmd(nc, in_maps, core_ids=list(range(8)))
    return np.stack([np.asarray(r["out"], dtype=np.float32) for r in res.results], 0)
```
